# Optimizing a Trainium2 kernel written in Bass

```python
import math
import jax, jax.numpy as jnp
from jax import lax
import numpy as np

D_MODEL = 1024
BATCH = 8
SEQ = 4096
DEPTH = 1

ATTN_HEADS = 4
ATTN_HEAD_DIM = 64
ATTN_V_DIM = 2 * ATTN_HEAD_DIM
ATTN_WIDTH = ATTN_HEADS * ATTN_V_DIM
QK_WIDTH = ATTN_HEADS * 2 * ATTN_HEAD_DIM
ROPE_THETA = 500000.0
ROPE_DIM = ATTN_HEAD_DIM // 4
Q_BLOCK = 128
LRU_WIDTH = D_MODEL - ATTN_WIDTH
LRU_BLOCKS = 8
LRU_BLOCK_DIM = LRU_WIDTH // LRU_BLOCKS
CONV_WIDTH = 4
LRU_C = 8.0
N_DIRS = 2
IN_WIDTH = 2 * QK_WIDTH + ATTN_WIDTH + 2 * LRU_WIDTH
N_GROUPS = 4
EXPERTS_PER_GROUP = 8
N_EXPERTS = N_GROUPS * EXPERTS_PER_GROUP
TOP_K = 2
D_EXPERT = 256
EPS = 1e-6

kernel_name = 'hybrid_diffattn_rglru_hmoe_encoder'


def rmsnorm(x, g):
    xf = x.astype(jnp.float32)
    y = xf * lax.rsqrt(jnp.mean(xf * xf, axis=-1, keepdims=True) + EPS)
    return (y * g.astype(jnp.float32)).astype(x.dtype)


def rope_tables(seq_len):
    pos = jnp.arange(seq_len, dtype=jnp.float32)
    inv_freq = ROPE_THETA ** (-jnp.arange(0, ROPE_DIM, 2, dtype=jnp.float32) / ROPE_DIM)
    ang = pos[:, None] * inv_freq[None, :]
    return jnp.cos(ang), jnp.sin(ang)


def partial_rope(t, cos, sin):
    c = cos[None, :, None, None, :].astype(t.dtype)
    s = sin[None, :, None, None, :].astype(t.dtype)
    half = ROPE_DIM // 2
    t1, t2, rest = t[..., :half], t[..., half:ROPE_DIM], t[..., ROPE_DIM:]
    return jnp.concatenate([t1 * c - t2 * s, t2 * c + t1 * s, rest], axis=-1)


def diff_attention(q, k, v, lam, lambda_init, subln_g):
    B, S = q.shape[0], q.shape[1]
    n_blk = S // Q_BLOCK
    q = q * (ATTN_HEAD_DIM ** -0.5)
    qb = q.reshape(B, n_blk, Q_BLOCK, ATTN_HEADS, 2, ATTN_HEAD_DIM).transpose(1, 0, 2, 3, 4, 5)

    def one_block(q_blk):
        s = jnp.einsum('bqhmd,bkhmd->bhmqk', q_blk, k).astype(jnp.float32)
        p = jax.nn.softmax(s, axis=-1)
        a = p[:, :, 0] - lam * p[:, :, 1]
        return jnp.einsum('bhqk,bkhe->bqhe', a.astype(v.dtype), v)

    o = lax.map(one_block, qb)
    o = o.transpose(1, 0, 2, 3, 4).reshape(B, S, ATTN_HEADS, ATTN_V_DIM)
    o = rmsnorm(o, subln_g) * (1.0 - lambda_init)
    return o.reshape(B, S, ATTN_WIDTH)


def centred_depthwise_conv(x, w, b):
    C = x.shape[-1]
    left = CONV_WIDTH // 2
    y = lax.conv_general_dilated(
        x, w[:, None, :].astype(x.dtype), window_strides=(1,),
        padding=[(left, CONV_WIDTH - 1 - left)],
        dimension_numbers=('NWC', 'WIO', 'NWC'), feature_group_count=C)
    return y + b.astype(x.dtype)


def _linear_recurrence(c1, c2):
    a1, b1 = c1
    a2, b2 = c2
    return a1 * a2, a2 * b1 + b2


def rg_lru(x, w_r, b_r, w_i, b_i, lam_param, reverse):
    B, S, C = x.shape
    xf = x.astype(jnp.float32)
    xb = xf.reshape(B, S, LRU_BLOCKS, LRU_BLOCK_DIM)
    r = jax.nn.sigmoid(jnp.einsum('bshi,hij->bshj', xb, w_r.astype(jnp.float32)).reshape(B, S, C) + b_r)
    i = jax.nn.sigmoid(jnp.einsum('bshi,hij->bshj', xb, w_i.astype(jnp.float32)).reshape(B, S, C) + b_i)
    log_a = -LRU_C * jax.nn.softplus(-lam_param.astype(jnp.float32)) * r
    a = jnp.exp(log_a)
    u = jnp.sqrt(-jnp.expm1(2.0 * log_a)) * (i * xf)
    _, h = lax.associative_scan(_linear_recurrence, (a, u), axis=1, reverse=reverse)
    return h


def hierarchical_moe(h, w_grp, w_exp, w_gate, w_up, w_down):
    B, S, D = h.shape
    t = h.reshape(B * S, D)
    g_prob = jax.nn.softmax((t @ w_grp).astype(jnp.float32), axis=-1)
    g_top_p, g_idx = lax.top_k(g_prob, 1)
    e_logits = (t @ w_exp).astype(jnp.float32).reshape(B * S, N_GROUPS, EXPERTS_PER_GROUP)
    e_in_grp = jnp.take_along_axis(e_logits, g_idx[:, :, None], axis=1)[:, 0]
    e_top_logit, e_idx = lax.top_k(e_in_grp, TOP_K)
    e_w = jax.nn.softmax(e_top_logit, axis=-1) * g_top_p
    global_idx = g_idx * EXPERTS_PER_GROUP + e_idx
    comb = jnp.sum(jax.nn.one_hot(global_idx, N_EXPERTS, dtype=jnp.float32) * e_w[..., None], axis=1)
    comb = comb.astype(t.dtype)
    out = jnp.zeros_like(t)
    for e in range(N_EXPERTS):
        hid = jax.nn.silu(t @ w_gate[e]) * (t @ w_up[e])
        out = out + comb[:, e:e + 1] * (hid @ w_down[e])
    return out.reshape(B, S, D)


def setup_inputs(seed: int = 0) -> dict:
    key = jax.random.key(seed)
    ks = jax.random.split(key, 24)
    f32 = jnp.float32
    nrm = lambda k, shape, scale: jax.random.normal(k, shape, f32) * scale
    a_c = jax.random.uniform(ks[15], (DEPTH, N_DIRS, LRU_WIDTH), f32, 0.9, 0.999)
    a0 = a_c ** (1.0 / LRU_C)
    lru_lambda = jnp.log(a0) - jnp.log1p(-a0)
    return {
        'x': jax.random.normal(ks[0], (BATCH, SEQ, D_MODEL), f32),
        'norm1_g': 1.0 + nrm(ks[1], (DEPTH, D_MODEL), 0.02),
        'w_in': nrm(ks[2], (DEPTH, D_MODEL, IN_WIDTH), D_MODEL ** -0.5),
        'lambda_q1': nrm(ks[3], (DEPTH, ATTN_HEAD_DIM), 0.1),
        'lambda_k1': nrm(ks[4], (DEPTH, ATTN_HEAD_DIM), 0.1),
        'lambda_q2': nrm(ks[5], (DEPTH, ATTN_HEAD_DIM), 0.1),
        'lambda_k2': nrm(ks[6], (DEPTH, ATTN_HEAD_DIM), 0.1),
        'subln_g': 1.0 + nrm(ks[7], (DEPTH, ATTN_V_DIM), 0.02),
        'conv_w': nrm(ks[8], (DEPTH, CONV_WIDTH, LRU_WIDTH), CONV_WIDTH ** -0.5),
        'conv_b': nrm(ks[9], (DEPTH, LRU_WIDTH), 0.01),
        'lru_w_r': nrm(ks[10], (DEPTH, N_DIRS, LRU_BLOCKS, LRU_BLOCK_DIM, LRU_BLOCK_DIM), LRU_BLOCK_DIM ** -0.5),
        'lru_b_r': nrm(ks[11], (DEPTH, N_DIRS, LRU_WIDTH), 0.01),
        'lru_w_i': nrm(ks[12], (DEPTH, N_DIRS, LRU_BLOCKS, LRU_BLOCK_DIM, LRU_BLOCK_DIM), LRU_BLOCK_DIM ** -0.5),
        'lru_b_i': nrm(ks[13], (DEPTH, N_DIRS, LRU_WIDTH), 0.01),
        'lru_lambda': lru_lambda,
        'w_out': nrm(ks[14], (DEPTH, D_MODEL, D_MODEL), D_MODEL ** -0.5),
        'norm2_g': 1.0 + nrm(ks[16], (DEPTH, D_MODEL), 0.02),
        'w_grp': nrm(ks[17], (DEPTH, D_MODEL, N_GROUPS), D_MODEL ** -0.5),
        'w_exp': nrm(ks[18], (DEPTH, D_MODEL, N_EXPERTS), D_MODEL ** -0.5),
        'w_gate': nrm(ks[19], (DEPTH, N_EXPERTS, D_MODEL, D_EXPERT), D_MODEL ** -0.5),
        'w_up': nrm(ks[20], (DEPTH, N_EXPERTS, D_MODEL, D_EXPERT), D_MODEL ** -0.5),
        'w_down': nrm(ks[21], (DEPTH, N_EXPERTS, D_EXPERT, D_MODEL), D_EXPERT ** -0.5),
        'final_g': 1.0 + nrm(ks[22], (D_MODEL,), 0.02),
    }


def reference(x, norm1_g, w_in, lambda_q1, lambda_k1, lambda_q2, lambda_k2, subln_g,
              conv_w, conv_b, lru_w_r, lru_b_r, lru_w_i, lru_b_i, lru_lambda, w_out,
              norm2_g, w_grp, w_exp, w_gate, w_up, w_down, final_g):
    B, S, _ = x.shape
    cos, sin = rope_tables(S)
    for l in range(DEPTH):
        lambda_init = 0.8 - 0.6 * math.exp(-0.3 * l)
        h = rmsnorm(x, norm1_g[l])
        proj = h @ w_in[l]
        q, k, v, xr, gate = jnp.split(
            proj, [QK_WIDTH, 2 * QK_WIDTH, 2 * QK_WIDTH + ATTN_WIDTH,
                   2 * QK_WIDTH + ATTN_WIDTH + LRU_WIDTH], axis=-1)
        q = partial_rope(q.reshape(B, S, ATTN_HEADS, 2, ATTN_HEAD_DIM), cos, sin)
        k = partial_rope(k.reshape(B, S, ATTN_HEADS, 2, ATTN_HEAD_DIM), cos, sin)
        v = v.reshape(B, S, ATTN_HEADS, ATTN_V_DIM)
        lam = (jnp.exp(jnp.sum(lambda_q1[l].astype(jnp.float32) * lambda_k1[l].astype(jnp.float32)))
               - jnp.exp(jnp.sum(lambda_q2[l].astype(jnp.float32) * lambda_k2[l].astype(jnp.float32)))
               + lambda_init)
        attn_out = diff_attention(q, k, v, lam, lambda_init, subln_g[l])
        xc = centred_depthwise_conv(xr, conv_w[l], conv_b[l])
        h_fwd = rg_lru(xc, lru_w_r[l, 0], lru_b_r[l, 0], lru_w_i[l, 0], lru_b_i[l, 0], lru_lambda[l, 0], False)
        h_bwd = rg_lru(xc, lru_w_r[l, 1], lru_b_r[l, 1], lru_w_i[l, 1], lru_b_i[l, 1], lru_lambda[l, 1], True)
        rnn_out = (h_fwd + h_bwd).astype(x.dtype) * jax.nn.gelu(gate)
        mix = jnp.concatenate([attn_out, rnn_out], axis=-1) @ w_out[l]
        x = x + mix
        h = rmsnorm(x, norm2_g[l])
        x = x + hierarchical_moe(h, w_grp[l], w_exp[l], w_gate[l], w_up[l], w_down[l])
    return rmsnorm(x, final_g)
```

```python
import math
from contextlib import ExitStack

import numpy as np
import concourse.bass as bass
import concourse.mybir as mybir
from concourse.bass_utils import run_bass_kernel_spmd

F32 = mybir.dt.float32
BF16 = mybir.dt.bfloat16
AF = mybir.ActivationFunctionType
ALU = mybir.AluOpType
AX = mybir.AxisListType

I32 = mybir.dt.int32
_DTSZ = {F32: 4, BF16: 2, I32: 4}

S = 4096
D = 1024
NT = 32
NCH = 8
NE = 32
EPS = 1e-6


def _rect(ap):
    t = ap.tensor
    sz = _DTSZ.get(ap.dtype, 4)
    dims = ap.ap
    lo = 0
    hi = 0
    space = str(ap.space).upper()
    if not ('SB' in space or 'PSUM' in space):
        for st, cnt in dims:
            if st >= 0:
                hi += st * (cnt - 1)
            else:
                lo += st * (cnt - 1)
        off = ap.offset
        return (t.name, 0, 1, (off + lo) * sz, (off + hi + 1) * sz)
    pstride = 1
    for s in list(t.shape)[1:]:
        pstride *= s
    off = ap.offset
    p0 = off // pstride
    f0 = off % pstride
    pcnt = dims[0][1]
    for st, cnt in dims[1:]:
        if st >= 0:
            hi += st * (cnt - 1)
        else:
            lo += st * (cnt - 1)
    return (t.name, p0, p0 + pcnt, (f0 + lo) * sz, (f0 + hi + 1) * sz)


def _overlap(a, b):
    return a[1] < b[2] and b[1] < a[2] and a[3] < b[4] and b[3] < a[4]


def _covers(a, b):
    return a[1] <= b[1] and a[2] >= b[2] and a[3] <= b[3] and a[4] >= b[4]


class Op:
    __slots__ = ('idx', 'eng', 'fn', 'deps', 'is_dma', 'signal', 'count', 'dsem', 'dcount', 'dprev')

    def __init__(self, idx, eng, fn, is_dma):
        self.idx = idx
        self.eng = eng
        self.fn = fn
        self.is_dma = is_dma
        self.deps = set()
        self.signal = False
        self.count = 0
        self.dsem = None
        self.dcount = 0
        self.dprev = 0


class Prog:
    ENGINES = ['sync', 'scalar', 'vector', 'gpsimd', 'tensor']

    def __init__(self, nc, n_dma_sems=32, same_engine_sync=True):
        self.nc = nc
        self.ops = []
        self.recs = {}
        self.n_dma_sems = n_dma_sems
        self.same_engine_sync = same_engine_sync

    def op(self, eng, fn, reads=(), writes=(), is_dma=False, partial=False):
        o = Op(len(self.ops), eng, fn, is_dma)
        self.ops.append(o)
        rrs = [_rect(ap) for ap in reads]
        wrs = [_rect(ap) for ap in writes]
        for r in rrs:
            for rec in self.recs.setdefault(r[0], []):
                if rec[2] and _overlap(rec[0], r):
                    o.deps.add(rec[1])
        for r in wrs:
            for rec in self.recs.setdefault(r[0], []):
                if _overlap(rec[0], r):
                    if partial and rec[2] and len(rec) > 4 and rec[4]:
                        continue
                    o.deps.add(rec[1])
        o.deps.discard(o.idx)
        for r in rrs:
            lst = self.recs[r[0]]
            done = False
            if not is_dma:
                for rec in lst:
                    if (not rec[2]) and rec[3] == eng and rec[0] == r and not self.ops[rec[1]].is_dma:
                        rec[1] = o.idx
                        done = True
                        break
            if not done:
                lst.append([r, o.idx, False, eng])
        for r in wrs:
            lst = self.recs[r[0]]
            if not partial:
                lst[:] = [rec for rec in lst if not (_covers(r, rec[0]) and rec[1] != o.idx)]
            lst.append([r, o.idx, True, eng, partial])
        return o

    def dma(self, out, in_, eng='sync'):
        return self.op(eng, lambda e: e.dma_start(out=out, in_=in_), reads=[in_], writes=[out], is_dma=True)

    def mm(self, out, lhsT, rhs, start=True, stop=True, **kw):
        rd = [lhsT, rhs] + ([] if start else [out])
        return self.op('tensor', lambda e: e.matmul(out, lhsT, rhs, start=start, stop=stop, **kw),
                       reads=rd, writes=[out])

    def transpose(self, out, in_, ident):
        return self.op('tensor', lambda e: e.transpose(out, in_, ident), reads=[in_, ident], writes=[out])

    def act(self, out, in_, func, bias=None, scale=None, accum_out=None):
        kw = {}
        rd = [in_]
        wr = [out]
        if bias is not None:
            kw['bias'] = bias
            if not isinstance(bias, (int, float)):
                rd.append(bias)
        if scale is not None:
            kw['scale'] = scale
            if not isinstance(scale, (int, float)):
                rd.append(scale)
        if accum_out is not None:
            kw['accum_out'] = accum_out
            wr.append(accum_out)
        return self.op('scalar', lambda e: e.activation(out, in_, func, **kw), reads=rd, writes=wr)

    def tt(self, out, in0, in1, op, eng='vector'):
        return self.op(eng, lambda e: e.tensor_tensor(out, in0, in1, op), reads=[in0, in1], writes=[out])

    def ts(self, out, in0, s1, s2, op0, op1=None, eng='vector'):
        rd = [in0]
        if not isinstance(s1, (int, float)):
            rd.append(s1)
        if s2 is not None and not isinstance(s2, (int, float)):
            rd.append(s2)
        kw = {}
        if op1 is not None:
            kw['op1'] = op1
        return self.op(eng, lambda e: e.tensor_scalar(out, in0, s1, s2, op0, **kw), reads=rd, writes=[out])

    def stt(self, out, in0, scalar, in1, op0, op1, eng='vector'):
        rd = [in0, in1]
        if not isinstance(scalar, (int, float)):
            rd.append(scalar)
        return self.op(eng, lambda e: e.scalar_tensor_tensor(out, in0, scalar, in1, op0, op1),
                       reads=rd, writes=[out])

    def copy(self, out, in_, eng='vector'):
        return self.op(eng, lambda e: e.tensor_copy(out, in_), reads=[in_], writes=[out])

    def memset(self, out, val, eng='vector'):
        return self.op(eng, lambda e: e.memset(out, val), reads=[], writes=[out])

    def acopy(self, out, in_):
        return self.op('scalar', lambda e: e.copy(out, in_), reads=[in_], writes=[out])

    def recip(self, out, in_):
        return self.op('vector', lambda e: e.reciprocal(out, in_), reads=[in_], writes=[out])

    def emit(self):
        nc = self.nc
        ops = self.ops
        ses = self.same_engine_sync
        for o in ops:
            for d in o.deps:
                p = ops[d]
                if p.is_dma:
                    continue
                if p.eng != o.eng or o.is_dma or (ses and p.eng != 'tensor'):
                    p.signal = True
        cnt = {e: 0 for e in self.ENGINES}
        for o in ops:
            if o.is_dma:
                continue
            if o.signal:
                cnt[o.eng] += 1
            o.count = cnt[o.eng]
        dcum = [0] * self.n_dma_sems
        k = 0
        for o in ops:
            if o.is_dma:
                s = k % self.n_dma_sems
                k += 1
                o.dsem = s
                o.dprev = dcum[s]
                dcum[s] += 16
                o.dcount = dcum[s]
        with ExitStack() as es:
            esem = {e: es.enter_context(nc.semaphore('s_' + e)) for e in self.ENGINES}
            dsems = [es.enter_context(nc.semaphore('d_%d' % i)) for i in range(self.n_dma_sems)]
            block = es.enter_context(nc.Block())
            per_eng = {e: [o for o in ops if o.eng == e] for e in self.ENGINES}
            final_counts = list(dcum)

            def make(ename):
                def body(eng):
                    waited = {}
                    for o in per_eng[ename]:
                        need = {}
                        for d in o.deps:
                            p = ops[d]
                            if p.is_dma:
                                key = ('d', p.dsem)
                                v = p.dcount
                            else:
                                if p.eng == ename and not o.is_dma:
                                    if ename == 'tensor' or not ses:
                                        continue
                                if not p.signal:
                                    continue
                                key = ('e', p.eng)
                                v = p.count
                            if need.get(key, 0) < v:
                                need[key] = v
                        if o.is_dma and o.dprev > 0:
                            key = ('d', o.dsem)
                            if need.get(key, 0) < o.dprev:
                                need[key] = o.dprev
                        for key, v in need.items():
                            if waited.get(key, 0) >= v:
                                continue
                            waited[key] = v
                            sem = dsems[key[1]] if key[0] == 'd' else esem[key[1]]
                            eng.wait_ge(sem, v)
                        ins = o.fn(eng)
                        if o.is_dma:
                            ins.then_inc(dsems[o.dsem], 16)
                        elif o.signal:
                            ins.then_inc(esem[ename], 1)
                    if ename == 'sync':
                        for i, c in enumerate(final_counts):
                            if c > 0 and waited.get(('d', i), 0) < c:
                                eng.wait_ge(dsems[i], c)
                return body

            block.sync(make('sync'))
            block.scalar(make('scalar'))
            block.vector(make('vector'))
            block.gpsimd(make('gpsimd'))
            block.tensor(make('tensor'))


def pipeline(n, stages):
    for it in range(n + len(stages) - 1):
        for k, f in enumerate(stages):
            t = it - k
            if 0 <= t < n:
                f(t)


class Arena:
    def __init__(self, A, nwords):
        self.A = A
        self.n = nwords
        self.top = 0

    def f32(self, n):
        off = self.top
        self.top += n
        assert self.top <= self.n, ("arena overflow", self.top, self.n)
        return self.A[:, off:off + n]

    def bf16(self, n):
        w = (n + 1) // 2
        off = self.top
        self.top += w
        assert self.top <= self.n, ("arena overflow", self.top, self.n)
        return self.A[:, off:off + w].bitcast(BF16)

    def sub(self, start, n):
        a = Arena(self.A, start + n)
        a.top = start
        return a

    def mark(self):
        return self.top

    def release(self, m):
        self.top = m


def build_nc(phases=6, debug=False, stop6=99, subB=99):
    nc = bass.Bass("TRN2", target_bir_lowering=False)

    def din(name, shape, dt=F32):
        return nc.dram_tensor(name, list(shape), dt, kind="ExternalInput").ap()

    def dscr(name, shape, dt=F32):
        return nc.dram_tensor(name, list(shape), dt, kind="Internal").ap()

    x = din("x", [S, D])
    w_in = din("w_in", [D, 2560])
    perm_d = din("perm", [128, 128])
    g1rep_d = din("g1rep", [128, D])
    g2rep_d = din("g2rep", [128, D])
    fgrep_d = din("fgrep", [128, D])
    ropec_d = din("ropec", [128, S])
    ropes_d = din("ropes", [128, S])
    lam4_d = din("lam4", [128, 4, 64])
    subln_d = din("subln", [128, 1])
    convw_d = din("convw", [128, 4, 4])
    convb_d = din("convb", [128, 4])
    wbd_d = din("wbd", [128, 16, 128])
    lrub_d = din("lrub", [128, 16])
    lrul_d = din("lrul", [128, 8])
    w_out = din("w_out", [D, D])
    w_rt = din("w_rt", [D, 36])
    w_gate = din("w_gate", [NE, D, 256])
    w_up = din("w_up", [NE, D, 256])
    w_down = din("w_down", [NE, 256, D])
    ident_d = din("ident", [128, 128])
    out = nc.dram_tensor("out", [S, D], F32, kind="ExternalOutput").ap()

    hT_d = dscr("hT_d", [8, 128, S], BF16)
    rnn_d = dscr("rnn_d", [4, 128, S], BF16)
    x1_d = dscr("x1_d", [S, D])
    h2_d = dscr("h2_d", [S, D], BF16)
    xs_d = dscr("xs_d", [64 * 256, D], BF16)
    ys_d = dscr("ys_d", [64 * 256, D], BF16)
    wall_d = dscr("wall_d", [NE * 128, 6144], BF16)
    pidx_d = din("pidx", [128, 1])
    umat_d = din("umat", [128, 128])
    thr_d = din("thr", [128, 64])

    dbg = {}
    if debug:
        dbg['attnT'] = nc.dram_tensor("dbg_attnT", [128, 4 * S], F32, kind="ExternalOutput").ap()
        dbg['rnnT'] = nc.dram_tensor("dbg_rnnT", [128, 4 * S], F32, kind="ExternalOutput").ap()
        dbg['qT'] = nc.dram_tensor("dbg_qT", [128, 4 * S], F32, kind="ExternalOutput").ap()
        dbg['kT'] = nc.dram_tensor("dbg_kT", [128, 4 * S], F32, kind="ExternalOutput").ap()
        dbg['v'] = nc.dram_tensor("dbg_v", [128, 32 * 512], F32, kind="ExternalOutput").ap()
        dbg['x1'] = nc.dram_tensor("dbg_x1", [S, D], F32, kind="ExternalOutput").ap()
        dbg['route'] = nc.dram_tensor("dbg_route", [128, 192], F32, kind="ExternalOutput").ap()
        dbg['lg'] = nc.dram_tensor("dbg_lg", [128, 32 * 36], F32, kind="ExternalOutput").ap()

    NW = 53000
    with ExitStack() as es:
        A = es.enter_context(nc.sbuf_tensor("A", [128, NW], F32))
        PS = es.enter_context(nc.psum_tensor("PS", [128, 8, 512], F32))
        P = Prog(nc)
        ar = Arena(A, NW)

        def bank(i):
            return PS[:, i, :]

        def bank_bf(i):
            return PS[:, i, :].bitcast(BF16)

        ident = ar.bf16(128)
        ones = ar.bf16(128)
        g1rep = ar.f32(D)
        epsT = ar.f32(1)
        oneT = ar.f32(1)
        small = ar.f32(64)
        cst = ar.f32(8)
        lrub = ar.f32(16)
        convw = ar.f32(16)
        convb = ar.f32(4)
        subg = ar.f32(1)
        neglam = ar.f32(1)
        base = ar.mark()

        m = ar.mark()
        tmpI = ar.f32(128)
        P.dma(tmpI, ident_d)
        P.copy(ident, tmpI)
        P.memset(ones, 1.0)
        P.memset(epsT, EPS)
        P.memset(oneT, 1.0)
        P.dma(g1rep, g1rep_d)
        P.dma(lrub, lrub_d)
        P.dma(convw, convw_d.rearrange("p a b -> p (a b)"))
        P.dma(convb, convb_d)
        P.dma(subg, subln_d)
        lrul = ar.f32(8)
        P.dma(lrul, lrul_d)
        P.act(lrul, lrul, AF.Exp, scale=-1.0)
        P.ts(lrul, lrul, 1.0, None, ALU.add)
        P.act(lrul, lrul, AF.Ln)
        P.ts(cst, lrul, -8.0, None, ALU.mult)
        lam4 = ar.f32(256)
        P.dma(lam4, lam4_d.rearrange("p a b -> p (a b)"))
        pr = ar.f32(128)
        P.tt(pr[:, 0:64], lam4[:, 0:64], lam4[:, 64:128], ALU.mult)
        P.tt(pr[:, 64:128], lam4[:, 128:192], lam4[:, 192:256], ALU.mult)
        P.op('vector', lambda e: e.reduce_sum(small[:, 0:1], pr[:, 0:64], AX.X), reads=[pr[:, 0:64]], writes=[small[:, 0:1]])
        P.op('vector', lambda e: e.reduce_sum(small[:, 1:2], pr[:, 64:128], AX.X), reads=[pr[:, 64:128]], writes=[small[:, 1:2]])
        P.act(small[:, 0:2], small[:, 0:2], AF.Exp)
        P.tt(small[:, 2:3], small[:, 1:2], small[:, 0:1], ALU.subtract)
        P.ts(neglam, small[:, 2:3], -0.2, None, ALU.add)
        P.ts(subg, subg, 0.8, None, ALU.mult)
        ar.release(m)

        m = ar.mark()
        xt = [ar.f32(D) for _ in range(4)]
        junk = ar.f32(D)
        hb = [ar.bf16(D) for _ in range(2)]
        hTc = [ar.bf16(8 * 512) for _ in range(2)]
        ss = ar.f32(NT)
        rs = ar.f32(NT)
        def p1_s1(t):
            xb = xt[t % 4]
            P.dma(xb, x[t * 128:(t + 1) * 128, :])
            P.act(junk, xb, AF.Square, accum_out=ss[:, t:t + 1])
            P.act(rs[:, t:t + 1], ss[:, t:t + 1], AF.Ln, scale=1.0 / D, bias=epsT[:, 0:1])
            P.act(rs[:, t:t + 1], rs[:, t:t + 1], AF.Exp, scale=-0.5)
            P.stt(hb[t % 2], xb, rs[:, t:t + 1], g1rep, ALU.mult, ALU.mult)

        def p1_s2(t):
            ch, tt = divmod(t, 4)
            pb = bank_bf(t % 2)
            for fc in range(8):
                P.transpose(pb[:, fc * 128:(fc + 1) * 128], hb[t % 2][:, fc * 128:(fc + 1) * 128], ident)
            dst = hTc[ch % 2].rearrange("p (f t) -> p f t", f=8)[:, :, tt * 128:(tt + 1) * 128]
            src = pb.rearrange("p (f t) -> p f t", f=8)
            if t % 2 == 0:
                P.copy(dst, src, eng='vector')
            else:
                P.op('scalar', lambda e, dst=dst, src=src: e.copy(dst, src), reads=[src], writes=[dst])
            if tt == 3:
                P.dma(hT_d[:, :, ch * 512:(ch + 1) * 512].rearrange("f p t -> p f t"),
                      hTc[ch % 2].rearrange("p (f t) -> p f t", f=8), eng='gpsimd')

        pipeline(NT, [p1_s1, p1_s2])
        ar.release(m)

        if phases >= 2:
            m = ar.mark()
            wl = ar.bf16(8 * 1024)
            wlv = wl.rearrange("p (f n) -> p f n", f=8)
            wbd = ar.bf16(16 * 128)
            wbdv = wbd.rearrange("p (a n) -> p a n", a=16)
            m_stg = ar.mark()
            stg = [ar.f32(1024) for _ in range(2)]
            for fc in range(8):
                P.dma(stg[fc % 2], w_in[fc * 128:(fc + 1) * 128, 1536:2560])
                (P.copy if fc % 2 == 0 else P.acopy)(wlv[:, fc, :], stg[fc % 2])
            for a in range(4):
                st = stg[a % 2][:, 0:512]
                P.dma(st.rearrange("p (a n) -> p a n", a=4), wbd_d[:, a * 4:(a + 1) * 4, :])
                (P.copy if a % 2 == 0 else P.acopy)(wbd[:, a * 512:(a + 1) * 512], st)
            ar.release(m_stg)
            hc = [ar.bf16(8 * 512) for _ in range(2)]
            XRb = [ar.f32(S + 4) for _ in range(2)]
            Gb = [ar.f32(S) for _ in range(2)]
            XCb = [ar.f32(S) for _ in range(2)]
            xcbb = [ar.bf16(S) for _ in range(2)]
            IB = ar.f32(S)
            AB = ar.f32(S)
            HB = ar.f32(S)
            QN = 2
            QS = S // QN
            hcn = [0]

            def project_steps(ct):
                XR = XRb[ct % 2]
                G = Gb[ct % 2]
                steps = []
                for ch in range(NCH):
                    def st(ch=ch):
                        if ch == 0:
                            P.memset(XR[:, 0:2], 0.0)
                            P.memset(XR[:, S + 2:S + 4], 0.0)
                        hcb = hc[hcn[0] % 2]
                        hcn[0] += 1
                        hcv = hcb.rearrange("p (f t) -> p f t", f=8)
                        P.dma(hcv, hT_d[:, :, ch * 512:(ch + 1) * 512].rearrange("f p t -> p f t"))
                        pa = bank(2 * (ch % 2))
                        pg = bank(2 * (ch % 2) + 1)
                        for fc in range(8):
                            P.mm(pa, wlv[:, fc, ct * 128:(ct + 1) * 128], hcv[:, fc, :], start=(fc == 0), stop=(fc == 7))
                        for fc in range(8):
                            P.mm(pg, wlv[:, fc, 512 + ct * 128:512 + (ct + 1) * 128], hcv[:, fc, :], start=(fc == 0), stop=(fc == 7))
                        o_ = XR[:, 2 + ch * 512:2 + (ch + 1) * 512]
                        P.op('scalar', lambda e, o=o_, i=pa: e.copy(o, i), reads=[pa], writes=[o_])
                        P.copy(G[:, ch * 512:(ch + 1) * 512], pg, eng='vector')
                    steps.append(st)
                return steps

            def conv_steps(ct):
                XR = XRb[ct % 2]
                XC = XCb[ct % 2]
                xcb = xcbb[ct % 2]
                cw = lambda j: convw[:, ct * 4 + j:ct * 4 + j + 1]
                steps = []
                for q in range(QN):
                    def st(q=q):
                        sl = slice(q * QS, (q + 1) * QS)
                        P.act(XC[:, sl], XR[:, q * QS:q * QS + QS], AF.Identity, scale=cw(0), bias=convb[:, ct:ct + 1])
                        for j in range(1, 4):
                            P.stt(XC[:, sl], XR[:, j + q * QS:j + q * QS + QS], cw(j), XC[:, sl], ALU.mult, ALU.add)
                        P.op('scalar', lambda e, o=xcb[:, sl], i_=XC[:, sl]: e.copy(o, i_), reads=[XC[:, sl]], writes=[xcb[:, sl]])
                    steps.append(st)
                return steps

            def y_steps(ct):
                R = XRb[ct % 2][:, 0:S]
                XC = XCb[ct % 2]
                xcb = xcbb[ct % 2]
                G = Gb[ct % 2]
                rnnb = xcbb[ct % 2]
                steps = []
                for dr in range(2):
                    ir = (0 * 2 + dr) * 4 + ct
                    ii = (1 * 2 + dr) * 4 + ct
                    ci = dr * 4 + ct
                    chs = list(range(NCH)) if dr == 0 else list(range(NCH - 1, -1, -1))
                    for ch in chs:
                        def sg(ch=ch, ir=ir, ii=ii):
                            pr_ = bank(4 + 2 * (ch % 2))
                            pi_ = bank(4 + 2 * (ch % 2) + 1)
                            cs = slice(ch * 512, (ch + 1) * 512)
                            P.mm(pr_, wbdv[:, ir, :], xcb[:, cs])
                            P.mm(pi_, wbdv[:, ii, :], xcb[:, cs])
                            P.act(R[:, cs], pr_, AF.Sigmoid, bias=lrub[:, ir:ir + 1])
                            P.act(IB[:, cs], pi_, AF.Sigmoid, bias=lrub[:, ii:ii + 1])
                        steps.append(sg)
                    qorder = list(range(QN)) if dr == 0 else list(range(QN - 1, -1, -1))
                    for q in qorder:
                        def s1(q=q, ci=ci):
                            sl = slice(q * QS, (q + 1) * QS)
                            P.tt(IB[:, sl], IB[:, sl], XC[:, sl], ALU.mult)
                            P.act(AB[:, sl], R[:, sl], AF.Exp, scale=cst[:, ci:ci + 1])
                        steps.append(s1)
                    for q in qorder:
                        def s2(q=q):
                            sl = slice(q * QS, (q + 1) * QS)
                            P.act(R[:, sl], AB[:, sl], AF.Square)
                        steps.append(s2)
                    for q in qorder:
                        def s3(q=q, dr=dr):
                            sl = slice(q * QS, (q + 1) * QS)
                            P.act(R[:, sl], R[:, sl], AF.Sqrt, scale=-1.0, bias=oneT[:, 0:1])
                            P.tt(IB[:, sl], IB[:, sl], R[:, sl], ALU.mult)
                            if dr == 0:
                                init = 0.0 if q == 0 else HB[:, q * QS - 1:q * QS]
                                rd = [AB[:, sl], IB[:, sl]] + ([] if q == 0 else [init])
                                P.op('vector', lambda e, sl=sl, init=init: e.tensor_tensor_scan(HB[:, sl], AB[:, sl], IB[:, sl], init, ALU.mult, ALU.add),
                                     reads=rd, writes=[HB[:, sl]])
                            else:
                                init = 0.0 if q == QN - 1 else R[:, (q + 1) * QS:(q + 1) * QS + 1]
                                rd = [AB[:, sl], IB[:, sl]] + ([] if q == QN - 1 else [init])
                                rs_ = slice((q + 1) * QS - 1, q * QS - 1 if q > 0 else None, -1)
                                P.op('vector', lambda e, rs_=rs_, init=init, R=R: e.tensor_tensor_scan(R[:, rs_], AB[:, rs_], IB[:, rs_], init, ALU.mult, ALU.add),
                                     reads=rd, writes=[R[:, sl]])
                                P.tt(HB[:, sl], HB[:, sl], R[:, sl], ALU.add)
                        steps.append(s3)
                for q in range(QN):
                    def g1(q=q):
                        sl = slice(q * QS, (q + 1) * QS)
                        P.act(R[:, sl], G[:, sl], AF.Square)
                        P.ts(R[:, sl], R[:, sl], 0.044715, 1.0, ALU.mult, ALU.add)
                        P.tt(R[:, sl], R[:, sl], G[:, sl], ALU.mult)
                    steps.append(g1)
                for q in range(QN):
                    def g2(q=q):
                        sl = slice(q * QS, (q + 1) * QS)
                        P.act(R[:, sl], R[:, sl], AF.Sigmoid, scale=1.5957691216057308)
                        P.tt(R[:, sl], R[:, sl], G[:, sl], ALU.mult)
                        P.tt(rnnb[:, sl], HB[:, sl], R[:, sl], ALU.mult)
                    steps.append(g2)

                def fin():
                    P.dma(rnn_d[ct], rnnb)
                    if debug:
                        P.copy(AB, rnnb)
                        P.dma(dbg['rnnT'][:, ct * S:(ct + 1) * S], AB)
                steps.append(fin)
                return steps

            for f_ in project_steps(0) + conv_steps(0):
                f_()
            for ct in range(4):
                ys = y_steps(ct)
                xs_ = (project_steps(ct + 1) + conv_steps(ct + 1)) if ct + 1 < 4 else []
                pos = {}
                npj = NCH if xs_ else 0
                for i_ in range(npj):
                    pos.setdefault(1 + 2 * i_, []).append(xs_[i_])
                for i_, f_ in enumerate(xs_[npj:]):
                    pos.setdefault(26 + 3 * i_, []).append(f_)
                for i_, f_ in enumerate(ys):
                    f_()
                    for g_ in pos.get(i_, []):
                        g_()
            ar.release(m)

        if phases >= 3:
            attn_start = ar.mark()
            attnT = ar.bf16(4 * S)
            attnTv = attnT.rearrange("p (h t) -> p h t", h=4)
            m_attn = ar.mark()
            ar3 = ar.sub(attn_start, m_attn - attn_start)
            QT = ar.bf16(4 * S)
            KT = ar.bf16(4 * S)
            V = ar.bf16(32 * 512)
            QTv = QT.rearrange("p (h t) -> p h t", h=4)
            KTv = KT.rearrange("p (h t) -> p h t", h=4)
            Vv = V.rearrange("p (k e) -> p k e", k=32)
            m3 = ar.mark()
            wa = ar.bf16(8 * 1536)
            wav = wa.rearrange("p (f n) -> p f n", f=8)
            stg = [ar.f32(1536) for _ in range(2)]
            for fc in range(8):
                P.dma(stg[fc % 2], w_in[fc * 128:(fc + 1) * 128, 0:1536])
                (P.copy if fc % 2 == 0 else P.acopy)(wav[:, fc, 0:1536], stg[fc % 2])
            permb = ar.bf16(128)
            P.dma(stg[0][:, 0:128], perm_d)
            P.copy(permb, stg[0][:, 0:128])
            hc = [ar3.bf16(8 * 512) for _ in range(2)]
            rc = [ar3.f32(512) for _ in range(2)]
            rsn = [ar3.f32(512) for _ in range(2)]
            t1 = [ar3.f32(512) for _ in range(2)]
            t2 = [ar3.f32(512) for _ in range(2)]
            qb = [ar.bf16(512) for _ in range(2)]
            k = 0
            for ch in range(NCH):
                hcb = hc[ch % 2]
                hcv = hcb.rearrange("p (f t) -> p f t", f=8)
                P.dma(hcv, hT_d[:, :, ch * 512:(ch + 1) * 512].rearrange("f p t -> p f t"))
                P.dma(rc[ch % 2], ropec_d[:, ch * 512:(ch + 1) * 512])
                P.dma(rsn[ch % 2], ropes_d[:, ch * 512:(ch + 1) * 512])
                sl = slice(ch * 512, (ch + 1) * 512)
                for qk in range(2):
                    dstv = QTv if qk == 0 else KTv
                    for h in range(4):
                        pq = bank(2 * (k % 2))
                        pqs = bank(2 * (k % 2) + 1)
                        c0 = qk * 512 + h * 128
                        for fc in range(8):
                            P.mm(pq, wav[:, fc, c0:c0 + 128], hcv[:, fc, :], start=(fc == 0), stop=(fc == 7))
                        P.acopy(qb[k % 2], pq)
                        P.mm(pqs, permb, qb[k % 2])
                        P.op('vector', lambda e, o=t1[k % 2], a=pq, b_=rc[ch % 2]: e.tensor_tensor(o, a, b_, ALU.mult),
                             reads=[pq, rc[ch % 2], qb[k % 2]], writes=[t1[k % 2]])
                        P.tt(t2[k % 2], pqs, rsn[ch % 2], ALU.mult)
                        P.tt(dstv[:, h, sl], t1[k % 2], t2[k % 2], ALU.add)
                        k += 1
                for tt in range(4):
                    pv = bank(4 + (tt % 2))
                    for fc in range(8):
                        P.mm(pv, hcv[:, fc, tt * 128:(tt + 1) * 128], wav[:, fc, 1024:1536], start=(fc == 0), stop=(fc == 7))
                    P.op('scalar', lambda e, o=Vv[:, ch * 4 + tt, :], i=pv: e.copy(o, i), reads=[pv], writes=[Vv[:, ch * 4 + tt, :]])
            ar.release(m3)
            if debug:
                m = ar.mark()
                tmpf = ar.f32(4 * S)
                P.copy(tmpf, QT)
                P.dma(dbg['qT'], tmpf)
                P.copy(tmpf, KT)
                P.dma(dbg['kT'], tmpf)
                P.copy(tmpf, V)
                P.dma(dbg['v'], tmpf)
                ar.release(m)

        if phases >= 4:
            m4 = ar.mark()
            E = [ar.bf16(1024) for _ in range(3)]
            rz1 = ar.f32(512)
            rz2 = ar.f32(512)
            ob = ar.f32(512)
            tb = ar.f32(512)
            sqb = ar.bf16(512)
            msb = ar.f32(512)
            SC = 0.125
            blk = 0
            cst_f = [ar.f32(2048) for _ in range(3)]
            cst_b = [ar.bf16(2048) for _ in range(3)]
            zacc2 = [ar.f32(512) for _ in range(2)]
            o1s = [ar.f32(512) for _ in range(2)]
            o2s = [ar.f32(512) for _ in range(2)]
            z1s = [ar.f32(512) for _ in range(2)]
            zh = ar.bf16(512)
            zl = ar.bf16(512)

            def convert_steps(e_):
                srcs = (w_gate[e_].rearrange("(f p) n -> p f n", p=128), w_up[e_].rearrange("(f p) n -> p f n", p=128),
                        w_down[e_].rearrange("(j p) n -> p j n", p=128))
                steps = []
                for q_ in range(3):
                    a_ = 8 if q_ < 2 else 2

                    def st(q_=q_, a_=a_):
                        P.dma(cst_f[q_].rearrange("p (a n) -> p a n", a=a_), srcs[q_])
                        P.copy(cst_b[q_], cst_f[q_])
                        P.dma(wall_d[e_ * 128:(e_ + 1) * 128, q_ * 2048:(q_ + 1) * 2048], cst_b[q_])
                    steps.append(st)
                return steps

            def epilogue_steps(h, qc, pb_):
                qs = slice(qc * 512, (qc + 1) * 512)
                B7 = bank(7)
                za = zacc2[pb_]

                def e1():
                    P.copy(zh, za)

                def e2():
                    P.tt(zl, za, zh, ALU.subtract)

                def e3():
                    P.mm(B7, ones, zh, start=True, stop=False)
                    P.mm(B7, ones, zl, start=False, stop=True)

                def e4():
                    P.act(rz2, B7, AF.Ln)
                    P.act(rz2, rz2, AF.Exp, scale=-1.0)

                def e5():
                    P.act(rz1, z1s[pb_], AF.Ln)
                    P.act(rz1, rz1, AF.Exp, scale=-1.0)

                def e6():
                    P.tt(ob, o1s[pb_], rz1, ALU.mult)
                    P.tt(tb, o2s[pb_], rz2, ALU.mult)

                def e7():
                    P.stt(ob, tb, neglam[:, 0:1], ob, ALU.mult, ALU.add)

                def e8():
                    P.tt(sqb, ob, ob, ALU.mult)

                def e9():
                    P.mm(B7, ones, sqb)

                def e10():
                    P.act(msb, B7, AF.Ln, scale=1.0 / 128, bias=epsT[:, 0:1])
                    P.act(msb, msb, AF.Exp, scale=-0.5)

                def e11():
                    P.stt(attnTv[:, h, qs], ob, subg[:, 0:1], msb, ALU.mult, ALU.mult)
                return [e1, e2, e3, e4, e5, e6, e7, e8, e9, e10, e11]

            pending = []
            for h in range(4):
                for qc in range(NCH):
                    qs = slice(qc * 512, (qc + 1) * 512)
                    O1, O2, Z1 = bank(4), bank(5), bank(6)
                    pb_ = blk % 2
                    sched = {}
                    for i_, f_ in enumerate(pending):
                        sched.setdefault(1 + i_, []).append(f_)
                    for i_, f_ in enumerate(convert_steps(blk)):
                        sched.setdefault(14 + 6 * i_, []).append(f_)
                    pending = []

                    def qk(kt):
                        sb = 2 * (kt % 2)
                        ks = slice(kt * 128, (kt + 1) * 128)
                        P.mm(bank(sb), KTv[0:64, h, ks], QTv[0:64, h, qs], tile_position=(0, 0))
                        P.mm(bank(sb + 1), KTv[64:128, h, ks], QTv[64:128, h, qs], tile_position=(64, 0))

                    qk(0)
                    for kt in range(32):
                        if kt + 1 < 32:
                            qk(kt + 1)
                        sb = 2 * (kt % 2)
                        Eb = E[kt % 3]
                        P.act(Eb.rearrange("p (a n) -> p a n", a=2), PS[:, sb:sb + 2, :], AF.Exp, scale=SC)
                        st = (kt == 0)
                        sp = (kt == 31)
                        vs = Vv[:, kt, h * 128:(h + 1) * 128]
                        P.mm(O1, vs, Eb[:, 0:512], start=st, stop=sp)
                        P.mm(O2, vs, Eb[:, 512:1024], start=st, stop=sp)
                        P.mm(Z1, ones, Eb[:, 0:512], start=st, stop=sp)
                        if kt == 0:
                            P.copy(zacc2[pb_], Eb[:, 512:1024])
                        else:
                            P.tt(zacc2[pb_], zacc2[pb_], Eb[:, 512:1024], ALU.add)
                        for f_ in sched.get(kt, []):
                            f_()
                    P.copy(o1s[pb_], O1)
                    P.copy(o2s[pb_], O2)
                    P.copy(z1s[pb_], Z1)
                    pending = epilogue_steps(h, qc, pb_)
                    blk += 1
            for f_ in pending:
                f_()
            ar.release(m4)
            if debug:
                m = ar.mark()
                tmpf = ar.f32(4 * S)
                P.copy(tmpf, attnT)
                P.dma(dbg['attnT'], tmpf)
                ar.release(m)

        if phases >= 5:
            ar.release(m_attn)
            m5 = ar.mark()
            rnnT = ar.bf16(4 * S)
            rnnTv = rnnT.rearrange("p (c t) -> p c t", c=4)
            for ct in range(4):
                P.dma(rnnTv[:, ct, :], rnn_d[ct])
            wo = ar.bf16(8 * 1024)
            wov = wo.rearrange("p (f n) -> p f n", f=8)
            m_stg5 = ar.mark()
            stg = [ar.f32(1024) for _ in range(2)]
            for fc in range(8):
                P.dma(stg[fc % 2], w_out[fc * 128:(fc + 1) * 128, :])
                (P.copy if fc % 2 == 0 else P.acopy)(wov[:, fc, :], stg[fc % 2])
            ar.release(m_stg5)
            xt = [ar.f32(D) for _ in range(4)]
            x1t = [ar.f32(D) for _ in range(3)]

            def p5_stage(t):
                xb = xt[t % 4]
                P.dma(xb, x[t * 128:(t + 1) * 128, :])
                ts_ = slice(t * 128, (t + 1) * 128)
                for nch in range(2):
                    po = bank(6 + nch)
                    for fc in range(8):
                        lhs = attnTv[:, fc, ts_] if fc < 4 else rnnTv[:, fc - 4, ts_]
                        P.mm(po, lhs, wov[:, fc, nch * 512:(nch + 1) * 512], start=(fc == 0), stop=(fc == 7))
                    P.tt(x1t[t % 3][:, nch * 512:(nch + 1) * 512], po, xb[:, nch * 512:(nch + 1) * 512], ALU.add)
                P.dma(x1_d[t * 128:(t + 1) * 128, :], x1t[t % 3], eng='gpsimd')
                if debug:
                    P.dma(dbg['x1'][t * 128:(t + 1) * 128, :], x1t[t % 3])

            if phases < 6:
                for t in range(NT):
                    p5_stage(t)

        if phases >= 6:
            TS = 256
            NTL = 64
            NSLOT = NTL * TS
            BIG = 1.0e30
            fgrep = ar.f32(D)
            P.dma(fgrep, fgrep_d)
            slotA = ar.f32(32).bitcast(I32)
            slotB = ar.f32(32).bitcast(I32)
            wA = ar.f32(32)
            wB = ar.f32(32)
            EI = ar.f32(NTL).bitcast(I32)
            sm = ar.f32(64)
            mA = ar.mark()
            g2rep = ar.f32(D)
            P.dma(g2rep, g2rep_d)
            whi = ar.bf16(8 * 36)
            wlo = ar.bf16(8 * 36)
            whiv = whi.rearrange("p (f n) -> p f n", f=8)
            wlov = wlo.rearrange("p (f n) -> p f n", f=8)
            wrt = ar.f32(8 * 36)
            wrt2 = ar.f32(8 * 36)
            P.dma(wrt.rearrange("p (f n) -> p f n", f=8), w_rt.rearrange("(f p) n -> p f n", p=128))
            P.copy(whi, wrt)
            P.copy(wrt2, whi)
            P.tt(wlo, wrt, wrt2, ALU.subtract)
            umat = ar.bf16(128)
            tmpU = ar.f32(128)
            P.dma(tmpU, umat_d)
            P.copy(umat, tmpU)
            thr = ar.f32(NTL)
            P.dma(thr, thr_d)
            pidx = ar.f32(1)
            P.dma(pidx, pidx_d)
            hib = [ar.bf16(D) for _ in range(2)]
            LG = ar.f32(NT * 36)
            LGv = LG.rearrange("p (t n) -> p t n", t=NT)
            h2 = [ar.f32(D) for _ in range(2)]
            lo = [ar.bf16(D) for _ in range(2)]
            hiT = [ar.bf16(8 * 128) for _ in range(2)]
            loT = [ar.bf16(8 * 128) for _ in range(2)]
            junk = ar.f32(D)
            ssq = ar.f32(NT)
            rsq = ar.f32(NT)
            def a_s1(t):
                b = t % 2
                xin = x1t[t % 3]
                P.act(junk, xin, AF.Square, accum_out=ssq[:, t:t + 1])
                P.act(rsq[:, t:t + 1], ssq[:, t:t + 1], AF.Ln, scale=1.0 / D, bias=epsT[:, 0:1])
                P.act(rsq[:, t:t + 1], rsq[:, t:t + 1], AF.Exp, scale=-0.5)
                P.stt(h2[b], xin, rsq[:, t:t + 1], g2rep, ALU.mult, ALU.mult)
                hi = hib[b]
                P.op('scalar', lambda e, o=hi, i_=h2[b]: e.copy(o, i_), reads=[h2[b]], writes=[hi])
                P.tt(lo[b], h2[b], hi, ALU.subtract)
                P.dma(h2_d[t * 128:(t + 1) * 128, :], hi, eng='gpsimd')

            def a_s2(t):
                b = t % 2
                hi = hib[b]
                pb = bank_bf(2 * b)
                pl = bank_bf(2 * b + 1)
                for fc in range(8):
                    P.transpose(pb[:, fc * 128:(fc + 1) * 128], hi[:, fc * 128:(fc + 1) * 128], ident)
                for fc in range(8):
                    P.transpose(pl[:, fc * 128:(fc + 1) * 128], lo[b][:, fc * 128:(fc + 1) * 128], ident)
                P.copy(hiT[b], pb)
                P.op('scalar', lambda e, o=loT[b], i=pl: e.copy(o, i), reads=[pl], writes=[loT[b]])

            def a_s3(t):
                b = t % 2
                hv = hiT[b].rearrange("p (f t) -> p f t", f=8)
                lv = loT[b].rearrange("p (f t) -> p f t", f=8)
                plg = bank(4 + b)[:, 0:36]
                n = 0
                for fc in range(8):
                    for (lh, rh) in ((hv[:, fc, :], whiv[:, fc, :]), (hv[:, fc, :], wlov[:, fc, :]), (lv[:, fc, :], whiv[:, fc, :])):
                        P.mm(plg, lh, rh, start=(n == 0), stop=(n == 23))
                        n += 1
                P.copy(LGv[:, t, :], plg)

            pipeline(NT, [p5_stage, a_s1, a_s2, a_s3])
            if debug:
                P.dma(dbg['lg'], LG)
            GL = LGv[:, :, 0:4]
            EL = LGv[:, :, 4:36].rearrange("p t (g e) -> p t g e", g=4)
            T1 = ar.f32(NT * 32)
            T2 = ar.f32(NT * 32)
            MA = ar.f32(NT * 32)
            MS = ar.f32(NT * 32)
            ELM = ar.f32(NT * 32)
            v3 = lambda a: a.rearrange("p (t e) -> p t e", t=NT)
            v4 = lambda a: a.rearrange("p (t g e) -> p t g e", t=NT, g=4)
            g4 = ar.f32(NT * 4)
            g4b = ar.f32(NT * 4)
            g4v = g4.rearrange("p (t g) -> p t g", g=4)
            g4bv = g4b.rearrange("p (t g) -> p t g", g=4)
            gmax = ar.f32(NT)
            gtp = ar.f32(NT)
            v1 = ar.f32(NT)
            v2 = ar.f32(NT)
            d21 = ar.f32(NT)
            bc3 = lambda a, n: a.unsqueeze(2).to_broadcast([128, NT, n])
            red = lambda o, i, op: P.op('vector', lambda e: e.tensor_reduce(o, i, AX.X, op), reads=[i], writes=[o])
            red(gmax, GL, ALU.max)
            P.tt(g4v, GL, bc3(gmax, 4), ALU.subtract)
            P.act(g4b, g4, AF.Exp)
            red(gtp, g4bv, ALU.add)
            P.recip(gtp, gtp)
            P.tt(g4v, GL, bc3(gmax, 4), ALU.is_ge)
            P.ts(g4, g4, BIG, -BIG, ALU.mult, ALU.add)
            P.tt(v4(ELM), EL, g4v.unsqueeze(3).to_broadcast([128, NT, 4, 8]), ALU.add)
            red(v1, v3(ELM), ALU.max)
            P.tt(v3(MA), v3(ELM), bc3(v1, 32), ALU.is_ge)
            P.stt(T1, MA, -BIG, ELM, ALU.mult, ALU.add)
            red(v2, v3(T1), ALU.max)
            P.tt(v3(MS), v3(ELM), bc3(v2, 32), ALU.is_ge)
            P.tt(T2, MS, MA, ALU.subtract)
            P.tt(d21, v2, v1, ALU.subtract)
            P.act(d21, d21, AF.Exp)
            P.ts(T1[:, 0:NT], d21, 1.0, None, ALU.add)
            P.recip(T1[:, 0:NT], T1[:, 0:NT])
            P.tt(wA, T1[:, 0:NT], gtp, ALU.mult)
            P.tt(wB, wA, d21, ALU.mult)
            MSb = ar.bf16(NT * 32)
            P.copy(MSb, MS)
            MSbv = v3(MSb)
            ppos = PS[:, 0:2, :].rearrange("p a n -> p (a n)")
            pcs = PS[:, 2:4, :].rearrange("p a n -> p (a n)")
            for t in range(NT):
                P.mm(ppos[:, t * 32:(t + 1) * 32], umat, MSbv[:, t, :])
                P.mm(pcs[:, t * 32:(t + 1) * 32], ones, MSbv[:, t, :])
            CS = ar.f32(NT * 32)
            P.copy(CS, pcs)
            BASE = ar.f32((NT + 1) * 32)
            P.memset(BASE[:, 0:32], 0.0)
            for t in range(NT):
                P.tt(BASE[:, (t + 1) * 32:(t + 2) * 32], BASE[:, t * 32:(t + 1) * 32], CS[:, t * 32:(t + 1) * 32], ALU.add)
            ntot = BASE[:, NT * 32:(NT + 1) * 32]
            npad = ar.f32(32)
            P.ts(npad, ntot, 0.0, None, ALU.is_gt)
            for kk in range(1, S // TS):
                P.stt(npad, ntot, float(kk * TS), npad, ALU.is_gt, ALU.add)
            P.ts(npad, npad, float(TS), None, ALU.mult)
            onesf = ar.f32(32)
            P.memset(onesf, 1.0)
            endp = ar.f32(32)
            P.op('vector', lambda e: e.tensor_tensor_scan(endp, onesf, npad, 0.0, ALU.mult, ALU.add), reads=[onesf, npad], writes=[endp])
            startp = ar.f32(32)
            P.tt(startp, endp, npad, ALU.subtract)
            P.tt(T1, ppos, BASE[:, 0:NT * 32], ALU.add)
            P.tt(v3(T1), v3(T1), startp.unsqueeze(1).to_broadcast([128, NT, 32]), ALU.add)
            P.tt(ELM, T1, MA, ALU.mult)
            sf = ar.f32(NT)
            red(sf, v3(ELM), ALU.add)
            P.copy(slotA, sf)
            P.tt(ELM, T1, T2, ALU.mult)
            red(sf, v3(ELM), ALU.add)
            P.copy(slotB, sf)
            TE = ar.f32(NTL * 32)
            TEv = TE.rearrange("p (i e) -> p i e", i=NTL)
            P.tt(TEv, endp.unsqueeze(1).to_broadcast([128, NTL, 32]), thr.unsqueeze(2).to_broadcast([128, NTL, 32]), ALU.is_le)
            eif = ar.f32(NTL)
            red(eif, TEv, ALU.add)
            P.ts(eif, eif, 31.0, None, ALU.min)
            P.ts(eif, eif, 128.0, pidx[:, 0:1], ALU.mult, ALU.add)
            inval = ar.f32(NTL)
            P.ts(inval, thr, endp[:, 31:32], 1.0e6, ALU.is_ge, ALU.mult)
            P.tt(eif, eif, inval, ALU.add)
            P.copy(EI, eif)
            if debug:
                dtmp = ar.f32(192)
                P.copy(dtmp[:, 0:32], slotA)
                P.copy(dtmp[:, 32:64], slotB)
                P.copy(dtmp[:, 64:96], wA)
                P.copy(dtmp[:, 96:128], wB)
                P.copy(dtmp[:, 128:192], EI)
                P.dma(dbg['route'], dtmp)
            hst = [ar.bf16(D) for _ in range(3)]
            for t in range(NT if stop6 >= 2 else 0):
                hb_ = hst[t % 3]
                P.dma(hb_, h2_d[t * 128:(t + 1) * 128, :])
                for sl_ in (slotA, slotB):
                    P.op('gpsimd', lambda e, sl_=sl_, t=t, hb_=hb_: e.indirect_dma_start(
                        out=xs_d[:, :], out_offset=bass.IndirectOffsetOnAxis(ap=sl_[:, t:t + 1], axis=0),
                        in_=hb_, in_offset=None),
                        reads=[hb_, sl_[:, t:t + 1]], writes=[xs_d], is_dma=True, partial=True)
            ar.release(mA)
            mB = ar.mark()
            NWB = 3
            wB_ = [ar.bf16(6144) for _ in range(NWB)]
            xg = [ar.bf16(2 * D) for _ in range(3)]
            xgT = [ar.bf16(8 * TS) for _ in range(2)]
            sg = [ar.f32(TS) for _ in range(2)]
            hid = [ar.bf16(2 * TS) for _ in range(2)]
            ysb = [ar.bf16(2 * D) for _ in range(2)]

            def b_load(i):
                kw_ = dict(bounds_check=NE * 128 - 1, oob_is_err=False) if i >= 32 else {}
                wb = wB_[i % NWB]
                P.op('gpsimd', lambda e, wb=wb, i=i, kw_=kw_: e.indirect_dma_start(
                    out=wb, out_offset=None, in_=wall_d[:, :],
                    in_offset=bass.IndirectOffsetOnAxis(ap=EI[:, i:i + 1], axis=0), **kw_),
                    reads=[wall_d, EI[:, i:i + 1]], writes=[wb], is_dma=True)
                P.dma(xg[i % 3].rearrange("p (a n) -> p a n", a=2), xs_d[i * TS:(i + 1) * TS, :].rearrange("(a p) n -> p a n", p=128))

            def b_s1(i):
                xgv = xg[i % 3].rearrange("p (a n) -> p a n", a=2)
                xTv = xgT[i % 2].rearrange("p (f t) -> p f t", f=8)
                for a in range(2):
                    pt = bank_bf(a)
                    for fc in range(8):
                        P.transpose(pt[:, fc * 128:(fc + 1) * 128], xgv[:, a, fc * 128:(fc + 1) * 128], ident)
                    src = pt.rearrange("p (f t) -> p f t", f=8)
                    dst = xTv[:, :, a * 128:(a + 1) * 128]
                    if a == 0:
                        P.copy(dst, src)
                    else:
                        P.op('scalar', lambda e, o=dst, i_=src: e.copy(o, i_), reads=[src], writes=[dst])

            def b_s2(i):
                wb = wB_[i % NWB]
                xTv = xgT[i % 2].rearrange("p (f t) -> p f t", f=8)
                wgv = wb[:, 0:2048].rearrange("p (f n) -> p f n", f=8)
                wuv = wb[:, 2048:4096].rearrange("p (f n) -> p f n", f=8)
                hv = hid[i % 2].rearrange("p (j t) -> p j t", j=2)
                for jj in range(2):
                    pg = bank(2 + jj)[:, 0:TS]
                    pu = bank(4 + jj)[:, 0:TS]
                    for fc in range(8):
                        P.mm(pg, wgv[:, fc, jj * 128:(jj + 1) * 128], xTv[:, fc, :], start=(fc == 0), stop=(fc == 7))
                    for fc in range(8):
                        P.mm(pu, wuv[:, fc, jj * 128:(jj + 1) * 128], xTv[:, fc, :], start=(fc == 0), stop=(fc == 7))
                    P.act(sg[jj], pg, AF.Silu)
                    P.tt(hv[:, jj, :], sg[jj], pu, ALU.mult)

            def b_s3(i):
                wb = wB_[i % NWB]
                wdv = wb[:, 4096:6144].rearrange("p (j n) -> p j n", j=2)
                hv = hid[i % 2].rearrange("p (j t) -> p j t", j=2)
                yv = ysb[i % 2].rearrange("p (a n) -> p a n", a=2)
                k = 0
                for a in range(2):
                    for nch in range(2):
                        pd = bank(6 + (k % 2))
                        for jj in range(2):
                            P.mm(pd, hv[:, jj, a * 128:(a + 1) * 128], wdv[:, jj, nch * 512:(nch + 1) * 512], start=(jj == 0), stop=(jj == 1))
                        o_ = yv[:, a, nch * 512:(nch + 1) * 512]
                        if k % 2 == 0:
                            P.copy(o_, pd)
                        else:
                            P.op('scalar', lambda e, o=o_, i_=pd: e.copy(o, i_), reads=[pd], writes=[o_])
                        k += 1
                P.dma(ys_d[i * TS:(i + 1) * TS, :].rearrange("(a p) n -> p a n", p=128), yv)

            if stop6 >= 3:
                b_load(0)
                for it in range(NTL + 1):
                    if it + 1 < NTL:
                        b_load(it + 1)
                    if it >= 1:
                        b_s2(it - 1)
                    if it < NTL:
                        b_s1(it)
                    if it >= 1:
                        b_s3(it - 1)
            ar.release(mB)
            YA = [ar.bf16(D) for _ in range(3)]
            YB = [ar.bf16(D) for _ in range(3)]
            x1c = [ar.f32(D) for _ in range(3)]
            outb = [ar.f32(D) for _ in range(2)]
            junk2 = ar.f32(D)
            ss3 = ar.f32(NT)
            rs3 = ar.f32(NT)
            if stop6 < 4:
                zz = ar.f32(D)
                P.memset(zz, 0.0)
                P.dma(out[0:128, :], zz)

            def c_s1(t):
                b = t % 3
                P.dma(x1c[b], x1_d[t * 128:(t + 1) * 128, :])
                for (yy, sl_) in ((YA[b], slotA), (YB[b], slotB)):
                    P.op('gpsimd', lambda e, yy=yy, sl_=sl_, t=t: e.indirect_dma_start(
                        out=yy, out_offset=None, in_=ys_d[:, :],
                        in_offset=bass.IndirectOffsetOnAxis(ap=sl_[:, t:t + 1], axis=0)),
                        reads=[ys_d, sl_[:, t:t + 1]], writes=[yy], is_dma=True)

            def c_s2(t):
                b = t % 3
                P.stt(x1c[b], YA[b], wA[:, t:t + 1], x1c[b], ALU.mult, ALU.add)
                P.stt(x1c[b], YB[b], wB[:, t:t + 1], x1c[b], ALU.mult, ALU.add)
                P.act(junk2, x1c[b], AF.Square, accum_out=ss3[:, t:t + 1])
                P.act(rs3[:, t:t + 1], ss3[:, t:t + 1], AF.Ln, scale=1.0 / D, bias=epsT[:, 0:1])
                P.act(rs3[:, t:t + 1], rs3[:, t:t + 1], AF.Exp, scale=-0.5)

            def c_s3(t):
                b = t % 3
                P.stt(outb[t % 2], x1c[b], rs3[:, t:t + 1], fgrep, ALU.mult, ALU.mult)
                P.dma(out[t * 128:(t + 1) * 128, :], outb[t % 2])

            if stop6 >= 4:
                pipeline(NT, [c_s1, c_s2, c_s3])
        else:
            m = ar.mark()
            z = ar.f32(D)
            P.memset(z, 0.0)
            P.dma(out[0:128, :], z)
            ar.release(m)

        P.emit()
    return nc


def _rope_tables():
    pos = np.arange(S, dtype=np.float32)
    inv = (np.float32(500000.0) ** (-np.arange(0, 16, 2, dtype=np.float32) / np.float32(16))).astype(np.float32)
    ang = pos[:, None] * inv[None, :]
    cos = np.cos(ang).astype(np.float32)
    sin = np.sin(ang).astype(np.float32)
    C = np.ones((128, S), np.float32)
    Sn = np.zeros((128, S), np.float32)
    for mth in range(2):
        for d in range(16):
            p = mth * 64 + d
            C[p] = cos[:, d % 8]
            Sn[p] = -sin[:, d % 8] if d < 8 else sin[:, d % 8]
    return C, Sn


def _shared_inputs(norm1_g, w_in, lambda_q1, lambda_k1, lambda_q2, lambda_k2, subln_g, conv_w, conv_b,
                   lru_w_r, lru_b_r, lru_w_i, lru_b_i, lru_lambda, w_out, norm2_g, w_grp, w_exp,
                   w_gate, w_up, w_down, final_g):
    f = np.float32
    c = np.ascontiguousarray
    w_in0 = c(w_in[0], dtype=f)
    perm = np.arange(128)
    for sh in range(2):
        b0 = sh * 64
        perm[b0:b0 + 8] = np.arange(b0 + 8, b0 + 16)
        perm[b0 + 8:b0 + 16] = np.arange(b0, b0 + 8)
    permm = np.zeros((128, 128), f)
    permm[perm, np.arange(128)] = 1.0
    C, Sn = _rope_tables()
    lam4 = np.stack([lambda_q1[0], lambda_k1[0], lambda_q2[0], lambda_k2[0]], 0).astype(f)
    lam4 = c(np.broadcast_to(lam4[None], (128, 4, 64)))
    convw = c(conv_w[0].reshape(4, 4, 128).transpose(2, 1, 0), dtype=f)
    convb = c(conv_b[0].reshape(4, 128).T, dtype=f)
    wbd = np.zeros((128, 16, 128), f)
    lrub = np.zeros((128, 16), f)
    for gi, (wm, bm) in enumerate(((lru_w_r[0], lru_b_r[0]), (lru_w_i[0], lru_b_i[0]))):
        for dr in range(2):
            for ct in range(4):
                a = (gi * 2 + dr) * 4 + ct
                for bl in range(2):
                    wbd[bl * 64:(bl + 1) * 64, a, bl * 64:(bl + 1) * 64] = wm[dr, ct * 2 + bl]
                lrub[:, a] = bm[dr, ct * 128:(ct + 1) * 128]
    lrul = np.zeros((128, 8), f)
    for dr in range(2):
        for ct in range(4):
            lrul[:, dr * 4 + ct] = lru_lambda[0, dr, ct * 128:(ct + 1) * 128]
    rep = lambda v: c(np.broadcast_to(np.asarray(v, f).reshape(1, -1), (128, v.size)))
    return {
        "w_in": w_in0,
        "perm": permm,
        "g1rep": rep(norm1_g[0]),
        "g2rep": rep(norm2_g[0]),
        "fgrep": rep(final_g),
        "ropec": C,
        "ropes": Sn,
        "lam4": lam4,
        "subln": c(subln_g[0].reshape(128, 1), dtype=f),
        "convw": convw,
        "convb": convb,
        "wbd": wbd,
        "lrub": lrub,
        "lrul": lrul,
        "w_out": c(w_out[0], dtype=f),
        "w_rt": c(np.concatenate([w_grp[0], w_exp[0]], axis=1), dtype=f),
        "w_gate": c(w_gate[0], dtype=f),
        "w_up": c(w_up[0], dtype=f),
        "w_down": c(w_down[0], dtype=f),
        "ident": np.eye(128, dtype=f),
        "umat": np.triu(np.ones((128, 128), f), 1),
        "thr": c(np.broadcast_to((np.arange(64, dtype=f) * 256.0)[None, :], (128, 64))),
        "pidx": np.arange(128, dtype=f).reshape(128, 1),
    }


def kernel(x, **params):
    x = np.asarray(x, dtype=np.float32)
    params = {k: np.asarray(v, dtype=np.float32) for k, v in params.items()}
    shared = _shared_inputs(**params)
    nc = build_nc()
    in_maps = []
    for b in range(8):
        mp = dict(shared)
        mp["x"] = np.ascontiguousarray(x[b])
        in_maps.append(mp)
    res = run_bass_kernel_spmd(nc, in_maps, core_ids=list(range(8)))
    return np.stack([np.asarray(r["out"], dtype=np.float32) for r in res.results], axis=0)
```

```python
import math
from contextlib import ExitStack

import numpy as np
import concourse.bass as bass
import concourse.mybir as mybir
from concourse.bass_utils import run_bass_kernel_spmd

F32 = mybir.dt.float32
BF16 = mybir.dt.bfloat16
AF = mybir.ActivationFunctionType
ALU = mybir.AluOpType
AX = mybir.AxisListType

I32 = mybir.dt.int32
_DTSZ = {F32: 4, BF16: 2, I32: 4}

S = 4096
D = 1024
NT = 32
NCH = 8
NE = 32
EPS = 1e-6


def _rect(ap):
    t = ap.tensor
    sz = _DTSZ.get(ap.dtype, 4)
    dims = ap.ap
    lo = 0
    hi = 0
    space = str(ap.space).upper()
    if not ('SB' in space or 'PSUM' in space):
        for st, cnt in dims:
            if st >= 0:
                hi += st * (cnt - 1)
            else:
                lo += st * (cnt - 1)
        off = ap.offset
        return (t.name, 0, 1, (off + lo) * sz, (off + hi + 1) * sz)
    pstride = 1
    for s in list(t.shape)[1:]:
        pstride *= s
    off = ap.offset
    p0 = off // pstride
    f0 = off % pstride
    pcnt = dims[0][1]
    for st, cnt in dims[1:]:
        if st >= 0:
            hi += st * (cnt - 1)
        else:
            lo += st * (cnt - 1)
    return (t.name, p0, p0 + pcnt, (f0 + lo) * sz, (f0 + hi + 1) * sz)


def _overlap(a, b):
    return a[1] < b[2] and b[1] < a[2] and a[3] < b[4] and b[3] < a[4]


def _covers(a, b):
    return a[1] <= b[1] and a[2] >= b[2] and a[3] <= b[3] and a[4] >= b[4]


class Op:
    __slots__ = ('idx', 'eng', 'fn', 'deps', 'is_dma', 'signal', 'count', 'dsem', 'dcount', 'dprev')

    def __init__(self, idx, eng, fn, is_dma):
        self.idx = idx
        self.eng = eng
        self.fn = fn
        self.is_dma = is_dma
        self.deps = set()
        self.signal = False
        self.count = 0
        self.dsem = None
        self.dcount = 0
        self.dprev = 0


class Prog:
    ENGINES = ['sync', 'scalar', 'vector', 'gpsimd', 'tensor']

    def __init__(self, nc, n_dma_sems=32, same_engine_sync=True):
        self.nc = nc
        self.ops = []
        self.recs = {}
        self.n_dma_sems = n_dma_sems
        self.same_engine_sync = same_engine_sync

    def op(self, eng, fn, reads=(), writes=(), is_dma=False, partial=False):
        o = Op(len(self.ops), eng, fn, is_dma)
        self.ops.append(o)
        rrs = [_rect(ap) for ap in reads]
        wrs = [_rect(ap) for ap in writes]
        for r in rrs:
            for rec in self.recs.setdefault(r[0], []):
                if rec[2] and _overlap(rec[0], r):
                    o.deps.add(rec[1])
        for r in wrs:
            for rec in self.recs.setdefault(r[0], []):
                if _overlap(rec[0], r):
                    if partial and rec[2] and len(rec) > 4 and rec[4]:
                        continue
                    o.deps.add(rec[1])
        o.deps.discard(o.idx)
        for r in rrs:
            lst = self.recs[r[0]]
            done = False
            if not is_dma:
                for rec in lst:
                    if (not rec[2]) and rec[3] == eng and rec[0] == r and not self.ops[rec[1]].is_dma:
                        rec[1] = o.idx
                        done = True
                        break
            if not done:
                lst.append([r, o.idx, False, eng])
        for r in wrs:
            lst = self.recs[r[0]]
            if not partial:
                lst[:] = [rec for rec in lst if not (_covers(r, rec[0]) and rec[1] != o.idx)]
            lst.append([r, o.idx, True, eng, partial])
        return o

    def dma(self, out, in_, eng='sync'):
        return self.op(eng, lambda e: e.dma_start(out=out, in_=in_), reads=[in_], writes=[out], is_dma=True)

    def mm(self, out, lhsT, rhs, start=True, stop=True, **kw):
        rd = [lhsT, rhs] + ([] if start else [out])
        return self.op('tensor', lambda e: e.matmul(out, lhsT, rhs, start=start, stop=stop, **kw),
                       reads=rd, writes=[out])

    def transpose(self, out, in_, ident):
        return self.op('tensor', lambda e: e.transpose(out, in_, ident), reads=[in_, ident], writes=[out])

    def act(self, out, in_, func, bias=None, scale=None, accum_out=None):
        kw = {}
        rd = [in_]
        wr = [out]
        if bias is not None:
            kw['bias'] = bias
            if not isinstance(bias, (int, float)):
                rd.append(bias)
        if scale is not None:
            kw['scale'] = scale
            if not isinstance(scale, (int, float)):
                rd.append(scale)
        if accum_out is not None:
            kw['accum_out'] = accum_out
            wr.append(accum_out)
        return self.op('scalar', lambda e: e.activation(out, in_, func, **kw), reads=rd, writes=wr)

    def tt(self, out, in0, in1, op, eng='vector'):
        return self.op(eng, lambda e: e.tensor_tensor(out, in0, in1, op), reads=[in0, in1], writes=[out])

    def ts(self, out, in0, s1, s2, op0, op1=None, eng='vector'):
        rd = [in0]
        if not isinstance(s1, (int, float)):
            rd.append(s1)
        if s2 is not None and not isinstance(s2, (int, float)):
            rd.append(s2)
        kw = {}
        if op1 is not None:
            kw['op1'] = op1
        return self.op(eng, lambda e: e.tensor_scalar(out, in0, s1, s2, op0, **kw), reads=rd, writes=[out])

    def stt(self, out, in0, scalar, in1, op0, op1, eng='vector'):
        rd = [in0, in1]
        if not isinstance(scalar, (int, float)):
            rd.append(scalar)
        return self.op(eng, lambda e: e.scalar_tensor_tensor(out, in0, scalar, in1, op0, op1),
                       reads=rd, writes=[out])

    def copy(self, out, in_, eng='vector'):
        return self.op(eng, lambda e: e.tensor_copy(out, in_), reads=[in_], writes=[out])

    def memset(self, out, val, eng='vector'):
        return self.op(eng, lambda e: e.memset(out, val), reads=[], writes=[out])

    def acopy(self, out, in_):
        return self.op('scalar', lambda e: e.copy(out, in_), reads=[in_], writes=[out])

    def recip(self, out, in_):
        return self.op('vector', lambda e: e.reciprocal(out, in_), reads=[in_], writes=[out])

    def emit(self):
        nc = self.nc
        ops = self.ops
        ses = self.same_engine_sync
        for o in ops:
            for d in o.deps:
                p = ops[d]
                if p.is_dma:
                    continue
                if p.eng != o.eng or o.is_dma or (ses and p.eng != 'tensor'):
                    p.signal = True
        cnt = {e: 0 for e in self.ENGINES}
        for o in ops:
            if o.is_dma:
                continue
            if o.signal:
                cnt[o.eng] += 1
            o.count = cnt[o.eng]
        dcum = [0] * self.n_dma_sems
        k = 0
        for o in ops:
            if o.is_dma:
                s = k % self.n_dma_sems
                k += 1
                o.dsem = s
                o.dprev = dcum[s]
                dcum[s] += 16
                o.dcount = dcum[s]
        with ExitStack() as es:
            esem = {e: es.enter_context(nc.semaphore('s_' + e)) for e in self.ENGINES}
            dsems = [es.enter_context(nc.semaphore('d_%d' % i)) for i in range(self.n_dma_sems)]
            block = es.enter_context(nc.Block())
            per_eng = {e: [o for o in ops if o.eng == e] for e in self.ENGINES}
            final_counts = list(dcum)

            def make(ename):
                def body(eng):
                    waited = {}
                    for o in per_eng[ename]:
                        need = {}
                        for d in o.deps:
                            p = ops[d]
                            if p.is_dma:
                                key = ('d', p.dsem)
                                v = p.dcount
                            else:
                                if p.eng == ename and not o.is_dma:
                                    if ename == 'tensor' or not ses:
                                        continue
                                if not p.signal:
                                    continue
                                key = ('e', p.eng)
                                v = p.count
                            if need.get(key, 0) < v:
                                need[key] = v
                        if o.is_dma and o.dprev > 0:
                            key = ('d', o.dsem)
                            if need.get(key, 0) < o.dprev:
                                need[key] = o.dprev
                        for key, v in need.items():
                            if waited.get(key, 0) >= v:
                                continue
                            waited[key] = v
                            sem = dsems[key[1]] if key[0] == 'd' else esem[key[1]]
                            eng.wait_ge(sem, v)
                        ins = o.fn(eng)
                        if o.is_dma:
                            ins.then_inc(dsems[o.dsem], 16)
                        elif o.signal:
                            ins.then_inc(esem[ename], 1)
                    if ename == 'sync':
                        for i, c in enumerate(final_counts):
                            if c > 0 and waited.get(('d', i), 0) < c:
                                eng.wait_ge(dsems[i], c)
                return body

            block.sync(make('sync'))
            block.scalar(make('scalar'))
            block.vector(make('vector'))
            block.gpsimd(make('gpsimd'))
            block.tensor(make('tensor'))


def pipeline(n, stages):
    for it in range(n + len(stages) - 1):
        for k, f in enumerate(stages):
            t = it - k
            if 0 <= t < n:
                f(t)


class Arena:
    def __init__(self, A, nwords):
        self.A = A
        self.n = nwords
        self.top = 0

    def f32(self, n):
        off = self.top
        self.top += n
        assert self.top <= self.n, ("arena overflow", self.top, self.n)
        return self.A[:, off:off + n]

    def bf16(self, n):
        w = (n + 1) // 2
        off = self.top
        self.top += w
        assert self.top <= self.n, ("arena overflow", self.top, self.n)
        return self.A[:, off:off + w].bitcast(BF16)

    def sub(self, start, n):
        a = Arena(self.A, start + n)
        a.top = start
        return a

    def mark(self):
        return self.top

    def release(self, m):
        self.top = m


def build_nc(phases=6, debug=False, stop6=99, subB=99):
    nc = bass.Bass("TRN2", target_bir_lowering=False)

    def din(name, shape, dt=F32):
        return nc.dram_tensor(name, list(shape), dt, kind="ExternalInput").ap()

    def dscr(name, shape, dt=F32):
        return nc.dram_tensor(name, list(shape), dt, kind="Internal").ap()

    x = din("x", [S, D])
    w_in = din("w_in", [D, 2560])
    perm_d = din("perm", [128, 128])
    g1rep_d = din("g1rep", [128, D])
    g2rep_d = din("g2rep", [128, D])
    fgrep_d = din("fgrep", [128, D])
    ropec_d = din("ropec", [128, S])
    ropes_d = din("ropes", [128, S])
    lam4_d = din("lam4", [128, 4, 64])
    subln_d = din("subln", [128, 1])
    convw_d = din("convw", [128, 4, 4])
    convb_d = din("convb", [128, 4])
    wbd_d = din("wbd", [128, 16, 128])
    lrub_d = din("lrub", [128, 16])
    lrul_d = din("lrul", [128, 8])
    w_out = din("w_out", [D, D])
    w_rt = din("w_rt", [D, 36])
    w_gate = din("w_gate", [NE, D, 256])
    w_up = din("w_up", [NE, D, 256])
    w_down = din("w_down", [NE, 256, D])
    ident_d = din("ident", [128, 128])
    out = nc.dram_tensor("out", [S, D], F32, kind="ExternalOutput").ap()

    hT_d = dscr("hT_d", [8, 128, S], BF16)
    rnn_d = dscr("rnn_d", [4, 128, S], BF16)
    x1_d = dscr("x1_d", [S, D])
    h2_d = dscr("h2_d", [S, D], BF16)
    xs_d = dscr("xs_d", [64 * 256, D], BF16)
    ys_d = dscr("ys_d", [64 * 256, D], BF16)
    wall_d = dscr("wall_d", [NE * 128, 6144], BF16)
    pidx_d = din("pidx", [128, 1])
    umat_d = din("umat", [128, 128])
    thr_d = din("thr", [128, 64])

    dbg = {}
    if debug:
        dbg['attnT'] = nc.dram_tensor("dbg_attnT", [128, 4 * S], F32, kind="ExternalOutput").ap()
        dbg['rnnT'] = nc.dram_tensor("dbg_rnnT", [128, 4 * S], F32, kind="ExternalOutput").ap()
        dbg['qT'] = nc.dram_tensor("dbg_qT", [128, 4 * S], F32, kind="ExternalOutput").ap()
        dbg['kT'] = nc.dram_tensor("dbg_kT", [128, 4 * S], F32, kind="ExternalOutput").ap()
        dbg['v'] = nc.dram_tensor("dbg_v", [128, 32 * 512], F32, kind="ExternalOutput").ap()
        dbg['x1'] = nc.dram_tensor("dbg_x1", [S, D], F32, kind="ExternalOutput").ap()
        dbg['route'] = nc.dram_tensor("dbg_route", [128, 192], F32, kind="ExternalOutput").ap()
        dbg['lg'] = nc.dram_tensor("dbg_lg", [128, 32 * 36], F32, kind="ExternalOutput").ap()

    NW = 53000
    with ExitStack() as es:
        A = es.enter_context(nc.sbuf_tensor("A", [128, NW], F32))
        PS = es.enter_context(nc.psum_tensor("PS", [128, 8, 512], F32))
        P = Prog(nc)
        ar = Arena(A, NW)

        def bank(i):
            return PS[:, i, :]

        def bank_bf(i):
            return PS[:, i, :].bitcast(BF16)

        ident = ar.bf16(128)
        ones = ar.bf16(128)
        g1rep = ar.f32(D)
        epsT = ar.f32(1)
        oneT = ar.f32(1)
        small = ar.f32(64)
        cst = ar.f32(8)
        lrub = ar.f32(16)
        convw = ar.f32(16)
        convb = ar.f32(4)
        subg = ar.f32(1)
        neglam = ar.f32(1)
        base = ar.mark()

        m = ar.mark()
        tmpI = ar.f32(128)
        P.dma(tmpI, ident_d)
        P.copy(ident, tmpI)
        P.memset(ones, 1.0)
        P.memset(epsT, EPS)
        P.memset(oneT, 1.0)
        P.dma(g1rep, g1rep_d)
        P.dma(lrub, lrub_d)
        P.dma(convw, convw_d.rearrange("p a b -> p (a b)"))
        P.dma(convb, convb_d)
        P.dma(subg, subln_d)
        lrul = ar.f32(8)
        P.dma(lrul, lrul_d)
        P.act(lrul, lrul, AF.Exp, scale=-1.0)
        P.ts(lrul, lrul, 1.0, None, ALU.add)
        P.act(lrul, lrul, AF.Ln)
        P.ts(cst, lrul, -8.0, None, ALU.mult)
        lam4 = ar.f32(256)
        P.dma(lam4, lam4_d.rearrange("p a b -> p (a b)"))
        pr = ar.f32(128)
        P.tt(pr[:, 0:64], lam4[:, 0:64], lam4[:, 64:128], ALU.mult)
        P.tt(pr[:, 64:128], lam4[:, 128:192], lam4[:, 192:256], ALU.mult)
        P.op('vector', lambda e: e.reduce_sum(small[:, 0:1], pr[:, 0:64], AX.X), reads=[pr[:, 0:64]], writes=[small[:, 0:1]])
        P.op('vector', lambda e: e.reduce_sum(small[:, 1:2], pr[:, 64:128], AX.X), reads=[pr[:, 64:128]], writes=[small[:, 1:2]])
        P.act(small[:, 0:2], small[:, 0:2], AF.Exp)
        P.tt(small[:, 2:3], small[:, 1:2], small[:, 0:1], ALU.subtract)
        P.ts(neglam, small[:, 2:3], -0.2, None, ALU.add)
        P.ts(subg, subg, 0.8, None, ALU.mult)
        ar.release(m)

        m = ar.mark()
        xt = [ar.f32(D) for _ in range(4)]
        junk = ar.f32(D)
        hb = [ar.bf16(D) for _ in range(2)]
        hTc = [ar.bf16(8 * 512) for _ in range(2)]
        ss = ar.f32(NT)
        rs = ar.f32(NT)
        def p1_s1(t):
            xb = xt[t % 4]
            P.dma(xb, x[t * 128:(t + 1) * 128, :])
            P.act(junk, xb, AF.Square, accum_out=ss[:, t:t + 1])
            P.act(rs[:, t:t + 1], ss[:, t:t + 1], AF.Ln, scale=1.0 / D, bias=epsT[:, 0:1])
            P.act(rs[:, t:t + 1], rs[:, t:t + 1], AF.Exp, scale=-0.5)
            P.stt(hb[t % 2], xb, rs[:, t:t + 1], g1rep, ALU.mult, ALU.mult)

        def p1_s2(t):
            ch, tt = divmod(t, 4)
            pb = bank_bf(t % 2)
            for fc in range(8):
                P.transpose(pb[:, fc * 128:(fc + 1) * 128], hb[t % 2][:, fc * 128:(fc + 1) * 128], ident)
            dst = hTc[ch % 2].rearrange("p (f t) -> p f t", f=8)[:, :, tt * 128:(tt + 1) * 128]
            src = pb.rearrange("p (f t) -> p f t", f=8)
            if t % 2 == 0:
                P.copy(dst, src, eng='vector')
            else:
                P.op('scalar', lambda e, dst=dst, src=src: e.copy(dst, src), reads=[src], writes=[dst])
            if tt == 3:
                P.dma(hT_d[:, :, ch * 512:(ch + 1) * 512].rearrange("f p t -> p f t"),
                      hTc[ch % 2].rearrange("p (f t) -> p f t", f=8), eng='gpsimd')

        pipeline(NT, [p1_s1, p1_s2])
        ar.release(m)

        if phases >= 2:
            m = ar.mark()
            wl = ar.bf16(8 * 1024)
            wlv = wl.rearrange("p (f n) -> p f n", f=8)
            wbd = ar.bf16(16 * 128)
            wbdv = wbd.rearrange("p (a n) -> p a n", a=16)
            m_stg = ar.mark()
            stg = [ar.f32(1024) for _ in range(2)]
            for fc in range(8):
                P.dma(stg[fc % 2], w_in[fc * 128:(fc + 1) * 128, 1536:2560])
                (P.copy if fc % 2 == 0 else P.acopy)(wlv[:, fc, :], stg[fc % 2])
            for a in range(4):
                st = stg[a % 2][:, 0:512]
                P.dma(st.rearrange("p (a n) -> p a n", a=4), wbd_d[:, a * 4:(a + 1) * 4, :])
                (P.copy if a % 2 == 0 else P.acopy)(wbd[:, a * 512:(a + 1) * 512], st)
            ar.release(m_stg)
            hc = [ar.bf16(8 * 512) for _ in range(2)]
            XRb = [ar.f32(S + 4) for _ in range(2)]
            Gb = [ar.f32(S) for _ in range(2)]
            XCb = [ar.f32(S) for _ in range(2)]
            xcbb = [ar.bf16(S) for _ in range(2)]
            IB = ar.f32(S)
            AB = ar.f32(S)
            HB = ar.f32(S)
            QN = 2
            QS = S // QN
            hcn = [0]

            def project_steps(ct):
                XR = XRb[ct % 2]
                G = Gb[ct % 2]
                steps = []
                for ch in range(NCH):
                    def st(ch=ch):
                        if ch == 0:
                            P.memset(XR[:, 0:2], 0.0)
                            P.memset(XR[:, S + 2:S + 4], 0.0)
                        hcb = hc[hcn[0] % 2]
                        hcn[0] += 1
                        hcv = hcb.rearrange("p (f t) -> p f t", f=8)
                        P.dma(hcv, hT_d[:, :, ch * 512:(ch + 1) * 512].rearrange("f p t -> p f t"))
                        pa = bank(2 * (ch % 2))
                        pg = bank(2 * (ch % 2) + 1)
                        for fc in range(8):
                            P.mm(pa, wlv[:, fc, ct * 128:(ct + 1) * 128], hcv[:, fc, :], start=(fc == 0), stop=(fc == 7))
                        for fc in range(8):
                            P.mm(pg, wlv[:, fc, 512 + ct * 128:512 + (ct + 1) * 128], hcv[:, fc, :], start=(fc == 0), stop=(fc == 7))
                        o_ = XR[:, 2 + ch * 512:2 + (ch + 1) * 512]
                        P.op('scalar', lambda e, o=o_, i=pa: e.copy(o, i), reads=[pa], writes=[o_])
                        P.copy(G[:, ch * 512:(ch + 1) * 512], pg, eng='vector')
                    steps.append(st)
                return steps

            def conv_steps(ct):
                XR = XRb[ct % 2]
                XC = XCb[ct % 2]
                xcb = xcbb[ct % 2]
                cw = lambda j: convw[:, ct * 4 + j:ct * 4 + j + 1]
                steps = []
                for q in range(QN):
                    def st(q=q):
                        sl = slice(q * QS, (q + 1) * QS)
                        P.act(XC[:, sl], XR[:, q * QS:q * QS + QS], AF.Identity, scale=cw(0), bias=convb[:, ct:ct + 1])
                        for j in range(1, 4):
                            P.stt(XC[:, sl], XR[:, j + q * QS:j + q * QS + QS], cw(j), XC[:, sl], ALU.mult, ALU.add)
                        P.op('scalar', lambda e, o=xcb[:, sl], i_=XC[:, sl]: e.copy(o, i_), reads=[XC[:, sl]], writes=[xcb[:, sl]])
                    steps.append(st)
                return steps

            def y_steps(ct):
                R = XRb[ct % 2][:, 0:S]
                XC = XCb[ct % 2]
                xcb = xcbb[ct % 2]
                G = Gb[ct % 2]
                rnnb = xcbb[ct % 2]
                steps = []
                for dr in range(2):
                    ir = (0 * 2 + dr) * 4 + ct
                    ii = (1 * 2 + dr) * 4 + ct
                    ci = dr * 4 + ct
                    chs = list(range(NCH)) if dr == 0 else list(range(NCH - 1, -1, -1))
                    for ch in chs:
                        def sg(ch=ch, ir=ir, ii=ii):
                            pr_ = bank(4 + 2 * (ch % 2))
                            pi_ = bank(4 + 2 * (ch % 2) + 1)
                            cs = slice(ch * 512, (ch + 1) * 512)
                            P.mm(pr_, wbdv[:, ir, :], xcb[:, cs])
                            P.mm(pi_, wbdv[:, ii, :], xcb[:, cs])
                            P.act(R[:, cs], pr_, AF.Sigmoid, bias=lrub[:, ir:ir + 1])
                            P.act(IB[:, cs], pi_, AF.Sigmoid, bias=lrub[:, ii:ii + 1])
                        steps.append(sg)
                    qorder = list(range(QN)) if dr == 0 else list(range(QN - 1, -1, -1))
                    for q in qorder:
                        def s1(q=q, ci=ci):
                            sl = slice(q * QS, (q + 1) * QS)
                            P.tt(IB[:, sl], IB[:, sl], XC[:, sl], ALU.mult)
                            P.act(AB[:, sl], R[:, sl], AF.Exp, scale=cst[:, ci:ci + 1])
                        steps.append(s1)
                    for q in qorder:
                        def s2(q=q):
                            sl = slice(q * QS, (q + 1) * QS)
                            P.act(R[:, sl], AB[:, sl], AF.Square)
                        steps.append(s2)
                    for q in qorder:
                        def s3(q=q, dr=dr):
                            sl = slice(q * QS, (q + 1) * QS)
                            P.act(R[:, sl], R[:, sl], AF.Sqrt, scale=-1.0, bias=oneT[:, 0:1])
                            P.tt(IB[:, sl], IB[:, sl], R[:, sl], ALU.mult)
                            if dr == 0:
                                init = 0.0 if q == 0 else HB[:, q * QS - 1:q * QS]
                                rd = [AB[:, sl], IB[:, sl]] + ([] if q == 0 else [init])
                                P.op('vector', lambda e, sl=sl, init=init: e.tensor_tensor_scan(HB[:, sl], AB[:, sl], IB[:, sl], init, ALU.mult, ALU.add),
                                     reads=rd, writes=[HB[:, sl]])
                            else:
                                init = 0.0 if q == QN - 1 else R[:, (q + 1) * QS:(q + 1) * QS + 1]
                                rd = [AB[:, sl], IB[:, sl]] + ([] if q == QN - 1 else [init])
                                rs_ = slice((q + 1) * QS - 1, q * QS - 1 if q > 0 else None, -1)
                                P.op('vector', lambda e, rs_=rs_, init=init, R=R: e.tensor_tensor_scan(R[:, rs_], AB[:, rs_], IB[:, rs_], init, ALU.mult, ALU.add),
                                     reads=rd, writes=[R[:, sl]])
                                P.tt(HB[:, sl], HB[:, sl], R[:, sl], ALU.add)
                        steps.append(s3)
                for q in range(QN):
                    def g1(q=q):
                        sl = slice(q * QS, (q + 1) * QS)
                        P.act(R[:, sl], G[:, sl], AF.Square)
                        P.ts(R[:, sl], R[:, sl], 0.044715, 1.0, ALU.mult, ALU.add)
                        P.tt(R[:, sl], R[:, sl], G[:, sl], ALU.mult)
                    steps.append(g1)
                for q in range(QN):
                    def g2(q=q):
                        sl = slice(q * QS, (q + 1) * QS)
                        P.act(R[:, sl], R[:, sl], AF.Sigmoid, scale=1.5957691216057308)
                        P.tt(R[:, sl], R[:, sl], G[:, sl], ALU.mult)
                        P.tt(rnnb[:, sl], HB[:, sl], R[:, sl], ALU.mult)
                    steps.append(g2)

                def fin():
                    P.dma(rnn_d[ct], rnnb)
                    if debug:
                        P.copy(AB, rnnb)
                        P.dma(dbg['rnnT'][:, ct * S:(ct + 1) * S], AB)
                steps.append(fin)
                return steps

            for f_ in project_steps(0) + conv_steps(0):
                f_()
            for ct in range(4):
                ys = y_steps(ct)
                xs_ = (project_steps(ct + 1) + conv_steps(ct + 1)) if ct + 1 < 4 else []
                pos = {}
                npj = NCH if xs_ else 0
                ppos_ = [1, 3, 6, 9, 11, 14, 17, 19]
                for i_ in range(npj):
                    pos.setdefault(ppos_[i_], []).append(xs_[i_])
                for i_, f_ in enumerate(xs_[npj:]):
                    pos.setdefault(23 + 4 * i_, []).append(f_)
                for i_, f_ in enumerate(ys):
                    f_()
                    for g_ in pos.get(i_, []):
                        g_()
            ar.release(m)

        if phases >= 3:
            attn_start = ar.mark()
            attnT = ar.bf16(4 * S)
            attnTv = attnT.rearrange("p (h t) -> p h t", h=4)
            m_attn = ar.mark()
            ar3 = ar.sub(attn_start, m_attn - attn_start)
            QT = ar.bf16(4 * S)
            KT = ar.bf16(4 * S)
            V = ar.bf16(32 * 512)
            QTv = QT.rearrange("p (h t) -> p h t", h=4)
            KTv = KT.rearrange("p (h t) -> p h t", h=4)
            Vv = V.rearrange("p (k e) -> p k e", k=32)
            m3 = ar.mark()
            wa = ar.bf16(8 * 1536)
            wav = wa.rearrange("p (f n) -> p f n", f=8)
            stg = [ar.f32(1536) for _ in range(2)]
            for fc in range(8):
                P.dma(stg[fc % 2], w_in[fc * 128:(fc + 1) * 128, 0:1536])
                (P.copy if fc % 2 == 0 else P.acopy)(wav[:, fc, 0:1536], stg[fc % 2])
            permb = ar.bf16(128)
            P.dma(stg[0][:, 0:128], perm_d)
            P.copy(permb, stg[0][:, 0:128])
            hc = [ar3.bf16(8 * 512) for _ in range(2)]
            rc = [ar3.f32(512) for _ in range(2)]
            rsn = [ar3.f32(512) for _ in range(2)]
            t1 = [ar3.f32(512) for _ in range(2)]
            t2 = [ar3.f32(512) for _ in range(2)]
            qb = [ar.bf16(512) for _ in range(2)]
            k = 0
            for ch in range(NCH):
                hcb = hc[ch % 2]
                hcv = hcb.rearrange("p (f t) -> p f t", f=8)
                P.dma(hcv, hT_d[:, :, ch * 512:(ch + 1) * 512].rearrange("f p t -> p f t"))
                P.dma(rc[ch % 2], ropec_d[:, ch * 512:(ch + 1) * 512])
                P.dma(rsn[ch % 2], ropes_d[:, ch * 512:(ch + 1) * 512])
                sl = slice(ch * 512, (ch + 1) * 512)
                for qk in range(2):
                    dstv = QTv if qk == 0 else KTv
                    for h in range(4):
                        pq = bank(2 * (k % 2))
                        pqs = bank(2 * (k % 2) + 1)
                        c0 = qk * 512 + h * 128
                        for fc in range(8):
                            P.mm(pq, wav[:, fc, c0:c0 + 128], hcv[:, fc, :], start=(fc == 0), stop=(fc == 7))
                        P.acopy(qb[k % 2], pq)
                        P.mm(pqs, permb, qb[k % 2])
                        P.op('vector', lambda e, o=t1[k % 2], a=pq, b_=rc[ch % 2]: e.tensor_tensor(o, a, b_, ALU.mult),
                             reads=[pq, rc[ch % 2], qb[k % 2]], writes=[t1[k % 2]])
                        P.tt(t2[k % 2], pqs, rsn[ch % 2], ALU.mult)
                        P.tt(dstv[:, h, sl], t1[k % 2], t2[k % 2], ALU.add)
                        k += 1
                for tt in range(4):
                    pv = bank(4 + (tt % 2))
                    for fc in range(8):
                        P.mm(pv, hcv[:, fc, tt * 128:(tt + 1) * 128], wav[:, fc, 1024:1536], start=(fc == 0), stop=(fc == 7))
                    P.op('scalar', lambda e, o=Vv[:, ch * 4 + tt, :], i=pv: e.copy(o, i), reads=[pv], writes=[Vv[:, ch * 4 + tt, :]])
            ar.release(m3)
            if debug:
                m = ar.mark()
                tmpf = ar.f32(4 * S)
                P.copy(tmpf, QT)
                P.dma(dbg['qT'], tmpf)
                P.copy(tmpf, KT)
                P.dma(dbg['kT'], tmpf)
                P.copy(tmpf, V)
                P.dma(dbg['v'], tmpf)
                ar.release(m)

        if phases >= 4:
            m4 = ar.mark()
            E = [ar.bf16(1024) for _ in range(3)]
            rz1 = ar.f32(512)
            rz2 = ar.f32(512)
            ob = ar.f32(512)
            tb = ar.f32(512)
            sqb = ar.bf16(512)
            msb = ar.f32(512)
            SC = 0.125
            blk = 0
            cst_f = [ar.f32(2048) for _ in range(3)]
            cst_b = [ar.bf16(2048) for _ in range(3)]
            zacc2 = [ar.f32(512) for _ in range(2)]
            o1s = [ar.f32(512) for _ in range(2)]
            o2s = [ar.f32(512) for _ in range(2)]
            z1s = [ar.f32(512) for _ in range(2)]
            zh = ar.bf16(512)
            zl = ar.bf16(512)

            def convert_steps(e_):
                srcs = (w_gate[e_].rearrange("(f p) n -> p f n", p=128), w_up[e_].rearrange("(f p) n -> p f n", p=128),
                        w_down[e_].rearrange("(j p) n -> p j n", p=128))
                steps = []
                for q_ in range(3):
                    a_ = 8 if q_ < 2 else 2

                    def st(q_=q_, a_=a_):
                        P.dma(cst_f[q_].rearrange("p (a n) -> p a n", a=a_), srcs[q_])
                        P.copy(cst_b[q_], cst_f[q_])
                        P.dma(wall_d[e_ * 128:(e_ + 1) * 128, q_ * 2048:(q_ + 1) * 2048], cst_b[q_])
                    steps.append(st)
                return steps

            def epilogue_steps(h, qc, pb_):
                qs = slice(qc * 512, (qc + 1) * 512)
                B7 = bank(7)
                za = zacc2[pb_]

                def e1():
                    P.copy(zh, za)

                def e2():
                    P.tt(zl, za, zh, ALU.subtract)

                def e3():
                    P.mm(B7, ones, zh, start=True, stop=False)
                    P.mm(B7, ones, zl, start=False, stop=True)

                def e4():
                    P.act(rz2, B7, AF.Ln)
                    P.act(rz2, rz2, AF.Exp, scale=-1.0)

                def e5():
                    P.act(rz1, z1s[pb_], AF.Ln)
                    P.act(rz1, rz1, AF.Exp, scale=-1.0)

                def e6():
                    P.tt(ob, o1s[pb_], rz1, ALU.mult)
                    P.tt(tb, o2s[pb_], rz2, ALU.mult)

                def e7():
                    P.stt(ob, tb, neglam[:, 0:1], ob, ALU.mult, ALU.add)

                def e8():
                    P.tt(sqb, ob, ob, ALU.mult)

                def e9():
                    P.mm(B7, ones, sqb)

                def e10():
                    P.act(msb, B7, AF.Ln, scale=1.0 / 128, bias=epsT[:, 0:1])
                    P.act(msb, msb, AF.Exp, scale=-0.5)

                def e11():
                    P.stt(attnTv[:, h, qs], ob, subg[:, 0:1], msb, ALU.mult, ALU.mult)
                return [e1, e2, e3, e4, e5, e6, e7, e8, e9, e10, e11]

            pending = []
            for h in range(4):
                for qc in range(NCH):
                    qs = slice(qc * 512, (qc + 1) * 512)
                    O1, O2, Z1 = bank(4), bank(5), bank(6)
                    pb_ = blk % 2
                    sched = {}
                    for i_, f_ in enumerate(pending):
                        sched.setdefault(1 + i_, []).append(f_)
                    for i_, f_ in enumerate(convert_steps(blk)):
                        sched.setdefault(14 + 6 * i_, []).append(f_)
                    pending = []

                    def qk(kt):
                        sb = 2 * (kt % 2)
                        ks = slice(kt * 128, (kt + 1) * 128)
                        P.mm(bank(sb), KTv[0:64, h, ks], QTv[0:64, h, qs], tile_position=(0, 0))
                        P.mm(bank(sb + 1), KTv[64:128, h, ks], QTv[64:128, h, qs], tile_position=(64, 0))

                    qk(0)
                    for kt in range(32):
                        if kt + 1 < 32:
                            qk(kt + 1)
                        sb = 2 * (kt % 2)
                        Eb = E[kt % 3]
                        P.act(Eb.rearrange("p (a n) -> p a n", a=2), PS[:, sb:sb + 2, :], AF.Exp, scale=SC)
                        st = (kt == 0)
                        sp = (kt == 31)
                        vs = Vv[:, kt, h * 128:(h + 1) * 128]
                        P.mm(O1, vs, Eb[:, 0:512], start=st, stop=sp)
                        P.mm(O2, vs, Eb[:, 512:1024], start=st, stop=sp)
                        P.mm(Z1, ones, Eb[:, 0:512], start=st, stop=sp)
                        if kt == 0:
                            P.copy(zacc2[pb_], Eb[:, 512:1024])
                        else:
                            P.tt(zacc2[pb_], zacc2[pb_], Eb[:, 512:1024], ALU.add)
                        for f_ in sched.get(kt, []):
                            f_()
                    P.copy(o1s[pb_], O1)
                    P.copy(o2s[pb_], O2)
                    P.copy(z1s[pb_], Z1)
                    pending = epilogue_steps(h, qc, pb_)
                    blk += 1
            for f_ in pending:
                f_()
            ar.release(m4)
            if debug:
                m = ar.mark()
                tmpf = ar.f32(4 * S)
                P.copy(tmpf, attnT)
                P.dma(dbg['attnT'], tmpf)
                ar.release(m)

        if phases >= 5:
            ar.release(m_attn)
            m5 = ar.mark()
            rnnT = ar.bf16(4 * S)
            rnnTv = rnnT.rearrange("p (c t) -> p c t", c=4)
            for ct in range(4):
                P.dma(rnnTv[:, ct, :], rnn_d[ct])
            wo = ar.bf16(8 * 1024)
            wov = wo.rearrange("p (f n) -> p f n", f=8)
            m_stg5 = ar.mark()
            stg = [ar.f32(1024) for _ in range(2)]
            for fc in range(8):
                P.dma(stg[fc % 2], w_out[fc * 128:(fc + 1) * 128, :])
                (P.copy if fc % 2 == 0 else P.acopy)(wov[:, fc, :], stg[fc % 2])
            ar.release(m_stg5)
            xt = [ar.f32(D) for _ in range(4)]
            x1t = [ar.f32(D) for _ in range(3)]

            def p5_stage(t):
                xb = xt[t % 4]
                P.dma(xb, x[t * 128:(t + 1) * 128, :])
                ts_ = slice(t * 128, (t + 1) * 128)
                for nch in range(2):
                    po = bank(6 + nch)
                    for fc in range(8):
                        lhs = attnTv[:, fc, ts_] if fc < 4 else rnnTv[:, fc - 4, ts_]
                        P.mm(po, lhs, wov[:, fc, nch * 512:(nch + 1) * 512], start=(fc == 0), stop=(fc == 7))
                    P.tt(x1t[t % 3][:, nch * 512:(nch + 1) * 512], po, xb[:, nch * 512:(nch + 1) * 512], ALU.add)
                P.dma(x1_d[t * 128:(t + 1) * 128, :], x1t[t % 3], eng='gpsimd')
                if debug:
                    P.dma(dbg['x1'][t * 128:(t + 1) * 128, :], x1t[t % 3])

            if phases < 6:
                for t in range(NT):
                    p5_stage(t)

        if phases >= 6:
            TS = 256
            NTL = 64
            NSLOT = NTL * TS
            BIG = 1.0e30
            fgrep = ar.f32(D)
            P.dma(fgrep, fgrep_d)
            slotA = ar.f32(32).bitcast(I32)
            slotB = ar.f32(32).bitcast(I32)
            wA = ar.f32(32)
            wB = ar.f32(32)
            EI = ar.f32(NTL).bitcast(I32)
            sm = ar.f32(64)
            mA = ar.mark()
            g2rep = ar.f32(D)
            P.dma(g2rep, g2rep_d)
            whi = ar.bf16(8 * 36)
            wlo = ar.bf16(8 * 36)
            whiv = whi.rearrange("p (f n) -> p f n", f=8)
            wlov = wlo.rearrange("p (f n) -> p f n", f=8)
            wrt = ar.f32(8 * 36)
            wrt2 = ar.f32(8 * 36)
            P.dma(wrt.rearrange("p (f n) -> p f n", f=8), w_rt.rearrange("(f p) n -> p f n", p=128))
            P.copy(whi, wrt)
            P.copy(wrt2, whi)
            P.tt(wlo, wrt, wrt2, ALU.subtract)
            umat = ar.bf16(128)
            tmpU = ar.f32(128)
            P.dma(tmpU, umat_d)
            P.copy(umat, tmpU)
            thr = ar.f32(NTL)
            P.dma(thr, thr_d)
            pidx = ar.f32(1)
            P.dma(pidx, pidx_d)
            hib = [ar.bf16(D) for _ in range(2)]
            LG = ar.f32(NT * 36)
            LGv = LG.rearrange("p (t n) -> p t n", t=NT)
            h2 = [ar.f32(D) for _ in range(2)]
            lo = [ar.bf16(D) for _ in range(2)]
            hiT = [ar.bf16(8 * 128) for _ in range(2)]
            loT = [ar.bf16(8 * 128) for _ in range(2)]
            junk = ar.f32(D)
            ssq = ar.f32(NT)
            rsq = ar.f32(NT)
            def a_s1(t):
                b = t % 2
                xin = x1t[t % 3]
                P.act(junk, xin, AF.Square, accum_out=ssq[:, t:t + 1])
                P.act(rsq[:, t:t + 1], ssq[:, t:t + 1], AF.Ln, scale=1.0 / D, bias=epsT[:, 0:1])
                P.act(rsq[:, t:t + 1], rsq[:, t:t + 1], AF.Exp, scale=-0.5)
                P.stt(h2[b], xin, rsq[:, t:t + 1], g2rep, ALU.mult, ALU.mult)
                hi = hib[b]
                P.op('scalar', lambda e, o=hi, i_=h2[b]: e.copy(o, i_), reads=[h2[b]], writes=[hi])
                P.tt(lo[b], h2[b], hi, ALU.subtract)
                P.dma(h2_d[t * 128:(t + 1) * 128, :], hi, eng='gpsimd')

            def a_s2(t):
                b = t % 2
                hi = hib[b]
                pb = bank_bf(2 * b)
                pl = bank_bf(2 * b + 1)
                for fc in range(8):
                    P.transpose(pb[:, fc * 128:(fc + 1) * 128], hi[:, fc * 128:(fc + 1) * 128], ident)
                for fc in range(8):
                    P.transpose(pl[:, fc * 128:(fc + 1) * 128], lo[b][:, fc * 128:(fc + 1) * 128], ident)
                P.copy(hiT[b], pb)
                P.op('scalar', lambda e, o=loT[b], i=pl: e.copy(o, i), reads=[pl], writes=[loT[b]])

            def a_s3(t):
                b = t % 2
                hv = hiT[b].rearrange("p (f t) -> p f t", f=8)
                lv = loT[b].rearrange("p (f t) -> p f t", f=8)
                plg = bank(4 + b)[:, 0:36]
                n = 0
                for fc in range(8):
                    for (lh, rh) in ((hv[:, fc, :], whiv[:, fc, :]), (hv[:, fc, :], wlov[:, fc, :]), (lv[:, fc, :], whiv[:, fc, :])):
                        P.mm(plg, lh, rh, start=(n == 0), stop=(n == 23))
                        n += 1
                P.copy(LGv[:, t, :], plg)

            pipeline(NT, [p5_stage, a_s1, a_s2, a_s3])
            if debug:
                P.dma(dbg['lg'], LG)
            GL = LGv[:, :, 0:4]
            EL = LGv[:, :, 4:36].rearrange("p t (g e) -> p t g e", g=4)
            T1 = ar.f32(NT * 32)
            T2 = ar.f32(NT * 32)
            MA = ar.f32(NT * 32)
            MS = ar.f32(NT * 32)
            ELM = ar.f32(NT * 32)
            v3 = lambda a: a.rearrange("p (t e) -> p t e", t=NT)
            v4 = lambda a: a.rearrange("p (t g e) -> p t g e", t=NT, g=4)
            g4 = ar.f32(NT * 4)
            g4b = ar.f32(NT * 4)
            g4v = g4.rearrange("p (t g) -> p t g", g=4)
            g4bv = g4b.rearrange("p (t g) -> p t g", g=4)
            gmax = ar.f32(NT)
            gtp = ar.f32(NT)
            v1 = ar.f32(NT)
            v2 = ar.f32(NT)
            d21 = ar.f32(NT)
            bc3 = lambda a, n: a.unsqueeze(2).to_broadcast([128, NT, n])
            red = lambda o, i, op: P.op('vector', lambda e: e.tensor_reduce(o, i, AX.X, op), reads=[i], writes=[o])
            red(gmax, GL, ALU.max)
            P.tt(g4v, GL, bc3(gmax, 4), ALU.subtract)
            P.act(g4b, g4, AF.Exp)
            red(gtp, g4bv, ALU.add)
            P.recip(gtp, gtp)
            P.tt(g4v, GL, bc3(gmax, 4), ALU.is_ge)
            P.ts(g4, g4, BIG, -BIG, ALU.mult, ALU.add)
            P.tt(v4(ELM), EL, g4v.unsqueeze(3).to_broadcast([128, NT, 4, 8]), ALU.add)
            red(v1, v3(ELM), ALU.max)
            P.tt(v3(MA), v3(ELM), bc3(v1, 32), ALU.is_ge)
            P.stt(T1, MA, -BIG, ELM, ALU.mult, ALU.add)
            red(v2, v3(T1), ALU.max)
            P.tt(v3(MS), v3(ELM), bc3(v2, 32), ALU.is_ge)
            P.tt(T2, MS, MA, ALU.subtract)
            P.tt(d21, v2, v1, ALU.subtract)
            P.act(d21, d21, AF.Exp)
            P.ts(T1[:, 0:NT], d21, 1.0, None, ALU.add)
            P.recip(T1[:, 0:NT], T1[:, 0:NT])
            P.tt(wA, T1[:, 0:NT], gtp, ALU.mult)
            P.tt(wB, wA, d21, ALU.mult)
            MSb = ar.bf16(NT * 32)
            P.copy(MSb, MS)
            MSbv = v3(MSb)
            ppos = PS[:, 0:2, :].rearrange("p a n -> p (a n)")
            pcs = PS[:, 2:4, :].rearrange("p a n -> p (a n)")
            for t in range(NT):
                P.mm(ppos[:, t * 32:(t + 1) * 32], umat, MSbv[:, t, :])
                P.mm(pcs[:, t * 32:(t + 1) * 32], ones, MSbv[:, t, :])
            CS = ar.f32(NT * 32)
            P.copy(CS, pcs)
            BASE = ar.f32((NT + 1) * 32)
            P.memset(BASE[:, 0:32], 0.0)
            for t in range(NT):
                P.tt(BASE[:, (t + 1) * 32:(t + 2) * 32], BASE[:, t * 32:(t + 1) * 32], CS[:, t * 32:(t + 1) * 32], ALU.add)
            ntot = BASE[:, NT * 32:(NT + 1) * 32]
            npad = ar.f32(32)
            P.ts(npad, ntot, 0.0, None, ALU.is_gt)
            for kk in range(1, S // TS):
                P.stt(npad, ntot, float(kk * TS), npad, ALU.is_gt, ALU.add)
            P.ts(npad, npad, float(TS), None, ALU.mult)
            onesf = ar.f32(32)
            P.memset(onesf, 1.0)
            endp = ar.f32(32)
            P.op('vector', lambda e: e.tensor_tensor_scan(endp, onesf, npad, 0.0, ALU.mult, ALU.add), reads=[onesf, npad], writes=[endp])
            startp = ar.f32(32)
            P.tt(startp, endp, npad, ALU.subtract)
            P.tt(T1, ppos, BASE[:, 0:NT * 32], ALU.add)
            P.tt(v3(T1), v3(T1), startp.unsqueeze(1).to_broadcast([128, NT, 32]), ALU.add)
            P.tt(ELM, T1, MA, ALU.mult)
            sf = ar.f32(NT)
            red(sf, v3(ELM), ALU.add)
            P.copy(slotA, sf)
            P.tt(ELM, T1, T2, ALU.mult)
            red(sf, v3(ELM), ALU.add)
            P.copy(slotB, sf)
            TE = ar.f32(NTL * 32)
            TEv = TE.rearrange("p (i e) -> p i e", i=NTL)
            P.tt(TEv, endp.unsqueeze(1).to_broadcast([128, NTL, 32]), thr.unsqueeze(2).to_broadcast([128, NTL, 32]), ALU.is_le)
            eif = ar.f32(NTL)
            red(eif, TEv, ALU.add)
            P.ts(eif, eif, 31.0, None, ALU.min)
            P.ts(eif, eif, 128.0, pidx[:, 0:1], ALU.mult, ALU.add)
            inval = ar.f32(NTL)
            P.ts(inval, thr, endp[:, 31:32], 1.0e6, ALU.is_ge, ALU.mult)
            P.tt(eif, eif, inval, ALU.add)
            P.copy(EI, eif)
            if debug:
                dtmp = ar.f32(192)
                P.copy(dtmp[:, 0:32], slotA)
                P.copy(dtmp[:, 32:64], slotB)
                P.copy(dtmp[:, 64:96], wA)
                P.copy(dtmp[:, 96:128], wB)
                P.copy(dtmp[:, 128:192], EI)
                P.dma(dbg['route'], dtmp)
            hst = [ar.bf16(D) for _ in range(3)]
            for t in range(NT if stop6 >= 2 else 0):
                hb_ = hst[t % 3]
                P.dma(hb_, h2_d[t * 128:(t + 1) * 128, :])
                for sl_ in (slotA, slotB):
                    P.op('gpsimd', lambda e, sl_=sl_, t=t, hb_=hb_: e.indirect_dma_start(
                        out=xs_d[:, :], out_offset=bass.IndirectOffsetOnAxis(ap=sl_[:, t:t + 1], axis=0),
                        in_=hb_, in_offset=None),
                        reads=[hb_, sl_[:, t:t + 1]], writes=[xs_d], is_dma=True, partial=True)
            ar.release(mA)
            mB = ar.mark()
            NWB = 3
            wB_ = [ar.bf16(6144) for _ in range(NWB)]
            xg = [ar.bf16(2 * D) for _ in range(3)]
            xgT = [ar.bf16(8 * TS) for _ in range(2)]
            sg = [ar.f32(TS) for _ in range(2)]
            hid = [ar.bf16(2 * TS) for _ in range(2)]
            ysb = [ar.bf16(2 * D) for _ in range(2)]

            def b_load(i):
                kw_ = dict(bounds_check=NE * 128 - 1, oob_is_err=False) if i >= 32 else {}
                wb = wB_[i % NWB]
                P.op('gpsimd', lambda e, wb=wb, i=i, kw_=kw_: e.indirect_dma_start(
                    out=wb, out_offset=None, in_=wall_d[:, :],
                    in_offset=bass.IndirectOffsetOnAxis(ap=EI[:, i:i + 1], axis=0), **kw_),
                    reads=[wall_d, EI[:, i:i + 1]], writes=[wb], is_dma=True)
                P.dma(xg[i % 3].rearrange("p (a n) -> p a n", a=2), xs_d[i * TS:(i + 1) * TS, :].rearrange("(a p) n -> p a n", p=128))

            def b_s1(i):
                xgv = xg[i % 3].rearrange("p (a n) -> p a n", a=2)
                xTv = xgT[i % 2].rearrange("p (f t) -> p f t", f=8)
                for a in range(2):
                    pt = bank_bf(a)
                    for fc in range(8):
                        P.transpose(pt[:, fc * 128:(fc + 1) * 128], xgv[:, a, fc * 128:(fc + 1) * 128], ident)
                    src = pt.rearrange("p (f t) -> p f t", f=8)
                    dst = xTv[:, :, a * 128:(a + 1) * 128]
                    if a == 0:
                        P.copy(dst, src)
                    else:
                        P.op('scalar', lambda e, o=dst, i_=src: e.copy(o, i_), reads=[src], writes=[dst])

            def b_s2(i):
                wb = wB_[i % NWB]
                xTv = xgT[i % 2].rearrange("p (f t) -> p f t", f=8)
                wgv = wb[:, 0:2048].rearrange("p (f n) -> p f n", f=8)
                wuv = wb[:, 2048:4096].rearrange("p (f n) -> p f n", f=8)
                hv = hid[i % 2].rearrange("p (j t) -> p j t", j=2)
                for jj in range(2):
                    pg = bank(2 + jj)[:, 0:TS]
                    pu = bank(4 + jj)[:, 0:TS]
                    for fc in range(8):
                        P.mm(pg, wgv[:, fc, jj * 128:(jj + 1) * 128], xTv[:, fc, :], start=(fc == 0), stop=(fc == 7))
                    for fc in range(8):
                        P.mm(pu, wuv[:, fc, jj * 128:(jj + 1) * 128], xTv[:, fc, :], start=(fc == 0), stop=(fc == 7))
                    P.act(sg[jj], pg, AF.Silu)
                    P.tt(hv[:, jj, :], sg[jj], pu, ALU.mult)

            def b_s3(i):
                wb = wB_[i % NWB]
                wdv = wb[:, 4096:6144].rearrange("p (j n) -> p j n", j=2)
                hv = hid[i % 2].rearrange("p (j t) -> p j t", j=2)
                yv = ysb[i % 2].rearrange("p (a n) -> p a n", a=2)
                k = 0
                for a in range(2):
                    for nch in range(2):
                        pd = bank(6 + (k % 2))
                        for jj in range(2):
                            P.mm(pd, hv[:, jj, a * 128:(a + 1) * 128], wdv[:, jj, nch * 512:(nch + 1) * 512], start=(jj == 0), stop=(jj == 1))
                        o_ = yv[:, a, nch * 512:(nch + 1) * 512]
                        if k % 2 == 0:
                            P.copy(o_, pd)
                        else:
                            P.op('scalar', lambda e, o=o_, i_=pd: e.copy(o, i_), reads=[pd], writes=[o_])
                        k += 1
                P.dma(ys_d[i * TS:(i + 1) * TS, :].rearrange("(a p) n -> p a n", p=128), yv)

            if stop6 >= 3:
                b_load(0)
                for it in range(NTL + 1):
                    if it + 1 < NTL:
                        b_load(it + 1)
                    if it >= 1:
                        b_s2(it - 1)
                    if it < NTL:
                        b_s1(it)
                    if it >= 1:
                        b_s3(it - 1)
            ar.release(mB)
            YA = [ar.bf16(D) for _ in range(3)]
            YB = [ar.bf16(D) for _ in range(3)]
            x1c = [ar.f32(D) for _ in range(3)]
            outb = [ar.f32(D) for _ in range(2)]
            junk2 = ar.f32(D)
            ss3 = ar.f32(NT)
            rs3 = ar.f32(NT)
            if stop6 < 4:
                zz = ar.f32(D)
                P.memset(zz, 0.0)
                P.dma(out[0:128, :], zz)

            def c_s1(t):
                b = t % 3
                P.dma(x1c[b], x1_d[t * 128:(t + 1) * 128, :])
                for (yy, sl_) in ((YA[b], slotA), (YB[b], slotB)):
                    P.op('gpsimd', lambda e, yy=yy, sl_=sl_, t=t: e.indirect_dma_start(
                        out=yy, out_offset=None, in_=ys_d[:, :],
                        in_offset=bass.IndirectOffsetOnAxis(ap=sl_[:, t:t + 1], axis=0)),
                        reads=[ys_d, sl_[:, t:t + 1]], writes=[yy], is_dma=True)

            def c_s2(t):
                b = t % 3
                P.stt(x1c[b], YA[b], wA[:, t:t + 1], x1c[b], ALU.mult, ALU.add)
                P.stt(x1c[b], YB[b], wB[:, t:t + 1], x1c[b], ALU.mult, ALU.add)
                P.act(junk2, x1c[b], AF.Square, accum_out=ss3[:, t:t + 1])
                P.act(rs3[:, t:t + 1], ss3[:, t:t + 1], AF.Ln, scale=1.0 / D, bias=epsT[:, 0:1])
                P.act(rs3[:, t:t + 1], rs3[:, t:t + 1], AF.Exp, scale=-0.5)

            def c_s3(t):
                b = t % 3
                P.stt(outb[t % 2], x1c[b], rs3[:, t:t + 1], fgrep, ALU.mult, ALU.mult)
                P.dma(out[t * 128:(t + 1) * 128, :], outb[t % 2])

            if stop6 >= 4:
                pipeline(NT, [c_s1, c_s2, c_s3])
        else:
            m = ar.mark()
            z = ar.f32(D)
            P.memset(z, 0.0)
            P.dma(out[0:128, :], z)
            ar.release(m)

        P.emit()
    return nc


def _rope_tables():
    pos = np.arange(S, dtype=np.float32)
    inv = (np.float32(500000.0) ** (-np.arange(0, 16, 2, dtype=np.float32) / np.float32(16))).astype(np.float32)
    ang = pos[:, None] * inv[None, :]
    cos = np.cos(ang).astype(np.float32)
    sin = np.sin(ang).astype(np.float32)
    C = np.ones((128, S), np.float32)
    Sn = np.zeros((128, S), np.float32)
    for mth in range(2):
        for d in range(16):
            p = mth * 64 + d
            C[p] = cos[:, d % 8]
            Sn[p] = -sin[:, d % 8] if d < 8 else sin[:, d % 8]
    return C, Sn


def _shared_inputs(norm1_g, w_in, lambda_q1, lambda_k1, lambda_q2, lambda_k2, subln_g, conv_w, conv_b,
                   lru_w_r, lru_b_r, lru_w_i, lru_b_i, lru_lambda, w_out, norm2_g, w_grp, w_exp,
                   w_gate, w_up, w_down, final_g):
    f = np.float32
    c = np.ascontiguousarray
    w_in0 = c(w_in[0], dtype=f)
    perm = np.arange(128)
    for sh in range(2):
        b0 = sh * 64
        perm[b0:b0 + 8] = np.arange(b0 + 8, b0 + 16)
        perm[b0 + 8:b0 + 16] = np.arange(b0, b0 + 8)
    permm = np.zeros((128, 128), f)
    permm[perm, np.arange(128)] = 1.0
    C, Sn = _rope_tables()
    lam4 = np.stack([lambda_q1[0], lambda_k1[0], lambda_q2[0], lambda_k2[0]], 0).astype(f)
    lam4 = c(np.broadcast_to(lam4[None], (128, 4, 64)))
    convw = c(conv_w[0].reshape(4, 4, 128).transpose(2, 1, 0), dtype=f)
    convb = c(conv_b[0].reshape(4, 128).T, dtype=f)
    wbd = np.zeros((128, 16, 128), f)
    lrub = np.zeros((128, 16), f)
    for gi, (wm, bm) in enumerate(((lru_w_r[0], lru_b_r[0]), (lru_w_i[0], lru_b_i[0]))):
        for dr in range(2):
            for ct in range(4):
                a = (gi * 2 + dr) * 4 + ct
                for bl in range(2):
                    wbd[bl * 64:(bl + 1) * 64, a, bl * 64:(bl + 1) * 64] = wm[dr, ct * 2 + bl]
                lrub[:, a] = bm[dr, ct * 128:(ct + 1) * 128]
    lrul = np.zeros((128, 8), f)
    for dr in range(2):
        for ct in range(4):
            lrul[:, dr * 4 + ct] = lru_lambda[0, dr, ct * 128:(ct + 1) * 128]
    rep = lambda v: c(np.broadcast_to(np.asarray(v, f).reshape(1, -1), (128, v.size)))
    return {
        "w_in": w_in0,
        "perm": permm,
        "g1rep": rep(norm1_g[0]),
        "g2rep": rep(norm2_g[0]),
        "fgrep": rep(final_g),
        "ropec": C,
        "ropes": Sn,
        "lam4": lam4,
        "subln": c(subln_g[0].reshape(128, 1), dtype=f),
        "convw": convw,
        "convb": convb,
        "wbd": wbd,
        "lrub": lrub,
        "lrul": lrul,
        "w_out": c(w_out[0], dtype=f),
        "w_rt": c(np.concatenate([w_grp[0], w_exp[0]], axis=1), dtype=f),
        "w_gate": c(w_gate[0], dtype=f),
        "w_up": c(w_up[0], dtype=f),
        "w_down": c(w_down[0], dtype=f),
        "ident": np.eye(128, dtype=f),
        "umat": np.triu(np.ones((128, 128), f), 1),
        "thr": c(np.broadcast_to((np.arange(64, dtype=f) * 256.0)[None, :], (128, 64))),
        "pidx": np.arange(128, dtype=f).reshape(128, 1),
    }


def kernel(x, **params):
    x = np.asarray(x, dtype=np.float32)
    params = {k: np.asarray(v, dtype=np.float32) for k, v in params.items()}
    shared = _shared_inputs(**params)
    nc = build_nc()
    in_maps = []
    for b in range(8):
        mp = dict(shared)
        mp["x"] = np.ascontiguousarray(x[b])
        in_maps.append(mp)
    res = run_bass_kernel_spmd(nc, in_maps, core_ids=list(range(8)))
    return np.stack([np.asarray(r["out"], dtype=np.float32) for r in res.results], axis=0)
```

```python
import math
from contextlib import ExitStack

import numpy as np
import concourse.bass as bass
import concourse.mybir as mybir
from concourse.bass_utils import run_bass_kernel_spmd

F32 = mybir.dt.float32
BF16 = mybir.dt.bfloat16
AF = mybir.ActivationFunctionType
ALU = mybir.AluOpType
AX = mybir.AxisListType

I32 = mybir.dt.int32
_DTSZ = {F32: 4, BF16: 2, I32: 4}

S = 4096
D = 1024
NT = 32
NCH = 8
NE = 32
EPS = 1e-6


def _rect(ap):
    t = ap.tensor
    sz = _DTSZ.get(ap.dtype, 4)
    dims = ap.ap
    lo = 0
    hi = 0
    space = str(ap.space).upper()
    if not ('SB' in space or 'PSUM' in space):
        for st, cnt in dims:
            if st >= 0:
                hi += st * (cnt - 1)
            else:
                lo += st * (cnt - 1)
        off = ap.offset
        return (t.name, 0, 1, (off + lo) * sz, (off + hi + 1) * sz)
    pstride = 1
    for s in list(t.shape)[1:]:
        pstride *= s
    off = ap.offset
    p0 = off // pstride
    f0 = off % pstride
    pcnt = dims[0][1]
    for st, cnt in dims[1:]:
        if st >= 0:
            hi += st * (cnt - 1)
        else:
            lo += st * (cnt - 1)
    return (t.name, p0, p0 + pcnt, (f0 + lo) * sz, (f0 + hi + 1) * sz)


def _overlap(a, b):
    return a[1] < b[2] and b[1] < a[2] and a[3] < b[4] and b[3] < a[4]


def _covers(a, b):
    return a[1] <= b[1] and a[2] >= b[2] and a[3] <= b[3] and a[4] >= b[4]


class Op:
    __slots__ = ('idx', 'eng', 'fn', 'deps', 'is_dma', 'signal', 'count', 'dsem', 'dcount', 'dprev')

    def __init__(self, idx, eng, fn, is_dma):
        self.idx = idx
        self.eng = eng
        self.fn = fn
        self.is_dma = is_dma
        self.deps = set()
        self.signal = False
        self.count = 0
        self.dsem = None
        self.dcount = 0
        self.dprev = 0


class Prog:
    ENGINES = ['sync', 'scalar', 'vector', 'gpsimd', 'tensor']

    def __init__(self, nc, n_dma_sems=32, same_engine_sync=True):
        self.nc = nc
        self.ops = []
        self.recs = {}
        self.n_dma_sems = n_dma_sems
        self.same_engine_sync = same_engine_sync

    def op(self, eng, fn, reads=(), writes=(), is_dma=False, partial=False):
        o = Op(len(self.ops), eng, fn, is_dma)
        self.ops.append(o)
        rrs = [_rect(ap) for ap in reads]
        wrs = [_rect(ap) for ap in writes]
        for r in rrs:
            for rec in self.recs.setdefault(r[0], []):
                if rec[2] and _overlap(rec[0], r):
                    o.deps.add(rec[1])
        for r in wrs:
            for rec in self.recs.setdefault(r[0], []):
                if _overlap(rec[0], r):
                    if partial and rec[2] and len(rec) > 4 and rec[4]:
                        continue
                    o.deps.add(rec[1])
        o.deps.discard(o.idx)
        for r in rrs:
            lst = self.recs[r[0]]
            done = False
            if not is_dma:
                for rec in lst:
                    if (not rec[2]) and rec[3] == eng and rec[0] == r and not self.ops[rec[1]].is_dma:
                        rec[1] = o.idx
                        done = True
                        break
            if not done:
                lst.append([r, o.idx, False, eng])
        for r in wrs:
            lst = self.recs[r[0]]
            if not partial:
                lst[:] = [rec for rec in lst if not (_covers(r, rec[0]) and rec[1] != o.idx)]
            lst.append([r, o.idx, True, eng, partial])
        return o

    def dma(self, out, in_, eng='sync'):
        return self.op(eng, lambda e: e.dma_start(out=out, in_=in_), reads=[in_], writes=[out], is_dma=True)

    def mm(self, out, lhsT, rhs, start=True, stop=True, **kw):
        rd = [lhsT, rhs] + ([] if start else [out])
        return self.op('tensor', lambda e: e.matmul(out, lhsT, rhs, start=start, stop=stop, **kw),
                       reads=rd, writes=[out])

    def transpose(self, out, in_, ident):
        return self.op('tensor', lambda e: e.transpose(out, in_, ident), reads=[in_, ident], writes=[out])

    def act(self, out, in_, func, bias=None, scale=None, accum_out=None):
        kw = {}
        rd = [in_]
        wr = [out]
        if bias is not None:
            kw['bias'] = bias
            if not isinstance(bias, (int, float)):
                rd.append(bias)
        if scale is not None:
            kw['scale'] = scale
            if not isinstance(scale, (int, float)):
                rd.append(scale)
        if accum_out is not None:
            kw['accum_out'] = accum_out
            wr.append(accum_out)
        return self.op('scalar', lambda e: e.activation(out, in_, func, **kw), reads=rd, writes=wr)

    def tt(self, out, in0, in1, op, eng='vector'):
        return self.op(eng, lambda e: e.tensor_tensor(out, in0, in1, op), reads=[in0, in1], writes=[out])

    def ts(self, out, in0, s1, s2, op0, op1=None, eng='vector'):
        rd = [in0]
        if not isinstance(s1, (int, float)):
            rd.append(s1)
        if s2 is not None and not isinstance(s2, (int, float)):
            rd.append(s2)
        kw = {}
        if op1 is not None:
            kw['op1'] = op1
        return self.op(eng, lambda e: e.tensor_scalar(out, in0, s1, s2, op0, **kw), reads=rd, writes=[out])

    def stt(self, out, in0, scalar, in1, op0, op1, eng='vector'):
        rd = [in0, in1]
        if not isinstance(scalar, (int, float)):
            rd.append(scalar)
        return self.op(eng, lambda e: e.scalar_tensor_tensor(out, in0, scalar, in1, op0, op1),
                       reads=rd, writes=[out])

    def copy(self, out, in_, eng='vector'):
        return self.op(eng, lambda e: e.tensor_copy(out, in_), reads=[in_], writes=[out])

    def memset(self, out, val, eng='vector'):
        return self.op(eng, lambda e: e.memset(out, val), reads=[], writes=[out])

    def acopy(self, out, in_):
        return self.op('scalar', lambda e: e.copy(out, in_), reads=[in_], writes=[out])

    def recip(self, out, in_):
        return self.op('vector', lambda e: e.reciprocal(out, in_), reads=[in_], writes=[out])

    def emit(self):
        nc = self.nc
        ops = self.ops
        ses = self.same_engine_sync
        for o in ops:
            for d in o.deps:
                p = ops[d]
                if p.is_dma:
                    continue
                if p.eng != o.eng or o.is_dma or (ses and p.eng != 'tensor'):
                    p.signal = True
        cnt = {e: 0 for e in self.ENGINES}
        for o in ops:
            if o.is_dma:
                continue
            if o.signal:
                cnt[o.eng] += 1
            o.count = cnt[o.eng]
        dcum = [0] * self.n_dma_sems
        k = 0
        for o in ops:
            if o.is_dma:
                s = k % self.n_dma_sems
                k += 1
                o.dsem = s
                o.dprev = dcum[s]
                dcum[s] += 16
                o.dcount = dcum[s]
        with ExitStack() as es:
            esem = {e: es.enter_context(nc.semaphore('s_' + e)) for e in self.ENGINES}
            dsems = [es.enter_context(nc.semaphore('d_%d' % i)) for i in range(self.n_dma_sems)]
            block = es.enter_context(nc.Block())
            per_eng = {e: [o for o in ops if o.eng == e] for e in self.ENGINES}
            final_counts = list(dcum)

            def make(ename):
                def body(eng):
                    waited = {}
                    for o in per_eng[ename]:
                        need = {}
                        for d in o.deps:
                            p = ops[d]
                            if p.is_dma:
                                key = ('d', p.dsem)
                                v = p.dcount
                            else:
                                if p.eng == ename and not o.is_dma:
                                    if ename == 'tensor' or not ses:
                                        continue
                                if not p.signal:
                                    continue
                                key = ('e', p.eng)
                                v = p.count
                            if need.get(key, 0) < v:
                                need[key] = v
                        if o.is_dma and o.dprev > 0:
                            key = ('d', o.dsem)
                            if need.get(key, 0) < o.dprev:
                                need[key] = o.dprev
                        for key, v in need.items():
                            if waited.get(key, 0) >= v:
                                continue
                            waited[key] = v
                            sem = dsems[key[1]] if key[0] == 'd' else esem[key[1]]
                            eng.wait_ge(sem, v)
                        ins = o.fn(eng)
                        if o.is_dma:
                            ins.then_inc(dsems[o.dsem], 16)
                        elif o.signal:
                            ins.then_inc(esem[ename], 1)
                    if ename == 'sync':
                        for i, c in enumerate(final_counts):
                            if c > 0 and waited.get(('d', i), 0) < c:
                                eng.wait_ge(dsems[i], c)
                return body

            block.sync(make('sync'))
            block.scalar(make('scalar'))
            block.vector(make('vector'))
            block.gpsimd(make('gpsimd'))
            block.tensor(make('tensor'))


def pipeline(n, stages):
    for it in range(n + len(stages) - 1):
        for k, f in enumerate(stages):
            t = it - k
            if 0 <= t < n:
                f(t)


class Arena:
    def __init__(self, A, nwords):
        self.A = A
        self.n = nwords
        self.top = 0

    def f32(self, n):
        off = self.top
        self.top += n
        assert self.top <= self.n, ("arena overflow", self.top, self.n)
        return self.A[:, off:off + n]

    def bf16(self, n):
        w = (n + 1) // 2
        off = self.top
        self.top += w
        assert self.top <= self.n, ("arena overflow", self.top, self.n)
        return self.A[:, off:off + w].bitcast(BF16)

    def sub(self, start, n):
        a = Arena(self.A, start + n)
        a.top = start
        return a

    def mark(self):
        return self.top

    def release(self, m):
        self.top = m


def build_nc(phases=6, debug=False, stop6=99, subB=99):
    nc = bass.Bass("TRN2", target_bir_lowering=False)

    def din(name, shape, dt=F32):
        return nc.dram_tensor(name, list(shape), dt, kind="ExternalInput").ap()

    def dscr(name, shape, dt=F32):
        return nc.dram_tensor(name, list(shape), dt, kind="Internal").ap()

    x = din("x", [S, D])
    w_in = din("w_in", [D, 2560])
    perm_d = din("perm", [128, 128])
    g1rep_d = din("g1rep", [128, D])
    g2rep_d = din("g2rep", [128, D])
    fgrep_d = din("fgrep", [128, D])
    ropec_d = din("ropec", [128, S])
    ropes_d = din("ropes", [128, S])
    lam4_d = din("lam4", [128, 4, 64])
    subln_d = din("subln", [128, 1])
    convw_d = din("convw", [128, 4, 4])
    convb_d = din("convb", [128, 4])
    wbd_d = din("wbd", [128, 16, 128])
    lrub_d = din("lrub", [128, 16])
    lrul_d = din("lrul", [128, 8])
    w_out = din("w_out", [D, D])
    w_rt = din("w_rt", [D, 36])
    w_gate = din("w_gate", [NE, D, 256])
    w_up = din("w_up", [NE, D, 256])
    w_down = din("w_down", [NE, 256, D])
    ident_d = din("ident", [128, 128])
    out = nc.dram_tensor("out", [S, D], F32, kind="ExternalOutput").ap()

    hT_d = dscr("hT_d", [8, 128, S], BF16)
    rnn_d = dscr("rnn_d", [4, 128, S], BF16)
    x1_d = dscr("x1_d", [S, D])
    h2_d = dscr("h2_d", [S, D], BF16)
    xs_d = dscr("xs_d", [64 * 256, D], BF16)
    ys_d = dscr("ys_d", [64 * 256, D], BF16)
    wall_d = dscr("wall_d", [NE * 128, 6144], BF16)
    pidx_d = din("pidx", [128, 1])
    umat_d = din("umat", [128, 128])
    thr_d = din("thr", [128, 64])

    dbg = {}
    if debug:
        dbg['attnT'] = nc.dram_tensor("dbg_attnT", [128, 4 * S], F32, kind="ExternalOutput").ap()
        dbg['rnnT'] = nc.dram_tensor("dbg_rnnT", [128, 4 * S], F32, kind="ExternalOutput").ap()
        dbg['qT'] = nc.dram_tensor("dbg_qT", [128, 4 * S], F32, kind="ExternalOutput").ap()
        dbg['kT'] = nc.dram_tensor("dbg_kT", [128, 4 * S], F32, kind="ExternalOutput").ap()
        dbg['v'] = nc.dram_tensor("dbg_v", [128, 32 * 512], F32, kind="ExternalOutput").ap()
        dbg['x1'] = nc.dram_tensor("dbg_x1", [S, D], F32, kind="ExternalOutput").ap()
        dbg['route'] = nc.dram_tensor("dbg_route", [128, 192], F32, kind="ExternalOutput").ap()
        dbg['lg'] = nc.dram_tensor("dbg_lg", [128, 32 * 36], F32, kind="ExternalOutput").ap()

    NW = 53000
    with ExitStack() as es:
        A = es.enter_context(nc.sbuf_tensor("A", [128, NW], F32))
        PS = es.enter_context(nc.psum_tensor("PS", [128, 8, 512], F32))
        P = Prog(nc)
        ar = Arena(A, NW)

        def bank(i):
            return PS[:, i, :]

        def bank_bf(i):
            return PS[:, i, :].bitcast(BF16)

        ident = ar.bf16(128)
        ones = ar.bf16(128)
        g1rep = ar.f32(D)
        epsT = ar.f32(1)
        oneT = ar.f32(1)
        small = ar.f32(64)
        cst = ar.f32(8)
        lrub = ar.f32(16)
        convw = ar.f32(16)
        convb = ar.f32(4)
        subg = ar.f32(1)
        neglam = ar.f32(1)
        base = ar.mark()

        m = ar.mark()
        tmpI = ar.f32(128)
        P.dma(tmpI, ident_d)
        P.copy(ident, tmpI)
        P.memset(ones, 1.0)
        P.memset(epsT, EPS)
        P.memset(oneT, 1.0)
        P.dma(g1rep, g1rep_d)
        P.dma(lrub, lrub_d)
        P.dma(convw, convw_d.rearrange("p a b -> p (a b)"))
        P.dma(convb, convb_d)
        P.dma(subg, subln_d)
        lrul = ar.f32(8)
        P.dma(lrul, lrul_d)
        P.act(lrul, lrul, AF.Exp, scale=-1.0)
        P.ts(lrul, lrul, 1.0, None, ALU.add)
        P.act(lrul, lrul, AF.Ln)
        P.ts(cst, lrul, -8.0, None, ALU.mult)
        lam4 = ar.f32(256)
        P.dma(lam4, lam4_d.rearrange("p a b -> p (a b)"))
        pr = ar.f32(128)
        P.tt(pr[:, 0:64], lam4[:, 0:64], lam4[:, 64:128], ALU.mult)
        P.tt(pr[:, 64:128], lam4[:, 128:192], lam4[:, 192:256], ALU.mult)
        P.op('vector', lambda e: e.reduce_sum(small[:, 0:1], pr[:, 0:64], AX.X), reads=[pr[:, 0:64]], writes=[small[:, 0:1]])
        P.op('vector', lambda e: e.reduce_sum(small[:, 1:2], pr[:, 64:128], AX.X), reads=[pr[:, 64:128]], writes=[small[:, 1:2]])
        P.act(small[:, 0:2], small[:, 0:2], AF.Exp)
        P.tt(small[:, 2:3], small[:, 1:2], small[:, 0:1], ALU.subtract)
        P.ts(neglam, small[:, 2:3], -0.2, None, ALU.add)
        P.ts(subg, subg, 0.8, None, ALU.mult)
        ar.release(m)

        m = ar.mark()
        xt = [ar.f32(D) for _ in range(4)]
        junk = ar.f32(D)
        hb = [ar.bf16(D) for _ in range(2)]
        hTc = [ar.bf16(8 * 512) for _ in range(2)]
        ss = ar.f32(NT)
        rs = ar.f32(NT)
        def p1_s1(t):
            xb = xt[t % 4]
            P.dma(xb, x[t * 128:(t + 1) * 128, :])
            P.act(junk, xb, AF.Square, accum_out=ss[:, t:t + 1])
            P.act(rs[:, t:t + 1], ss[:, t:t + 1], AF.Ln, scale=1.0 / D, bias=epsT[:, 0:1])
            P.act(rs[:, t:t + 1], rs[:, t:t + 1], AF.Exp, scale=-0.5)
            P.stt(hb[t % 2], xb, rs[:, t:t + 1], g1rep, ALU.mult, ALU.mult)

        def p1_s2(t):
            ch, tt = divmod(t, 4)
            pb = bank_bf(t % 2)
            for fc in range(8):
                P.transpose(pb[:, fc * 128:(fc + 1) * 128], hb[t % 2][:, fc * 128:(fc + 1) * 128], ident)
            dst = hTc[ch % 2].rearrange("p (f t) -> p f t", f=8)[:, :, tt * 128:(tt + 1) * 128]
            src = pb.rearrange("p (f t) -> p f t", f=8)
            if t % 2 == 0:
                P.copy(dst, src, eng='vector')
            else:
                P.op('scalar', lambda e, dst=dst, src=src: e.copy(dst, src), reads=[src], writes=[dst])
            if tt == 3:
                P.dma(hT_d[:, :, ch * 512:(ch + 1) * 512].rearrange("f p t -> p f t"),
                      hTc[ch % 2].rearrange("p (f t) -> p f t", f=8), eng='gpsimd')

        pipeline(NT, [p1_s1, p1_s2])
        ar.release(m)

        if phases >= 2:
            m = ar.mark()
            wbd = ar.bf16(16 * 128)
            wbdv = wbd.rearrange("p (a n) -> p a n", a=16)
            XR1 = ar.f32(S + 4)
            G1 = ar.f32(S)
            XC1 = ar.f32(S)
            xcb1 = ar.bf16(S)
            IB = ar.f32(S)
            AB = ar.f32(S)
            HB = ar.f32(S)
            wl = ar.bf16(8 * 1024)
            wlv = wl.rearrange("p (f n) -> p f n", f=8)
            m_stg = ar.mark()
            stg = [ar.f32(1024) for _ in range(2)]
            for fc in range(8):
                P.dma(stg[fc % 2], w_in[fc * 128:(fc + 1) * 128, 1536:2560])
                (P.copy if fc % 2 == 0 else P.acopy)(wlv[:, fc, :], stg[fc % 2])
            for a in range(4):
                st = stg[a % 2][:, 0:512]
                P.dma(st.rearrange("p (a n) -> p a n", a=4), wbd_d[:, a * 4:(a + 1) * 4, :])
                (P.copy if a % 2 == 0 else P.acopy)(wbd[:, a * 512:(a + 1) * 512], st)
            ar.release(m_stg)
            hc = [ar.bf16(8 * 512) for _ in range(2)]
            XR0 = ar.f32(S + 4)
            G0 = ar.f32(S)
            XC0 = ar.f32(S)
            xcb0 = ar.bf16(S)
            XRb = [XR0, XR1]
            Gb = [G0, G1]
            XCb = [XC0, XC1]
            xcbb = [xcb0, xcb1]
            arW = ar.sub(m + 32768, 6144 + 3072)
            wa_early = arW.bf16(8 * 1536)
            stg_early = [arW.f32(1536) for _ in range(2)]

            def wa_prep_steps():
                wav_ = wa_early.rearrange("p (f n) -> p f n", f=8)
                steps = []
                for fc in range(8):
                    def st_(fc=fc):
                        P.dma(stg_early[fc % 2], w_in[fc * 128:(fc + 1) * 128, 0:1536])
                        (P.copy if fc % 2 == 0 else P.acopy)(wav_[:, fc, 0:1536], stg_early[fc % 2])
                    steps.append(st_)
                return steps
            QN = 2
            QS = S // QN
            hcn = [0]

            def project_steps(ct):
                XR = XRb[ct % 2]
                G = Gb[ct % 2]
                steps = []
                for ch in range(NCH):
                    def st(ch=ch):
                        if ch == 0:
                            P.memset(XR[:, 0:2], 0.0)
                            P.memset(XR[:, S + 2:S + 4], 0.0)
                        hcb = hc[hcn[0] % 2]
                        hcn[0] += 1
                        hcv = hcb.rearrange("p (f t) -> p f t", f=8)
                        P.dma(hcv, hT_d[:, :, ch * 512:(ch + 1) * 512].rearrange("f p t -> p f t"))
                        pa = bank(2 * (ch % 2))
                        pg = bank(2 * (ch % 2) + 1)
                        for fc in range(8):
                            P.mm(pa, wlv[:, fc, ct * 128:(ct + 1) * 128], hcv[:, fc, :], start=(fc == 0), stop=(fc == 7))
                        for fc in range(8):
                            P.mm(pg, wlv[:, fc, 512 + ct * 128:512 + (ct + 1) * 128], hcv[:, fc, :], start=(fc == 0), stop=(fc == 7))
                        o_ = XR[:, 2 + ch * 512:2 + (ch + 1) * 512]
                        P.op('scalar', lambda e, o=o_, i=pa: e.copy(o, i), reads=[pa], writes=[o_])
                        P.copy(G[:, ch * 512:(ch + 1) * 512], pg, eng='vector')
                    steps.append(st)
                return steps

            def conv_steps(ct):
                XR = XRb[ct % 2]
                XC = XCb[ct % 2]
                xcb = xcbb[ct % 2]
                cw = lambda j: convw[:, ct * 4 + j:ct * 4 + j + 1]
                steps = []
                for q in range(QN):
                    def st(q=q):
                        sl = slice(q * QS, (q + 1) * QS)
                        P.act(XC[:, sl], XR[:, q * QS:q * QS + QS], AF.Identity, scale=cw(0), bias=convb[:, ct:ct + 1])
                        for j in range(1, 4):
                            P.stt(XC[:, sl], XR[:, j + q * QS:j + q * QS + QS], cw(j), XC[:, sl], ALU.mult, ALU.add)
                        P.op('scalar', lambda e, o=xcb[:, sl], i_=XC[:, sl]: e.copy(o, i_), reads=[XC[:, sl]], writes=[xcb[:, sl]])
                    steps.append(st)
                return steps

            def y_steps(ct):
                R = XRb[ct % 2][:, 0:S]
                XC = XCb[ct % 2]
                xcb = xcbb[ct % 2]
                G = Gb[ct % 2]
                rnnb = xcbb[ct % 2]
                steps = []
                for dr in range(2):
                    ir = (0 * 2 + dr) * 4 + ct
                    ii = (1 * 2 + dr) * 4 + ct
                    ci = dr * 4 + ct
                    chs = list(range(NCH)) if dr == 0 else list(range(NCH - 1, -1, -1))
                    for ch in chs:
                        def sg(ch=ch, ir=ir, ii=ii):
                            pr_ = bank(4 + 2 * (ch % 2))
                            pi_ = bank(4 + 2 * (ch % 2) + 1)
                            cs = slice(ch * 512, (ch + 1) * 512)
                            P.mm(pr_, wbdv[:, ir, :], xcb[:, cs])
                            P.mm(pi_, wbdv[:, ii, :], xcb[:, cs])
                            P.act(R[:, cs], pr_, AF.Sigmoid, bias=lrub[:, ir:ir + 1])
                            P.act(IB[:, cs], pi_, AF.Sigmoid, bias=lrub[:, ii:ii + 1])
                        steps.append(sg)
                    qorder = list(range(QN)) if dr == 0 else list(range(QN - 1, -1, -1))
                    for q in qorder:
                        def s1(q=q, ci=ci):
                            sl = slice(q * QS, (q + 1) * QS)
                            P.tt(IB[:, sl], IB[:, sl], XC[:, sl], ALU.mult)
                            P.act(AB[:, sl], R[:, sl], AF.Exp, scale=cst[:, ci:ci + 1])
                        steps.append(s1)
                    for q in qorder:
                        def s2(q=q):
                            sl = slice(q * QS, (q + 1) * QS)
                            P.act(R[:, sl], AB[:, sl], AF.Square)
                        steps.append(s2)
                    for q in qorder:
                        def s3(q=q, dr=dr):
                            sl = slice(q * QS, (q + 1) * QS)
                            P.act(R[:, sl], R[:, sl], AF.Sqrt, scale=-1.0, bias=oneT[:, 0:1])
                            P.tt(IB[:, sl], IB[:, sl], R[:, sl], ALU.mult)
                            if dr == 0:
                                init = 0.0 if q == 0 else HB[:, q * QS - 1:q * QS]
                                rd = [AB[:, sl], IB[:, sl]] + ([] if q == 0 else [init])
                                P.op('vector', lambda e, sl=sl, init=init: e.tensor_tensor_scan(HB[:, sl], AB[:, sl], IB[:, sl], init, ALU.mult, ALU.add),
                                     reads=rd, writes=[HB[:, sl]])
                            else:
                                init = 0.0 if q == QN - 1 else R[:, (q + 1) * QS:(q + 1) * QS + 1]
                                rd = [AB[:, sl], IB[:, sl]] + ([] if q == QN - 1 else [init])
                                rs_ = slice((q + 1) * QS - 1, q * QS - 1 if q > 0 else None, -1)
                                P.op('vector', lambda e, rs_=rs_, init=init, R=R: e.tensor_tensor_scan(R[:, rs_], AB[:, rs_], IB[:, rs_], init, ALU.mult, ALU.add),
                                     reads=rd, writes=[R[:, sl]])
                                P.tt(HB[:, sl], HB[:, sl], R[:, sl], ALU.add)
                        steps.append(s3)
                for q in range(QN):
                    def g1(q=q):
                        sl = slice(q * QS, (q + 1) * QS)
                        P.act(R[:, sl], G[:, sl], AF.Square)
                        P.ts(R[:, sl], R[:, sl], 0.044715, 1.0, ALU.mult, ALU.add)
                        P.tt(R[:, sl], R[:, sl], G[:, sl], ALU.mult)
                    steps.append(g1)
                for q in range(QN):
                    def g2(q=q):
                        sl = slice(q * QS, (q + 1) * QS)
                        P.act(R[:, sl], R[:, sl], AF.Sigmoid, scale=1.5957691216057308)
                        P.tt(R[:, sl], R[:, sl], G[:, sl], ALU.mult)
                        P.tt(rnnb[:, sl], HB[:, sl], R[:, sl], ALU.mult)
                    steps.append(g2)

                def fin():
                    P.dma(rnn_d[ct], rnnb)
                    if debug:
                        P.copy(AB, rnnb)
                        P.dma(dbg['rnnT'][:, ct * S:(ct + 1) * S], AB)
                steps.append(fin)
                return steps

            for f_ in project_steps(0) + conv_steps(0):
                f_()
            for ct in range(4):
                ys = y_steps(ct)
                xs_ = (project_steps(ct + 1) + conv_steps(ct + 1)) if ct + 1 < 4 else []
                pos = {}
                if ct == 3 and phases >= 3:
                    for i_, f_ in enumerate(wa_prep_steps()):
                        pos.setdefault(6 + 3 * i_, []).append(f_)
                npj = NCH if xs_ else 0
                ppos_ = [1, 3, 6, 9, 11, 14, 17, 19]
                for i_ in range(npj):
                    pos.setdefault(ppos_[i_], []).append(xs_[i_])
                for i_, f_ in enumerate(xs_[npj:]):
                    pos.setdefault(23 + 4 * i_, []).append(f_)
                for i_, f_ in enumerate(ys):
                    f_()
                    for g_ in pos.get(i_, []):
                        g_()
            ar.release(m)

        if phases >= 3:
            attn_start = ar.mark()
            attnT = ar.bf16(4 * S)
            attnTv = attnT.rearrange("p (h t) -> p h t", h=4)
            m_attn = ar.mark()
            ar3 = ar.sub(attn_start, m_attn - attn_start)
            QT = ar.bf16(4 * S)
            KT = ar.bf16(4 * S)
            V = ar.bf16(32 * 512)
            QTv = QT.rearrange("p (h t) -> p h t", h=4)
            KTv = KT.rearrange("p (h t) -> p h t", h=4)
            Vv = V.rearrange("p (k e) -> p k e", k=32)
            m3 = ar.mark()
            assert m3 == attn_start + 32768, (m3, attn_start)
            wa = ar.bf16(8 * 1536)
            wav = wa.rearrange("p (f n) -> p f n", f=8)
            stg = [ar.f32(1536) for _ in range(2)]
            permb = ar.bf16(128)
            P.dma(stg[0][:, 0:128], perm_d)
            P.copy(permb, stg[0][:, 0:128])
            hc = [ar3.bf16(8 * 512) for _ in range(2)]
            rc = [ar3.f32(512) for _ in range(2)]
            rsn = [ar3.f32(512) for _ in range(2)]
            t1 = [ar3.f32(512) for _ in range(2)]
            t2 = [ar3.f32(512) for _ in range(2)]
            qb = [ar.bf16(512) for _ in range(2)]
            k = 0
            for ch in range(NCH):
                hcb = hc[ch % 2]
                hcv = hcb.rearrange("p (f t) -> p f t", f=8)
                P.dma(hcv, hT_d[:, :, ch * 512:(ch + 1) * 512].rearrange("f p t -> p f t"))
                P.dma(rc[ch % 2], ropec_d[:, ch * 512:(ch + 1) * 512])
                P.dma(rsn[ch % 2], ropes_d[:, ch * 512:(ch + 1) * 512])
                sl = slice(ch * 512, (ch + 1) * 512)
                for qk in range(2):
                    dstv = QTv if qk == 0 else KTv
                    for h in range(4):
                        pq = bank(2 * (k % 2))
                        pqs = bank(2 * (k % 2) + 1)
                        c0 = qk * 512 + h * 128
                        for fc in range(8):
                            P.mm(pq, wav[:, fc, c0:c0 + 128], hcv[:, fc, :], start=(fc == 0), stop=(fc == 7))
                        P.acopy(qb[k % 2], pq)
                        P.mm(pqs, permb, qb[k % 2])
                        P.op('vector', lambda e, o=t1[k % 2], a=pq, b_=rc[ch % 2]: e.tensor_tensor(o, a, b_, ALU.mult),
                             reads=[pq, rc[ch % 2], qb[k % 2]], writes=[t1[k % 2]])
                        P.tt(t2[k % 2], pqs, rsn[ch % 2], ALU.mult)
                        P.tt(dstv[:, h, sl], t1[k % 2], t2[k % 2], ALU.add)
                        k += 1
                for tt in range(4):
                    pv = bank(4 + (tt % 2))
                    for fc in range(8):
                        P.mm(pv, hcv[:, fc, tt * 128:(tt + 1) * 128], wav[:, fc, 1024:1536], start=(fc == 0), stop=(fc == 7))
                    P.op('scalar', lambda e, o=Vv[:, ch * 4 + tt, :], i=pv: e.copy(o, i), reads=[pv], writes=[Vv[:, ch * 4 + tt, :]])
            ar.release(m3)
            if debug:
                m = ar.mark()
                tmpf = ar.f32(4 * S)
                P.copy(tmpf, QT)
                P.dma(dbg['qT'], tmpf)
                P.copy(tmpf, KT)
                P.dma(dbg['kT'], tmpf)
                P.copy(tmpf, V)
                P.dma(dbg['v'], tmpf)
                ar.release(m)

        if phases >= 4:
            m4 = ar.mark()
            E = [ar.bf16(1024) for _ in range(3)]
            rz1 = ar.f32(512)
            rz2 = ar.f32(512)
            ob = ar.f32(512)
            tb = ar.f32(512)
            sqb = ar.bf16(512)
            msb = ar.f32(512)
            SC = 0.125
            blk = 0
            cst_f = [ar.f32(2048) for _ in range(3)]
            cst_b = [ar.bf16(2048) for _ in range(3)]
            zacc2 = [ar.f32(512) for _ in range(2)]
            o1s = [ar.f32(512) for _ in range(2)]
            o2s = [ar.f32(512) for _ in range(2)]
            z1s = [ar.f32(512) for _ in range(2)]
            zh = ar.bf16(512)
            zl = ar.bf16(512)

            def convert_steps(e_):
                srcs = (w_gate[e_].rearrange("(f p) n -> p f n", p=128), w_up[e_].rearrange("(f p) n -> p f n", p=128),
                        w_down[e_].rearrange("(j p) n -> p j n", p=128))
                steps = []
                for q_ in range(3):
                    a_ = 8 if q_ < 2 else 2

                    def st(q_=q_, a_=a_):
                        P.dma(cst_f[q_].rearrange("p (a n) -> p a n", a=a_), srcs[q_])
                        P.copy(cst_b[q_], cst_f[q_])
                        P.dma(wall_d[e_ * 128:(e_ + 1) * 128, q_ * 2048:(q_ + 1) * 2048], cst_b[q_])
                    steps.append(st)
                return steps

            def epilogue_steps(h, qc, pb_):
                qs = slice(qc * 512, (qc + 1) * 512)
                B7 = bank(7)
                za = zacc2[pb_]

                def e1():
                    P.copy(zh, za)

                def e2():
                    P.tt(zl, za, zh, ALU.subtract)

                def e3():
                    P.mm(B7, ones, zh, start=True, stop=False)
                    P.mm(B7, ones, zl, start=False, stop=True)

                def e4():
                    P.act(rz2, B7, AF.Ln)
                    P.act(rz2, rz2, AF.Exp, scale=-1.0)

                def e5():
                    P.act(rz1, z1s[pb_], AF.Ln)
                    P.act(rz1, rz1, AF.Exp, scale=-1.0)

                def e6():
                    P.tt(ob, o1s[pb_], rz1, ALU.mult)
                    P.tt(tb, o2s[pb_], rz2, ALU.mult)

                def e7():
                    P.stt(ob, tb, neglam[:, 0:1], ob, ALU.mult, ALU.add)

                def e8():
                    P.tt(sqb, ob, ob, ALU.mult)

                def e9():
                    P.mm(B7, ones, sqb)

                def e10():
                    P.act(msb, B7, AF.Ln, scale=1.0 / 128, bias=epsT[:, 0:1])
                    P.act(msb, msb, AF.Exp, scale=-0.5)

                def e11():
                    P.stt(attnTv[:, h, qs], ob, subg[:, 0:1], msb, ALU.mult, ALU.mult)
                return [e1, e2, e3, e4, e5, e6, e7, e8, e9, e10, e11]

            pending = []
            for h in range(4):
                for qc in range(NCH):
                    qs = slice(qc * 512, (qc + 1) * 512)
                    O1, O2, Z1 = bank(4), bank(5), bank(6)
                    pb_ = blk % 2
                    sched = {}
                    for i_, f_ in enumerate(pending):
                        sched.setdefault(1 + i_, []).append(f_)
                    for i_, f_ in enumerate(convert_steps(blk)):
                        sched.setdefault(14 + 6 * i_, []).append(f_)
                    pending = []

                    def qk(kt):
                        sb = 2 * (kt % 2)
                        ks = slice(kt * 128, (kt + 1) * 128)
                        P.mm(bank(sb), KTv[0:64, h, ks], QTv[0:64, h, qs], tile_position=(0, 0))
                        P.mm(bank(sb + 1), KTv[64:128, h, ks], QTv[64:128, h, qs], tile_position=(64, 0))

                    qk(0)
                    for kt in range(32):
                        if kt + 1 < 32:
                            qk(kt + 1)
                        sb = 2 * (kt % 2)
                        Eb = E[kt % 3]
                        P.act(Eb.rearrange("p (a n) -> p a n", a=2), PS[:, sb:sb + 2, :], AF.Exp, scale=SC)
                        st = (kt == 0)
                        sp = (kt == 31)
                        vs = Vv[:, kt, h * 128:(h + 1) * 128]
                        P.mm(O1, vs, Eb[:, 0:512], start=st, stop=sp)
                        P.mm(O2, vs, Eb[:, 512:1024], start=st, stop=sp)
                        P.mm(Z1, ones, Eb[:, 0:512], start=st, stop=sp)
                        if kt == 0:
                            P.copy(zacc2[pb_], Eb[:, 512:1024])
                        else:
                            P.tt(zacc2[pb_], zacc2[pb_], Eb[:, 512:1024], ALU.add)
                        for f_ in sched.get(kt, []):
                            f_()
                    P.copy(o1s[pb_], O1)
                    P.copy(o2s[pb_], O2)
                    P.copy(z1s[pb_], Z1)
                    pending = epilogue_steps(h, qc, pb_)
                    blk += 1
            for f_ in pending:
                f_()
            ar.release(m4)
            if debug:
                m = ar.mark()
                tmpf = ar.f32(4 * S)
                P.copy(tmpf, attnT)
                P.dma(dbg['attnT'], tmpf)
                ar.release(m)

        if phases >= 5:
            ar.release(m_attn)
            m5 = ar.mark()
            rnnT = ar.bf16(4 * S)
            rnnTv = rnnT.rearrange("p (c t) -> p c t", c=4)
            for ct in range(4):
                P.dma(rnnTv[:, ct, :], rnn_d[ct])
            wo = ar.bf16(8 * 1024)
            wov = wo.rearrange("p (f n) -> p f n", f=8)
            m_stg5 = ar.mark()
            stg = [ar.f32(1024) for _ in range(2)]
            for fc in range(8):
                P.dma(stg[fc % 2], w_out[fc * 128:(fc + 1) * 128, :])
                (P.copy if fc % 2 == 0 else P.acopy)(wov[:, fc, :], stg[fc % 2])
            ar.release(m_stg5)
            xt = [ar.f32(D) for _ in range(4)]
            x1t = [ar.f32(D) for _ in range(3)]

            def p5_stage(t):
                xb = xt[t % 4]
                P.dma(xb, x[t * 128:(t + 1) * 128, :])
                ts_ = slice(t * 128, (t + 1) * 128)
                for nch in range(2):
                    po = bank(6 + nch)
                    for fc in range(8):
                        lhs = attnTv[:, fc, ts_] if fc < 4 else rnnTv[:, fc - 4, ts_]
                        P.mm(po, lhs, wov[:, fc, nch * 512:(nch + 1) * 512], start=(fc == 0), stop=(fc == 7))
                    P.tt(x1t[t % 3][:, nch * 512:(nch + 1) * 512], po, xb[:, nch * 512:(nch + 1) * 512], ALU.add)
                P.dma(x1_d[t * 128:(t + 1) * 128, :], x1t[t % 3], eng='gpsimd')
                if debug:
                    P.dma(dbg['x1'][t * 128:(t + 1) * 128, :], x1t[t % 3])

            if phases < 6:
                for t in range(NT):
                    p5_stage(t)

        if phases >= 6:
            TS = 256
            NTL = 64
            NSLOT = NTL * TS
            BIG = 1.0e30
            fgrep = ar.f32(D)
            P.dma(fgrep, fgrep_d)
            slotA = ar.f32(32).bitcast(I32)
            slotB = ar.f32(32).bitcast(I32)
            wA = ar.f32(32)
            wB = ar.f32(32)
            EI = ar.f32(NTL).bitcast(I32)
            sm = ar.f32(64)
            mA = ar.mark()
            g2rep = ar.f32(D)
            P.dma(g2rep, g2rep_d)
            whi = ar.bf16(8 * 36)
            wlo = ar.bf16(8 * 36)
            whiv = whi.rearrange("p (f n) -> p f n", f=8)
            wlov = wlo.rearrange("p (f n) -> p f n", f=8)
            wrt = ar.f32(8 * 36)
            wrt2 = ar.f32(8 * 36)
            P.dma(wrt.rearrange("p (f n) -> p f n", f=8), w_rt.rearrange("(f p) n -> p f n", p=128))
            P.copy(whi, wrt)
            P.copy(wrt2, whi)
            P.tt(wlo, wrt, wrt2, ALU.subtract)
            umat = ar.bf16(128)
            tmpU = ar.f32(128)
            P.dma(tmpU, umat_d)
            P.copy(umat, tmpU)
            thr = ar.f32(NTL)
            P.dma(thr, thr_d)
            pidx = ar.f32(1)
            P.dma(pidx, pidx_d)
            hib = [ar.bf16(D) for _ in range(2)]
            LG = ar.f32(NT * 36)
            LGv = LG.rearrange("p (t n) -> p t n", t=NT)
            h2 = [ar.f32(D) for _ in range(2)]
            lo = [ar.bf16(D) for _ in range(2)]
            hiT = [ar.bf16(8 * 128) for _ in range(2)]
            loT = [ar.bf16(8 * 128) for _ in range(2)]
            junk = ar.f32(D)
            ssq = ar.f32(NT)
            rsq = ar.f32(NT)
            def a_s1(t):
                b = t % 2
                xin = x1t[t % 3]
                P.act(junk, xin, AF.Square, accum_out=ssq[:, t:t + 1])
                P.act(rsq[:, t:t + 1], ssq[:, t:t + 1], AF.Ln, scale=1.0 / D, bias=epsT[:, 0:1])
                P.act(rsq[:, t:t + 1], rsq[:, t:t + 1], AF.Exp, scale=-0.5)
                P.stt(h2[b], xin, rsq[:, t:t + 1], g2rep, ALU.mult, ALU.mult)
                hi = hib[b]
                P.op('scalar', lambda e, o=hi, i_=h2[b]: e.copy(o, i_), reads=[h2[b]], writes=[hi])
                P.tt(lo[b], h2[b], hi, ALU.subtract)
                P.dma(h2_d[t * 128:(t + 1) * 128, :], hi, eng='gpsimd')

            def a_s2(t):
                b = t % 2
                hi = hib[b]
                pb = bank_bf(2 * b)
                pl = bank_bf(2 * b + 1)
                for fc in range(8):
                    P.transpose(pb[:, fc * 128:(fc + 1) * 128], hi[:, fc * 128:(fc + 1) * 128], ident)
                for fc in range(8):
                    P.transpose(pl[:, fc * 128:(fc + 1) * 128], lo[b][:, fc * 128:(fc + 1) * 128], ident)
                P.copy(hiT[b], pb)
                P.op('scalar', lambda e, o=loT[b], i=pl: e.copy(o, i), reads=[pl], writes=[loT[b]])

            def a_s3(t):
                b = t % 2
                hv = hiT[b].rearrange("p (f t) -> p f t", f=8)
                lv = loT[b].rearrange("p (f t) -> p f t", f=8)
                plg = bank(4 + b)[:, 0:36]
                n = 0
                for fc in range(8):
                    for (lh, rh) in ((hv[:, fc, :], whiv[:, fc, :]), (hv[:, fc, :], wlov[:, fc, :]), (lv[:, fc, :], whiv[:, fc, :])):
                        P.mm(plg, lh, rh, start=(n == 0), stop=(n == 23))
                        n += 1
                P.copy(LGv[:, t, :], plg)

            pipeline(NT, [p5_stage, a_s1, a_s2, a_s3])
            if debug:
                P.dma(dbg['lg'], LG)
            GL = LGv[:, :, 0:4]
            EL = LGv[:, :, 4:36].rearrange("p t (g e) -> p t g e", g=4)
            T1 = ar.f32(NT * 32)
            T2 = ar.f32(NT * 32)
            MA = ar.f32(NT * 32)
            MS = ar.f32(NT * 32)
            ELM = ar.f32(NT * 32)
            v3 = lambda a: a.rearrange("p (t e) -> p t e", t=NT)
            v4 = lambda a: a.rearrange("p (t g e) -> p t g e", t=NT, g=4)
            g4 = ar.f32(NT * 4)
            g4b = ar.f32(NT * 4)
            g4v = g4.rearrange("p (t g) -> p t g", g=4)
            g4bv = g4b.rearrange("p (t g) -> p t g", g=4)
            gmax = ar.f32(NT)
            gtp = ar.f32(NT)
            v1 = ar.f32(NT)
            v2 = ar.f32(NT)
            d21 = ar.f32(NT)
            bc3 = lambda a, n: a.unsqueeze(2).to_broadcast([128, NT, n])
            red = lambda o, i, op: P.op('vector', lambda e: e.tensor_reduce(o, i, AX.X, op), reads=[i], writes=[o])
            red(gmax, GL, ALU.max)
            P.tt(g4v, GL, bc3(gmax, 4), ALU.subtract)
            P.act(g4b, g4, AF.Exp)
            red(gtp, g4bv, ALU.add)
            P.recip(gtp, gtp)
            P.tt(g4v, GL, bc3(gmax, 4), ALU.is_ge)
            P.ts(g4, g4, BIG, -BIG, ALU.mult, ALU.add)
            P.tt(v4(ELM), EL, g4v.unsqueeze(3).to_broadcast([128, NT, 4, 8]), ALU.add)
            red(v1, v3(ELM), ALU.max)
            P.tt(v3(MA), v3(ELM), bc3(v1, 32), ALU.is_ge)
            P.stt(T1, MA, -BIG, ELM, ALU.mult, ALU.add)
            red(v2, v3(T1), ALU.max)
            P.tt(v3(MS), v3(ELM), bc3(v2, 32), ALU.is_ge)
            P.tt(T2, MS, MA, ALU.subtract)
            P.tt(d21, v2, v1, ALU.subtract)
            P.act(d21, d21, AF.Exp)
            P.ts(T1[:, 0:NT], d21, 1.0, None, ALU.add)
            P.recip(T1[:, 0:NT], T1[:, 0:NT])
            P.tt(wA, T1[:, 0:NT], gtp, ALU.mult)
            P.tt(wB, wA, d21, ALU.mult)
            MSb = ar.bf16(NT * 32)
            P.copy(MSb, MS)
            MSbv = v3(MSb)
            ppos = PS[:, 0:2, :].rearrange("p a n -> p (a n)")
            pcs = PS[:, 2:4, :].rearrange("p a n -> p (a n)")
            for t in range(NT):
                P.mm(ppos[:, t * 32:(t + 1) * 32], umat, MSbv[:, t, :])
                P.mm(pcs[:, t * 32:(t + 1) * 32], ones, MSbv[:, t, :])
            CS = ar.f32(NT * 32)
            P.copy(CS, pcs)
            BASE = ar.f32((NT + 1) * 32)
            P.memset(BASE[:, 0:32], 0.0)
            for t in range(NT):
                P.tt(BASE[:, (t + 1) * 32:(t + 2) * 32], BASE[:, t * 32:(t + 1) * 32], CS[:, t * 32:(t + 1) * 32], ALU.add)
            ntot = BASE[:, NT * 32:(NT + 1) * 32]
            npad = ar.f32(32)
            P.ts(npad, ntot, 0.0, None, ALU.is_gt)
            for kk in range(1, S // TS):
                P.stt(npad, ntot, float(kk * TS), npad, ALU.is_gt, ALU.add)
            P.ts(npad, npad, float(TS), None, ALU.mult)
            onesf = ar.f32(32)
            P.memset(onesf, 1.0)
            endp = ar.f32(32)
            P.op('vector', lambda e: e.tensor_tensor_scan(endp, onesf, npad, 0.0, ALU.mult, ALU.add), reads=[onesf, npad], writes=[endp])
            startp = ar.f32(32)
            P.tt(startp, endp, npad, ALU.subtract)
            P.tt(T1, ppos, BASE[:, 0:NT * 32], ALU.add)
            P.tt(v3(T1), v3(T1), startp.unsqueeze(1).to_broadcast([128, NT, 32]), ALU.add)
            P.tt(ELM, T1, MA, ALU.mult)
            sf = ar.f32(NT)
            red(sf, v3(ELM), ALU.add)
            P.copy(slotA, sf)
            P.tt(ELM, T1, T2, ALU.mult)
            red(sf, v3(ELM), ALU.add)
            P.copy(slotB, sf)
            TE = ar.f32(NTL * 32)
            TEv = TE.rearrange("p (i e) -> p i e", i=NTL)
            P.tt(TEv, endp.unsqueeze(1).to_broadcast([128, NTL, 32]), thr.unsqueeze(2).to_broadcast([128, NTL, 32]), ALU.is_le)
            eif = ar.f32(NTL)
            red(eif, TEv, ALU.add)
            P.ts(eif, eif, 31.0, None, ALU.min)
            P.ts(eif, eif, 128.0, pidx[:, 0:1], ALU.mult, ALU.add)
            inval = ar.f32(NTL)
            P.ts(inval, thr, endp[:, 31:32], 1.0e6, ALU.is_ge, ALU.mult)
            P.tt(eif, eif, inval, ALU.add)
            P.copy(EI, eif)
            if debug:
                dtmp = ar.f32(192)
                P.copy(dtmp[:, 0:32], slotA)
                P.copy(dtmp[:, 32:64], slotB)
                P.copy(dtmp[:, 64:96], wA)
                P.copy(dtmp[:, 96:128], wB)
                P.copy(dtmp[:, 128:192], EI)
                P.dma(dbg['route'], dtmp)
            hst = [ar.bf16(D) for _ in range(3)]
            for t in range(NT if stop6 >= 2 else 0):
                hb_ = hst[t % 3]
                P.dma(hb_, h2_d[t * 128:(t + 1) * 128, :])
                for sl_ in (slotA, slotB):
                    P.op('gpsimd', lambda e, sl_=sl_, t=t, hb_=hb_: e.indirect_dma_start(
                        out=xs_d[:, :], out_offset=bass.IndirectOffsetOnAxis(ap=sl_[:, t:t + 1], axis=0),
                        in_=hb_, in_offset=None),
                        reads=[hb_, sl_[:, t:t + 1]], writes=[xs_d], is_dma=True, partial=True)
            ar.release(mA)
            mB = ar.mark()
            NWB = 3
            wB_ = [ar.bf16(6144) for _ in range(NWB)]
            xg = [ar.bf16(2 * D) for _ in range(3)]
            xgT = [ar.bf16(8 * TS) for _ in range(2)]
            sg = [ar.f32(TS) for _ in range(2)]
            hid = [ar.bf16(2 * TS) for _ in range(2)]
            ysb = [ar.bf16(2 * D) for _ in range(2)]

            def b_load(i):
                kw_ = dict(bounds_check=NE * 128 - 1, oob_is_err=False) if i >= 32 else {}
                wb = wB_[i % NWB]
                P.op('gpsimd', lambda e, wb=wb, i=i, kw_=kw_: e.indirect_dma_start(
                    out=wb, out_offset=None, in_=wall_d[:, :],
                    in_offset=bass.IndirectOffsetOnAxis(ap=EI[:, i:i + 1], axis=0), **kw_),
                    reads=[wall_d, EI[:, i:i + 1]], writes=[wb], is_dma=True)
                P.dma(xg[i % 3].rearrange("p (a n) -> p a n", a=2), xs_d[i * TS:(i + 1) * TS, :].rearrange("(a p) n -> p a n", p=128))

            def b_s1(i):
                xgv = xg[i % 3].rearrange("p (a n) -> p a n", a=2)
                xTv = xgT[i % 2].rearrange("p (f t) -> p f t", f=8)
                for a in range(2):
                    pt = bank_bf(a)
                    for fc in range(8):
                        P.transpose(pt[:, fc * 128:(fc + 1) * 128], xgv[:, a, fc * 128:(fc + 1) * 128], ident)
                    src = pt.rearrange("p (f t) -> p f t", f=8)
                    dst = xTv[:, :, a * 128:(a + 1) * 128]
                    if a == 0:
                        P.copy(dst, src)
                    else:
                        P.op('scalar', lambda e, o=dst, i_=src: e.copy(o, i_), reads=[src], writes=[dst])

            def b_s2(i):
                wb = wB_[i % NWB]
                xTv = xgT[i % 2].rearrange("p (f t) -> p f t", f=8)
                wgv = wb[:, 0:2048].rearrange("p (f n) -> p f n", f=8)
                wuv = wb[:, 2048:4096].rearrange("p (f n) -> p f n", f=8)
                hv = hid[i % 2].rearrange("p (j t) -> p j t", j=2)
                for jj in range(2):
                    pg = bank(2 + jj)[:, 0:TS]
                    pu = bank(4 + jj)[:, 0:TS]
                    for fc in range(8):
                        P.mm(pg, wgv[:, fc, jj * 128:(jj + 1) * 128], xTv[:, fc, :], start=(fc == 0), stop=(fc == 7))
                    for fc in range(8):
                        P.mm(pu, wuv[:, fc, jj * 128:(jj + 1) * 128], xTv[:, fc, :], start=(fc == 0), stop=(fc == 7))
                    P.act(sg[jj], pg, AF.Silu)
                    P.tt(hv[:, jj, :], sg[jj], pu, ALU.mult)

            def b_s3(i):
                wb = wB_[i % NWB]
                wdv = wb[:, 4096:6144].rearrange("p (j n) -> p j n", j=2)
                hv = hid[i % 2].rearrange("p (j t) -> p j t", j=2)
                yv = ysb[i % 2].rearrange("p (a n) -> p a n", a=2)
                k = 0
                for a in range(2):
                    for nch in range(2):
                        pd = bank(6 + (k % 2))
                        for jj in range(2):
                            P.mm(pd, hv[:, jj, a * 128:(a + 1) * 128], wdv[:, jj, nch * 512:(nch + 1) * 512], start=(jj == 0), stop=(jj == 1))
                        o_ = yv[:, a, nch * 512:(nch + 1) * 512]
                        if k % 2 == 0:
                            P.copy(o_, pd)
                        else:
                            P.op('scalar', lambda e, o=o_, i_=pd: e.copy(o, i_), reads=[pd], writes=[o_])
                        k += 1
                P.dma(ys_d[i * TS:(i + 1) * TS, :].rearrange("(a p) n -> p a n", p=128), yv)

            if stop6 >= 3:
                b_load(0)
                for it in range(NTL + 1):
                    if it + 1 < NTL:
                        b_load(it + 1)
                    if it >= 1:
                        b_s2(it - 1)
                    if it < NTL:
                        b_s1(it)
                    if it >= 1:
                        b_s3(it - 1)
            ar.release(mB)
            YA = [ar.bf16(D) for _ in range(3)]
            YB = [ar.bf16(D) for _ in range(3)]
            x1c = [ar.f32(D) for _ in range(3)]
            outb = [ar.f32(D) for _ in range(2)]
            junk2 = ar.f32(D)
            ss3 = ar.f32(NT)
            rs3 = ar.f32(NT)
            if stop6 < 4:
                zz = ar.f32(D)
                P.memset(zz, 0.0)
                P.dma(out[0:128, :], zz)

            def c_s1(t):
                b = t % 3
                P.dma(x1c[b], x1_d[t * 128:(t + 1) * 128, :])
                for (yy, sl_) in ((YA[b], slotA), (YB[b], slotB)):
                    P.op('gpsimd', lambda e, yy=yy, sl_=sl_, t=t: e.indirect_dma_start(
                        out=yy, out_offset=None, in_=ys_d[:, :],
                        in_offset=bass.IndirectOffsetOnAxis(ap=sl_[:, t:t + 1], axis=0)),
                        reads=[ys_d, sl_[:, t:t + 1]], writes=[yy], is_dma=True)

            def c_s2(t):
                b = t % 3
                P.stt(x1c[b], YA[b], wA[:, t:t + 1], x1c[b], ALU.mult, ALU.add)
                P.stt(x1c[b], YB[b], wB[:, t:t + 1], x1c[b], ALU.mult, ALU.add)
                P.act(junk2, x1c[b], AF.Square, accum_out=ss3[:, t:t + 1])
                P.act(rs3[:, t:t + 1], ss3[:, t:t + 1], AF.Ln, scale=1.0 / D, bias=epsT[:, 0:1])
                P.act(rs3[:, t:t + 1], rs3[:, t:t + 1], AF.Exp, scale=-0.5)

            def c_s3(t):
                b = t % 3
                P.stt(outb[t % 2], x1c[b], rs3[:, t:t + 1], fgrep, ALU.mult, ALU.mult)
                P.dma(out[t * 128:(t + 1) * 128, :], outb[t % 2])

            if stop6 >= 4:
                pipeline(NT, [c_s1, c_s2, c_s3])
        else:
            m = ar.mark()
            z = ar.f32(D)
            P.memset(z, 0.0)
            P.dma(out[0:128, :], z)
            ar.release(m)

        P.emit()
    return nc


def _rope_tables():
    pos = np.arange(S, dtype=np.float32)
    inv = (np.float32(500000.0) ** (-np.arange(0, 16, 2, dtype=np.float32) / np.float32(16))).astype(np.float32)
    ang = pos[:, None] * inv[None, :]
    cos = np.cos(ang).astype(np.float32)
    sin = np.sin(ang).astype(np.float32)
    C = np.ones((128, S), np.float32)
    Sn = np.zeros((128, S), np.float32)
    for mth in range(2):
        for d in range(16):
            p = mth * 64 + d
            C[p] = cos[:, d % 8]
            Sn[p] = -sin[:, d % 8] if d < 8 else sin[:, d % 8]
    return C, Sn


def _shared_inputs(norm1_g, w_in, lambda_q1, lambda_k1, lambda_q2, lambda_k2, subln_g, conv_w, conv_b,
                   lru_w_r, lru_b_r, lru_w_i, lru_b_i, lru_lambda, w_out, norm2_g, w_grp, w_exp,
                   w_gate, w_up, w_down, final_g):
    f = np.float32
    c = np.ascontiguousarray
    w_in0 = c(w_in[0], dtype=f)
    perm = np.arange(128)
    for sh in range(2):
        b0 = sh * 64
        perm[b0:b0 + 8] = np.arange(b0 + 8, b0 + 16)
        perm[b0 + 8:b0 + 16] = np.arange(b0, b0 + 8)
    permm = np.zeros((128, 128), f)
    permm[perm, np.arange(128)] = 1.0
    C, Sn = _rope_tables()
    lam4 = np.stack([lambda_q1[0], lambda_k1[0], lambda_q2[0], lambda_k2[0]], 0).astype(f)
    lam4 = c(np.broadcast_to(lam4[None], (128, 4, 64)))
    convw = c(conv_w[0].reshape(4, 4, 128).transpose(2, 1, 0), dtype=f)
    convb = c(conv_b[0].reshape(4, 128).T, dtype=f)
    wbd = np.zeros((128, 16, 128), f)
    lrub = np.zeros((128, 16), f)
    for gi, (wm, bm) in enumerate(((lru_w_r[0], lru_b_r[0]), (lru_w_i[0], lru_b_i[0]))):
        for dr in range(2):
            for ct in range(4):
                a = (gi * 2 + dr) * 4 + ct
                for bl in range(2):
                    wbd[bl * 64:(bl + 1) * 64, a, bl * 64:(bl + 1) * 64] = wm[dr, ct * 2 + bl]
                lrub[:, a] = bm[dr, ct * 128:(ct + 1) * 128]
    lrul = np.zeros((128, 8), f)
    for dr in range(2):
        for ct in range(4):
            lrul[:, dr * 4 + ct] = lru_lambda[0, dr, ct * 128:(ct + 1) * 128]
    rep = lambda v: c(np.broadcast_to(np.asarray(v, f).reshape(1, -1), (128, v.size)))
    return {
        "w_in": w_in0,
        "perm": permm,
        "g1rep": rep(norm1_g[0]),
        "g2rep": rep(norm2_g[0]),
        "fgrep": rep(final_g),
        "ropec": C,
        "ropes": Sn,
        "lam4": lam4,
        "subln": c(subln_g[0].reshape(128, 1), dtype=f),
        "convw": convw,
        "convb": convb,
        "wbd": wbd,
        "lrub": lrub,
        "lrul": lrul,
        "w_out": c(w_out[0], dtype=f),
        "w_rt": c(np.concatenate([w_grp[0], w_exp[0]], axis=1), dtype=f),
        "w_gate": c(w_gate[0], dtype=f),
        "w_up": c(w_up[0], dtype=f),
        "w_down": c(w_down[0], dtype=f),
        "ident": np.eye(128, dtype=f),
        "umat": np.triu(np.ones((128, 128), f), 1),
        "thr": c(np.broadcast_to((np.arange(64, dtype=f) * 256.0)[None, :], (128, 64))),
        "pidx": np.arange(128, dtype=f).reshape(128, 1),
    }


def kernel(x, **params):
    x = np.asarray(x, dtype=np.float32)
    params = {k: np.asarray(v, dtype=np.float32) for k, v in params.items()}
    shared = _shared_inputs(**params)
    nc = build_nc()
    in_maps = []
    for b in range(8):
        mp = dict(shared)
        mp["x"] = np.ascontiguousarray(x[b])
        in_maps.append(mp)
    res = run_bass_kernel_spmd(nc, in_maps, core_ids=list(range(8)))
    return np.stack([np.asarray(r["out"], dtype=np.float32) for r in res.results], axis=0)
```

```python
import math
from contextlib import ExitStack

import numpy as np
import concourse.bass as bass
import concourse.mybir as mybir
from concourse.bass_utils import run_bass_kernel_spmd

F32 = mybir.dt.float32
BF16 = mybir.dt.bfloat16
AF = mybir.ActivationFunctionType
ALU = mybir.AluOpType
AX = mybir.AxisListType

I32 = mybir.dt.int32
_DTSZ = {F32: 4, BF16: 2, I32: 4}

S = 4096
D = 1024
NT = 32
NCH = 8
NE = 32
EPS = 1e-6


def _rect(ap):
    t = ap.tensor
    sz = _DTSZ.get(ap.dtype, 4)
    dims = ap.ap
    lo = 0
    hi = 0
    space = str(ap.space).upper()
    if not ('SB' in space or 'PSUM' in space):
        for st, cnt in dims:
            if st >= 0:
                hi += st * (cnt - 1)
            else:
                lo += st * (cnt - 1)
        off = ap.offset
        return (t.name, 0, 1, (off + lo) * sz, (off + hi + 1) * sz)
    pstride = 1
    for s in list(t.shape)[1:]:
        pstride *= s
    off = ap.offset
    p0 = off // pstride
    f0 = off % pstride
    pcnt = dims[0][1]
    for st, cnt in dims[1:]:
        if st >= 0:
            hi += st * (cnt - 1)
        else:
            lo += st * (cnt - 1)
    return (t.name, p0, p0 + pcnt, (f0 + lo) * sz, (f0 + hi + 1) * sz)


def _overlap(a, b):
    return a[1] < b[2] and b[1] < a[2] and a[3] < b[4] and b[3] < a[4]


def _covers(a, b):
    return a[1] <= b[1] and a[2] >= b[2] and a[3] <= b[3] and a[4] >= b[4]


class Op:
    __slots__ = ('idx', 'eng', 'fn', 'deps', 'is_dma', 'signal', 'count', 'dsem', 'dcount', 'dprev')

    def __init__(self, idx, eng, fn, is_dma):
        self.idx = idx
        self.eng = eng
        self.fn = fn
        self.is_dma = is_dma
        self.deps = set()
        self.signal = False
        self.count = 0
        self.dsem = None
        self.dcount = 0
        self.dprev = 0


class Prog:
    ENGINES = ['sync', 'scalar', 'vector', 'gpsimd', 'tensor']

    def __init__(self, nc, n_dma_sems=32, same_engine_sync=True):
        self.nc = nc
        self.ops = []
        self.recs = {}
        self.n_dma_sems = n_dma_sems
        self.same_engine_sync = same_engine_sync

    def op(self, eng, fn, reads=(), writes=(), is_dma=False, partial=False):
        o = Op(len(self.ops), eng, fn, is_dma)
        self.ops.append(o)
        rrs = [_rect(ap) for ap in reads]
        wrs = [_rect(ap) for ap in writes]
        for r in rrs:
            for rec in self.recs.setdefault(r[0], []):
                if rec[2] and _overlap(rec[0], r):
                    o.deps.add(rec[1])
        for r in wrs:
            for rec in self.recs.setdefault(r[0], []):
                if _overlap(rec[0], r):
                    if partial and rec[2] and len(rec) > 4 and rec[4]:
                        continue
                    o.deps.add(rec[1])
        o.deps.discard(o.idx)
        for r in rrs:
            lst = self.recs[r[0]]
            done = False
            if not is_dma:
                for rec in lst:
                    if (not rec[2]) and rec[3] == eng and rec[0] == r and not self.ops[rec[1]].is_dma:
                        rec[1] = o.idx
                        done = True
                        break
            if not done:
                lst.append([r, o.idx, False, eng])
        for r in wrs:
            lst = self.recs[r[0]]
            if not partial:
                lst[:] = [rec for rec in lst if not (_covers(r, rec[0]) and rec[1] != o.idx)]
            lst.append([r, o.idx, True, eng, partial])
        return o

    def dma(self, out, in_, eng='sync'):
        return self.op(eng, lambda e: e.dma_start(out=out, in_=in_), reads=[in_], writes=[out], is_dma=True)

    def mm(self, out, lhsT, rhs, start=True, stop=True, **kw):
        rd = [lhsT, rhs] + ([] if start else [out])
        return self.op('tensor', lambda e: e.matmul(out, lhsT, rhs, start=start, stop=stop, **kw),
                       reads=rd, writes=[out])

    def transpose(self, out, in_, ident):
        return self.op('tensor', lambda e: e.transpose(out, in_, ident), reads=[in_, ident], writes=[out])

    def act(self, out, in_, func, bias=None, scale=None, accum_out=None):
        kw = {}
        rd = [in_]
        wr = [out]
        if bias is not None:
            kw['bias'] = bias
            if not isinstance(bias, (int, float)):
                rd.append(bias)
        if scale is not None:
            kw['scale'] = scale
            if not isinstance(scale, (int, float)):
                rd.append(scale)
        if accum_out is not None:
            kw['accum_out'] = accum_out
            wr.append(accum_out)
        return self.op('scalar', lambda e: e.activation(out, in_, func, **kw), reads=rd, writes=wr)

    def tt(self, out, in0, in1, op, eng='vector'):
        return self.op(eng, lambda e: e.tensor_tensor(out, in0, in1, op), reads=[in0, in1], writes=[out])

    def ts(self, out, in0, s1, s2, op0, op1=None, eng='vector'):
        rd = [in0]
        if not isinstance(s1, (int, float)):
            rd.append(s1)
        if s2 is not None and not isinstance(s2, (int, float)):
            rd.append(s2)
        kw = {}
        if op1 is not None:
            kw['op1'] = op1
        return self.op(eng, lambda e: e.tensor_scalar(out, in0, s1, s2, op0, **kw), reads=rd, writes=[out])

    def stt(self, out, in0, scalar, in1, op0, op1, eng='vector'):
        rd = [in0, in1]
        if not isinstance(scalar, (int, float)):
            rd.append(scalar)
        return self.op(eng, lambda e: e.scalar_tensor_tensor(out, in0, scalar, in1, op0, op1),
                       reads=rd, writes=[out])

    def copy(self, out, in_, eng='vector'):
        return self.op(eng, lambda e: e.tensor_copy(out, in_), reads=[in_], writes=[out])

    def memset(self, out, val, eng='vector'):
        return self.op(eng, lambda e: e.memset(out, val), reads=[], writes=[out])

    def acopy(self, out, in_):
        return self.op('scalar', lambda e: e.copy(out, in_), reads=[in_], writes=[out])

    def recip(self, out, in_):
        return self.op('vector', lambda e: e.reciprocal(out, in_), reads=[in_], writes=[out])

    def emit(self):
        nc = self.nc
        ops = self.ops
        ses = self.same_engine_sync
        for o in ops:
            for d in o.deps:
                p = ops[d]
                if p.is_dma:
                    continue
                if p.eng != o.eng or o.is_dma or (ses and p.eng != 'tensor'):
                    p.signal = True
        cnt = {e: 0 for e in self.ENGINES}
        for o in ops:
            if o.is_dma:
                continue
            if o.signal:
                cnt[o.eng] += 1
            o.count = cnt[o.eng]
        dcum = [0] * self.n_dma_sems
        k = 0
        for o in ops:
            if o.is_dma:
                s = k % self.n_dma_sems
                k += 1
                o.dsem = s
                o.dprev = dcum[s]
                dcum[s] += 16
                o.dcount = dcum[s]
        with ExitStack() as es:
            esem = {e: es.enter_context(nc.semaphore('s_' + e)) for e in self.ENGINES}
            dsems = [es.enter_context(nc.semaphore('d_%d' % i)) for i in range(self.n_dma_sems)]
            block = es.enter_context(nc.Block())
            per_eng = {e: [o for o in ops if o.eng == e] for e in self.ENGINES}
            final_counts = list(dcum)

            def make(ename):
                def body(eng):
                    waited = {}
                    for o in per_eng[ename]:
                        need = {}
                        for d in o.deps:
                            p = ops[d]
                            if p.is_dma:
                                key = ('d', p.dsem)
                                v = p.dcount
                            else:
                                if p.eng == ename and not o.is_dma:
                                    if ename == 'tensor' or not ses:
                                        continue
                                if not p.signal:
                                    continue
                                key = ('e', p.eng)
                                v = p.count
                            if need.get(key, 0) < v:
                                need[key] = v
                        if o.is_dma and o.dprev > 0:
                            key = ('d', o.dsem)
                            if need.get(key, 0) < o.dprev:
                                need[key] = o.dprev
                        for key, v in need.items():
                            if waited.get(key, 0) >= v:
                                continue
                            waited[key] = v
                            sem = dsems[key[1]] if key[0] == 'd' else esem[key[1]]
                            eng.wait_ge(sem, v)
                        ins = o.fn(eng)
                        if o.is_dma:
                            ins.then_inc(dsems[o.dsem], 16)
                        elif o.signal:
                            ins.then_inc(esem[ename], 1)
                    if ename == 'sync':
                        for i, c in enumerate(final_counts):
                            if c > 0 and waited.get(('d', i), 0) < c:
                                eng.wait_ge(dsems[i], c)
                return body

            block.sync(make('sync'))
            block.scalar(make('scalar'))
            block.vector(make('vector'))
            block.gpsimd(make('gpsimd'))
            block.tensor(make('tensor'))


def pipeline(n, stages):
    for it in range(n + len(stages) - 1):
        for k, f in enumerate(stages):
            t = it - k
            if 0 <= t < n:
                f(t)


class Arena:
    def __init__(self, A, nwords):
        self.A = A
        self.n = nwords
        self.top = 0

    def f32(self, n):
        off = self.top
        self.top += n
        assert self.top <= self.n, ("arena overflow", self.top, self.n)
        return self.A[:, off:off + n]

    def bf16(self, n):
        w = (n + 1) // 2
        off = self.top
        self.top += w
        assert self.top <= self.n, ("arena overflow", self.top, self.n)
        return self.A[:, off:off + w].bitcast(BF16)

    def sub(self, start, n):
        a = Arena(self.A, start + n)
        a.top = start
        return a

    def mark(self):
        return self.top

    def release(self, m):
        self.top = m


def build_nc(phases=6, debug=False, stop6=99, subB=99):
    nc = bass.Bass("TRN2", target_bir_lowering=False)

    def din(name, shape, dt=F32):
        return nc.dram_tensor(name, list(shape), dt, kind="ExternalInput").ap()

    def dscr(name, shape, dt=F32):
        return nc.dram_tensor(name, list(shape), dt, kind="Internal").ap()

    x = din("x", [S, D])
    w_in = din("w_in", [D, 2560])
    perm_d = din("perm", [128, 128])
    g1rep_d = din("g1rep", [128, D])
    g2rep_d = din("g2rep", [128, D])
    fgrep_d = din("fgrep", [128, D])
    ropec_d = din("ropec", [128, S])
    ropes_d = din("ropes", [128, S])
    lam4_d = din("lam4", [128, 4, 64])
    subln_d = din("subln", [128, 1])
    convw_d = din("convw", [128, 4, 4])
    convb_d = din("convb", [128, 4])
    wbd_d = din("wbd", [128, 16, 128])
    lrub_d = din("lrub", [128, 16])
    lrul_d = din("lrul", [128, 8])
    w_out = din("w_out", [D, D])
    w_rt = din("w_rt", [D, 36])
    w_gate = din("w_gate", [NE, D, 256])
    w_up = din("w_up", [NE, D, 256])
    w_down = din("w_down", [NE, 256, D])
    ident_d = din("ident", [128, 128])
    out = nc.dram_tensor("out", [S, D], F32, kind="ExternalOutput").ap()

    hT_d = dscr("hT_d", [8, 128, S], BF16)
    rnn_d = dscr("rnn_d", [4, 128, S], BF16)
    x1_d = dscr("x1_d", [S, D])
    h2_d = dscr("h2_d", [S, D], BF16)
    xs_d = dscr("xs_d", [64 * 256, D], BF16)
    ys_d = dscr("ys_d", [64 * 256, D], BF16)
    wall_d = dscr("wall_d", [NE * 128, 6144], BF16)
    pidx_d = din("pidx", [128, 1])
    umat_d = din("umat", [128, 128])
    thr_d = din("thr", [128, 64])

    dbg = {}
    if debug:
        dbg['attnT'] = nc.dram_tensor("dbg_attnT", [128, 4 * S], F32, kind="ExternalOutput").ap()
        dbg['rnnT'] = nc.dram_tensor("dbg_rnnT", [128, 4 * S], F32, kind="ExternalOutput").ap()
        dbg['qT'] = nc.dram_tensor("dbg_qT", [128, 4 * S], F32, kind="ExternalOutput").ap()
        dbg['kT'] = nc.dram_tensor("dbg_kT", [128, 4 * S], F32, kind="ExternalOutput").ap()
        dbg['v'] = nc.dram_tensor("dbg_v", [128, 32 * 512], F32, kind="ExternalOutput").ap()
        dbg['x1'] = nc.dram_tensor("dbg_x1", [S, D], F32, kind="ExternalOutput").ap()
        dbg['route'] = nc.dram_tensor("dbg_route", [128, 192], F32, kind="ExternalOutput").ap()
        dbg['lg'] = nc.dram_tensor("dbg_lg", [128, 32 * 36], F32, kind="ExternalOutput").ap()

    NW = 53000
    with ExitStack() as es:
        A = es.enter_context(nc.sbuf_tensor("A", [128, NW], F32))
        PS = es.enter_context(nc.psum_tensor("PS", [128, 8, 512], F32))
        P = Prog(nc)
        ar = Arena(A, NW)

        def bank(i):
            return PS[:, i, :]

        def bank_bf(i):
            return PS[:, i, :].bitcast(BF16)

        ident = ar.bf16(128)
        ones = ar.bf16(128)
        g1rep = ar.f32(D)
        epsT = ar.f32(1)
        oneT = ar.f32(1)
        small = ar.f32(64)
        cst = ar.f32(8)
        lrub = ar.f32(16)
        convw = ar.f32(16)
        convb = ar.f32(4)
        subg = ar.f32(1)
        neglam = ar.f32(1)
        base = ar.mark()

        m = ar.mark()
        tmpI = ar.f32(128)
        P.dma(tmpI, ident_d)
        P.copy(ident, tmpI)
        P.memset(ones, 1.0)
        P.memset(epsT, EPS)
        P.memset(oneT, 1.0)
        P.dma(g1rep, g1rep_d)
        P.dma(lrub, lrub_d)
        P.dma(convw, convw_d.rearrange("p a b -> p (a b)"))
        P.dma(convb, convb_d)
        P.dma(subg, subln_d)
        lrul = ar.f32(8)
        P.dma(lrul, lrul_d)
        P.act(lrul, lrul, AF.Exp, scale=-1.0)
        P.ts(lrul, lrul, 1.0, None, ALU.add)
        P.act(lrul, lrul, AF.Ln)
        P.ts(cst, lrul, -8.0, None, ALU.mult)
        lam4 = ar.f32(256)
        P.dma(lam4, lam4_d.rearrange("p a b -> p (a b)"))
        pr = ar.f32(128)
        P.tt(pr[:, 0:64], lam4[:, 0:64], lam4[:, 64:128], ALU.mult)
        P.tt(pr[:, 64:128], lam4[:, 128:192], lam4[:, 192:256], ALU.mult)
        P.op('vector', lambda e: e.reduce_sum(small[:, 0:1], pr[:, 0:64], AX.X), reads=[pr[:, 0:64]], writes=[small[:, 0:1]])
        P.op('vector', lambda e: e.reduce_sum(small[:, 1:2], pr[:, 64:128], AX.X), reads=[pr[:, 64:128]], writes=[small[:, 1:2]])
        P.act(small[:, 0:2], small[:, 0:2], AF.Exp)
        P.tt(small[:, 2:3], small[:, 1:2], small[:, 0:1], ALU.subtract)
        P.ts(neglam, small[:, 2:3], -0.2, None, ALU.add)
        P.ts(subg, subg, 0.8, None, ALU.mult)
        ar.release(m)

        m = ar.mark()
        xt = [ar.f32(D) for _ in range(4)]
        junk = ar.f32(D)
        hb = [ar.bf16(D) for _ in range(2)]
        hTc = [ar.bf16(8 * 512) for _ in range(2)]
        ss = ar.f32(NT)
        rs = ar.f32(NT)
        def p1_s1(t):
            xb = xt[t % 4]
            P.dma(xb, x[t * 128:(t + 1) * 128, :])
            P.act(junk, xb, AF.Square, accum_out=ss[:, t:t + 1])
            P.act(rs[:, t:t + 1], ss[:, t:t + 1], AF.Ln, scale=1.0 / D, bias=epsT[:, 0:1])
            P.act(rs[:, t:t + 1], rs[:, t:t + 1], AF.Exp, scale=-0.5)
            P.stt(hb[t % 2], xb, rs[:, t:t + 1], g1rep, ALU.mult, ALU.mult)

        def p1_s2(t):
            ch, tt = divmod(t, 4)
            pb = bank_bf(t % 2)
            for fc in range(8):
                P.transpose(pb[:, fc * 128:(fc + 1) * 128], hb[t % 2][:, fc * 128:(fc + 1) * 128], ident)
            dst = hTc[ch % 2].rearrange("p (f t) -> p f t", f=8)[:, :, tt * 128:(tt + 1) * 128]
            src = pb.rearrange("p (f t) -> p f t", f=8)
            if t % 2 == 0:
                P.copy(dst, src, eng='vector')
            else:
                P.op('scalar', lambda e, dst=dst, src=src: e.copy(dst, src), reads=[src], writes=[dst])
            if tt == 3:
                P.dma(hT_d[:, :, ch * 512:(ch + 1) * 512].rearrange("f p t -> p f t"),
                      hTc[ch % 2].rearrange("p (f t) -> p f t", f=8), eng='gpsimd')

        pipeline(NT, [p1_s1, p1_s2])
        ar.release(m)

        if phases >= 2:
            m = ar.mark()
            wbd = ar.bf16(16 * 128)
            wbdv = wbd.rearrange("p (a n) -> p a n", a=16)
            XR1 = ar.f32(S + 4)
            G1 = ar.f32(S)
            XC1 = ar.f32(S)
            xcb1 = ar.bf16(S)
            IB = ar.f32(S)
            AB = ar.f32(S)
            HB = ar.f32(S)
            wl = ar.bf16(8 * 1024)
            wlv = wl.rearrange("p (f n) -> p f n", f=8)
            m_stg = ar.mark()
            stg = [ar.f32(1024) for _ in range(2)]
            for fc in range(8):
                P.dma(stg[fc % 2], w_in[fc * 128:(fc + 1) * 128, 1536:2560])
                (P.copy if fc % 2 == 0 else P.acopy)(wlv[:, fc, :], stg[fc % 2])
            for a in range(4):
                st = stg[a % 2][:, 0:512]
                P.dma(st.rearrange("p (a n) -> p a n", a=4), wbd_d[:, a * 4:(a + 1) * 4, :])
                (P.copy if a % 2 == 0 else P.acopy)(wbd[:, a * 512:(a + 1) * 512], st)
            ar.release(m_stg)
            hc = [ar.bf16(8 * 512) for _ in range(2)]
            XR0 = ar.f32(S + 4)
            G0 = ar.f32(S)
            XC0 = ar.f32(S)
            xcb0 = ar.bf16(S)
            XRb = [XR0, XR1]
            Gb = [G0, G1]
            XCb = [XC0, XC1]
            xcbb = [xcb0, xcb1]
            arW = ar.sub(m + 32768, 6144 + 3072)
            wa_early = arW.bf16(8 * 1536)
            stg_early = [arW.f32(1536) for _ in range(2)]

            def wa_prep_steps():
                wav_ = wa_early.rearrange("p (f n) -> p f n", f=8)
                steps = []
                for fc in range(8):
                    def st_(fc=fc):
                        P.dma(stg_early[fc % 2], w_in[fc * 128:(fc + 1) * 128, 0:1536])
                        (P.copy if fc % 2 == 0 else P.acopy)(wav_[:, fc, 0:1536], stg_early[fc % 2])
                    steps.append(st_)
                return steps
            QN = 2
            QS = S // QN
            hcn = [0]

            def project_steps(ct):
                XR = XRb[ct % 2]
                G = Gb[ct % 2]
                steps = []
                for ch in range(NCH):
                    def st(ch=ch):
                        if ch == 0:
                            P.memset(XR[:, 0:2], 0.0)
                            P.memset(XR[:, S + 2:S + 4], 0.0)
                        hcb = hc[hcn[0] % 2]
                        hcn[0] += 1
                        hcv = hcb.rearrange("p (f t) -> p f t", f=8)
                        P.dma(hcv, hT_d[:, :, ch * 512:(ch + 1) * 512].rearrange("f p t -> p f t"))
                        pa = bank(2 * (ch % 2))
                        pg = bank(2 * (ch % 2) + 1)
                        for fc in range(8):
                            P.mm(pa, wlv[:, fc, ct * 128:(ct + 1) * 128], hcv[:, fc, :], start=(fc == 0), stop=(fc == 7))
                        for fc in range(8):
                            P.mm(pg, wlv[:, fc, 512 + ct * 128:512 + (ct + 1) * 128], hcv[:, fc, :], start=(fc == 0), stop=(fc == 7))
                        o_ = XR[:, 2 + ch * 512:2 + (ch + 1) * 512]
                        P.op('scalar', lambda e, o=o_, i=pa: e.copy(o, i), reads=[pa], writes=[o_])
                        P.copy(G[:, ch * 512:(ch + 1) * 512], pg, eng='vector')
                    steps.append(st)
                return steps

            def conv_steps(ct):
                XR = XRb[ct % 2]
                XC = XCb[ct % 2]
                xcb = xcbb[ct % 2]
                cw = lambda j: convw[:, ct * 4 + j:ct * 4 + j + 1]
                steps = []
                for q in range(QN):
                    def st(q=q):
                        sl = slice(q * QS, (q + 1) * QS)
                        P.act(XC[:, sl], XR[:, q * QS:q * QS + QS], AF.Identity, scale=cw(0), bias=convb[:, ct:ct + 1])
                        for j in range(1, 4):
                            P.stt(XC[:, sl], XR[:, j + q * QS:j + q * QS + QS], cw(j), XC[:, sl], ALU.mult, ALU.add)
                        P.op('scalar', lambda e, o=xcb[:, sl], i_=XC[:, sl]: e.copy(o, i_), reads=[XC[:, sl]], writes=[xcb[:, sl]])
                    steps.append(st)
                return steps

            def y_steps(ct):
                R = XRb[ct % 2][:, 0:S]
                XC = XCb[ct % 2]
                xcb = xcbb[ct % 2]
                G = Gb[ct % 2]
                rnnb = xcbb[ct % 2]
                steps = []
                for dr in range(2):
                    ir = (0 * 2 + dr) * 4 + ct
                    ii = (1 * 2 + dr) * 4 + ct
                    ci = dr * 4 + ct
                    chs = list(range(NCH)) if dr == 0 else list(range(NCH - 1, -1, -1))
                    for ch in chs:
                        def sg(ch=ch, ir=ir, ii=ii):
                            pr_ = bank(4 + 2 * (ch % 2))
                            pi_ = bank(4 + 2 * (ch % 2) + 1)
                            cs = slice(ch * 512, (ch + 1) * 512)
                            P.mm(pr_, wbdv[:, ir, :], xcb[:, cs])
                            P.mm(pi_, wbdv[:, ii, :], xcb[:, cs])
                            P.act(R[:, cs], pr_, AF.Sigmoid, bias=lrub[:, ir:ir + 1])
                            P.act(IB[:, cs], pi_, AF.Sigmoid, bias=lrub[:, ii:ii + 1])
                        steps.append(sg)
                    qorder = list(range(QN)) if dr == 0 else list(range(QN - 1, -1, -1))
                    for q in qorder:
                        def s1(q=q, ci=ci):
                            sl = slice(q * QS, (q + 1) * QS)
                            P.tt(IB[:, sl], IB[:, sl], XC[:, sl], ALU.mult)
                            P.act(AB[:, sl], R[:, sl], AF.Exp, scale=cst[:, ci:ci + 1])
                        steps.append(s1)
                    for q in qorder:
                        def s2(q=q):
                            sl = slice(q * QS, (q + 1) * QS)
                            P.act(R[:, sl], AB[:, sl], AF.Square)
                        steps.append(s2)
                    for q in qorder:
                        def s3(q=q, dr=dr):
                            sl = slice(q * QS, (q + 1) * QS)
                            P.act(R[:, sl], R[:, sl], AF.Sqrt, scale=-1.0, bias=oneT[:, 0:1])
                            P.tt(IB[:, sl], IB[:, sl], R[:, sl], ALU.mult)
                            if dr == 0:
                                init = 0.0 if q == 0 else HB[:, q * QS - 1:q * QS]
                                rd = [AB[:, sl], IB[:, sl]] + ([] if q == 0 else [init])
                                P.op('vector', lambda e, sl=sl, init=init: e.tensor_tensor_scan(HB[:, sl], AB[:, sl], IB[:, sl], init, ALU.mult, ALU.add),
                                     reads=rd, writes=[HB[:, sl]])
                            else:
                                init = 0.0 if q == QN - 1 else R[:, (q + 1) * QS:(q + 1) * QS + 1]
                                rd = [AB[:, sl], IB[:, sl]] + ([] if q == QN - 1 else [init])
                                rs_ = slice((q + 1) * QS - 1, q * QS - 1 if q > 0 else None, -1)
                                P.op('vector', lambda e, rs_=rs_, init=init, R=R: e.tensor_tensor_scan(R[:, rs_], AB[:, rs_], IB[:, rs_], init, ALU.mult, ALU.add),
                                     reads=rd, writes=[R[:, sl]])
                                P.tt(HB[:, sl], HB[:, sl], R[:, sl], ALU.add)
                        steps.append(s3)
                for q in range(QN):
                    def g1(q=q):
                        sl = slice(q * QS, (q + 1) * QS)
                        P.act(R[:, sl], G[:, sl], AF.Square)
                        P.ts(R[:, sl], R[:, sl], 0.044715, 1.0, ALU.mult, ALU.add)
                        P.tt(R[:, sl], R[:, sl], G[:, sl], ALU.mult)
                    steps.append(g1)
                for q in range(QN):
                    def g2(q=q):
                        sl = slice(q * QS, (q + 1) * QS)
                        P.act(R[:, sl], R[:, sl], AF.Sigmoid, scale=1.5957691216057308)
                        P.tt(R[:, sl], R[:, sl], G[:, sl], ALU.mult)
                        P.tt(rnnb[:, sl], HB[:, sl], R[:, sl], ALU.mult)
                    steps.append(g2)

                def fin():
                    P.dma(rnn_d[ct], rnnb)
                    if debug:
                        P.copy(AB, rnnb)
                        P.dma(dbg['rnnT'][:, ct * S:(ct + 1) * S], AB)
                steps.append(fin)
                return steps

            for f_ in project_steps(0) + conv_steps(0):
                f_()
            for ct in range(4):
                ys = y_steps(ct)
                xs_ = (project_steps(ct + 1) + conv_steps(ct + 1)) if ct + 1 < 4 else []
                pos = {}
                if ct == 3 and phases >= 3:
                    for i_, f_ in enumerate(wa_prep_steps()):
                        pos.setdefault(6 + 3 * i_, []).append(f_)
                npj = NCH if xs_ else 0
                ppos_ = [1, 3, 6, 9, 11, 14, 17, 19]
                for i_ in range(npj):
                    pos.setdefault(ppos_[i_], []).append(xs_[i_])
                for i_, f_ in enumerate(xs_[npj:]):
                    pos.setdefault(23 + 4 * i_, []).append(f_)
                for i_, f_ in enumerate(ys):
                    f_()
                    for g_ in pos.get(i_, []):
                        g_()
            ar.release(m)

        if phases >= 3:
            attn_start = ar.mark()
            attnT = ar.bf16(4 * S)
            attnTv = attnT.rearrange("p (h t) -> p h t", h=4)
            m_attn = ar.mark()
            ar3 = ar.sub(attn_start, m_attn - attn_start)
            QT = ar.bf16(4 * S)
            KT = ar.bf16(4 * S)
            V = ar.bf16(32 * 512)
            QTv = QT.rearrange("p (h t) -> p h t", h=4)
            KTv = KT.rearrange("p (h t) -> p h t", h=4)
            Vv = V.rearrange("p (k e) -> p k e", k=32)
            m3 = ar.mark()
            assert m3 == attn_start + 32768, (m3, attn_start)
            wa = ar.bf16(8 * 1536)
            wav = wa.rearrange("p (f n) -> p f n", f=8)
            stg = [ar.f32(1536) for _ in range(2)]
            permb = ar.bf16(128)
            P.dma(stg[0][:, 0:128], perm_d)
            P.copy(permb, stg[0][:, 0:128])
            hc = [ar3.bf16(8 * 512) for _ in range(2)]
            rc = [ar3.f32(512) for _ in range(2)]
            rsn = [ar3.f32(512) for _ in range(2)]
            t1 = [ar3.f32(512) for _ in range(2)]
            t2 = [ar3.f32(512) for _ in range(2)]
            qb = [ar.bf16(512) for _ in range(2)]
            k = 0
            for ch in range(NCH):
                hcb = hc[ch % 2]
                hcv = hcb.rearrange("p (f t) -> p f t", f=8)
                P.dma(hcv, hT_d[:, :, ch * 512:(ch + 1) * 512].rearrange("f p t -> p f t"))
                P.dma(rc[ch % 2], ropec_d[:, ch * 512:(ch + 1) * 512])
                P.dma(rsn[ch % 2], ropes_d[:, ch * 512:(ch + 1) * 512])
                sl = slice(ch * 512, (ch + 1) * 512)
                for qk in range(2):
                    dstv = QTv if qk == 0 else KTv
                    for h in range(4):
                        pq = bank(2 * (k % 2))
                        pqs = bank(2 * (k % 2) + 1)
                        c0 = qk * 512 + h * 128
                        for fc in range(8):
                            P.mm(pq, wav[:, fc, c0:c0 + 128], hcv[:, fc, :], start=(fc == 0), stop=(fc == 7))
                        P.acopy(qb[k % 2], pq)
                        P.mm(pqs, permb, qb[k % 2])
                        P.op('vector', lambda e, o=t1[k % 2], a=pq, b_=rc[ch % 2]: e.tensor_tensor(o, a, b_, ALU.mult),
                             reads=[pq, rc[ch % 2], qb[k % 2]], writes=[t1[k % 2]])
                        P.tt(t2[k % 2], pqs, rsn[ch % 2], ALU.mult)
                        P.tt(dstv[:, h, sl], t1[k % 2], t2[k % 2], ALU.add)
                        k += 1
                for tt in range(4):
                    pv = bank(4 + (tt % 2))
                    for fc in range(8):
                        P.mm(pv, hcv[:, fc, tt * 128:(tt + 1) * 128], wav[:, fc, 1024:1536], start=(fc == 0), stop=(fc == 7))
                    P.op('scalar', lambda e, o=Vv[:, ch * 4 + tt, :], i=pv: e.copy(o, i), reads=[pv], writes=[Vv[:, ch * 4 + tt, :]])
            ar.release(m3)
            if debug:
                m = ar.mark()
                tmpf = ar.f32(4 * S)
                P.copy(tmpf, QT)
                P.dma(dbg['qT'], tmpf)
                P.copy(tmpf, KT)
                P.dma(dbg['kT'], tmpf)
                P.copy(tmpf, V)
                P.dma(dbg['v'], tmpf)
                ar.release(m)

        if phases >= 4:
            m4 = ar.mark()
            E = [ar.bf16(1024) for _ in range(3)]
            rz1 = ar.f32(512)
            rz2 = ar.f32(512)
            ob = ar.f32(512)
            tb = ar.f32(512)
            sqb = ar.bf16(512)
            msb = ar.f32(512)
            SC = 0.125
            blk = 0
            cst_f = [ar.f32(2048) for _ in range(3)]
            cst_b = [ar.bf16(2048) for _ in range(3)]
            zacc2 = [ar.f32(512) for _ in range(2)]
            o1s = [ar.f32(512) for _ in range(2)]
            o2s = [ar.f32(512) for _ in range(2)]
            z1s = [ar.f32(512) for _ in range(2)]
            zh = ar.bf16(512)
            zl = ar.bf16(512)

            def convert_steps(e_):
                srcs = (w_gate[e_].rearrange("(f p) n -> p f n", p=128), w_up[e_].rearrange("(f p) n -> p f n", p=128),
                        w_down[e_].rearrange("(j p) n -> p j n", p=128))
                steps = []
                for q_ in range(3):
                    a_ = 8 if q_ < 2 else 2

                    def st(q_=q_, a_=a_):
                        P.dma(cst_f[q_].rearrange("p (a n) -> p a n", a=a_), srcs[q_])
                        P.copy(cst_b[q_], cst_f[q_])
                        P.dma(wall_d[e_ * 128:(e_ + 1) * 128, q_ * 2048:(q_ + 1) * 2048], cst_b[q_])
                    steps.append(st)
                return steps

            def epilogue_steps(h, qc, pb_):
                qs = slice(qc * 512, (qc + 1) * 512)
                B7 = bank(7)
                za = zacc2[pb_]

                def e1():
                    P.copy(zh, za)

                def e2():
                    P.tt(zl, za, zh, ALU.subtract)

                def e3():
                    P.mm(B7, ones, zh, start=True, stop=False)
                    P.mm(B7, ones, zl, start=False, stop=True)

                def e4():
                    P.act(rz2, B7, AF.Ln)
                    P.act(rz2, rz2, AF.Exp, scale=-1.0)

                def e5():
                    P.act(rz1, z1s[pb_], AF.Ln)
                    P.act(rz1, rz1, AF.Exp, scale=-1.0)

                def e6():
                    P.tt(ob, o1s[pb_], rz1, ALU.mult)
                    P.tt(tb, o2s[pb_], rz2, ALU.mult)

                def e7():
                    P.stt(ob, tb, neglam[:, 0:1], ob, ALU.mult, ALU.add)

                def e8():
                    P.tt(sqb, ob, ob, ALU.mult)

                def e9():
                    P.mm(B7, ones, sqb)

                def e10():
                    P.act(msb, B7, AF.Ln, scale=1.0 / 128, bias=epsT[:, 0:1])
                    P.act(msb, msb, AF.Exp, scale=-0.5)

                def e11():
                    P.stt(attnTv[:, h, qs], ob, subg[:, 0:1], msb, ALU.mult, ALU.mult)
                return [e1, e2, e3, e4, e5, e6, e7, e8, e9, e10, e11]

            pending = []
            for h in range(4):
                for qc in range(NCH):
                    qs = slice(qc * 512, (qc + 1) * 512)
                    O1, O2, Z1 = bank(4), bank(5), bank(6)
                    pb_ = blk % 2
                    sched = {}
                    for i_, f_ in enumerate(pending):
                        sched.setdefault(1 + i_, []).append(f_)
                    for i_, f_ in enumerate(convert_steps(blk)):
                        sched.setdefault(14 + 6 * i_, []).append(f_)
                    pending = []

                    def qk(kt, h=h, qs=qs):
                        sb = 2 * (kt % 2)
                        ks = slice(kt * 128, (kt + 1) * 128)
                        P.mm(bank(sb), KTv[0:64, h, ks], QTv[0:64, h, qs], tile_position=(0, 0))
                        P.mm(bank(sb + 1), KTv[64:128, h, ks], QTv[64:128, h, qs], tile_position=(64, 0))

                    if blk == 0:
                        qk(0)
                    for kt in range(32):
                        if kt + 1 < 32:
                            qk(kt + 1)
                        elif blk + 1 < 4 * NCH:
                            nb = blk + 1
                            nh, nqc = divmod(nb, NCH)
                            qk(0, h=nh, qs=slice(nqc * 512, (nqc + 1) * 512))
                        sb = 2 * (kt % 2)
                        Eb = E[kt % 3]
                        P.act(Eb.rearrange("p (a n) -> p a n", a=2), PS[:, sb:sb + 2, :], AF.Exp, scale=SC)
                        st = (kt == 0)
                        sp = (kt == 31)
                        vs = Vv[:, kt, h * 128:(h + 1) * 128]
                        P.mm(O1, vs, Eb[:, 0:512], start=st, stop=sp)
                        P.mm(O2, vs, Eb[:, 512:1024], start=st, stop=sp)
                        P.mm(Z1, ones, Eb[:, 0:512], start=st, stop=sp)
                        if kt == 0:
                            P.copy(zacc2[pb_], Eb[:, 512:1024])
                        else:
                            P.tt(zacc2[pb_], zacc2[pb_], Eb[:, 512:1024], ALU.add)
                        for f_ in sched.get(kt, []):
                            f_()
                    P.copy(o1s[pb_], O1)
                    P.copy(o2s[pb_], O2)
                    P.copy(z1s[pb_], Z1)
                    pending = epilogue_steps(h, qc, pb_)
                    blk += 1
            for f_ in pending:
                f_()
            ar.release(m4)
            if debug:
                m = ar.mark()
                tmpf = ar.f32(4 * S)
                P.copy(tmpf, attnT)
                P.dma(dbg['attnT'], tmpf)
                ar.release(m)

        if phases >= 5:
            ar.release(m_attn)
            m5 = ar.mark()
            rnnT = ar.bf16(4 * S)
            rnnTv = rnnT.rearrange("p (c t) -> p c t", c=4)
            for ct in range(4):
                P.dma(rnnTv[:, ct, :], rnn_d[ct])
            wo = ar.bf16(8 * 1024)
            wov = wo.rearrange("p (f n) -> p f n", f=8)
            m_stg5 = ar.mark()
            stg = [ar.f32(1024) for _ in range(2)]
            for fc in range(8):
                P.dma(stg[fc % 2], w_out[fc * 128:(fc + 1) * 128, :])
                (P.copy if fc % 2 == 0 else P.acopy)(wov[:, fc, :], stg[fc % 2])
            ar.release(m_stg5)
            xt = [ar.f32(D) for _ in range(4)]
            x1t = [ar.f32(D) for _ in range(3)]

            def p5_stage(t):
                xb = xt[t % 4]
                P.dma(xb, x[t * 128:(t + 1) * 128, :])
                ts_ = slice(t * 128, (t + 1) * 128)
                for nch in range(2):
                    po = bank(6 + nch)
                    for fc in range(8):
                        lhs = attnTv[:, fc, ts_] if fc < 4 else rnnTv[:, fc - 4, ts_]
                        P.mm(po, lhs, wov[:, fc, nch * 512:(nch + 1) * 512], start=(fc == 0), stop=(fc == 7))
                    P.tt(x1t[t % 3][:, nch * 512:(nch + 1) * 512], po, xb[:, nch * 512:(nch + 1) * 512], ALU.add)
                P.dma(x1_d[t * 128:(t + 1) * 128, :], x1t[t % 3], eng='gpsimd')
                if debug:
                    P.dma(dbg['x1'][t * 128:(t + 1) * 128, :], x1t[t % 3])

            if phases < 6:
                for t in range(NT):
                    p5_stage(t)

        if phases >= 6:
            TS = 256
            NTL = 64
            NSLOT = NTL * TS
            BIG = 1.0e30
            fgrep = ar.f32(D)
            P.dma(fgrep, fgrep_d)
            slotA = ar.f32(32).bitcast(I32)
            slotB = ar.f32(32).bitcast(I32)
            wA = ar.f32(32)
            wB = ar.f32(32)
            EI = ar.f32(NTL).bitcast(I32)
            sm = ar.f32(64)
            mA = ar.mark()
            g2rep = ar.f32(D)
            P.dma(g2rep, g2rep_d)
            whi = ar.bf16(8 * 36)
            wlo = ar.bf16(8 * 36)
            whiv = whi.rearrange("p (f n) -> p f n", f=8)
            wlov = wlo.rearrange("p (f n) -> p f n", f=8)
            wrt = ar.f32(8 * 36)
            wrt2 = ar.f32(8 * 36)
            P.dma(wrt.rearrange("p (f n) -> p f n", f=8), w_rt.rearrange("(f p) n -> p f n", p=128))
            P.copy(whi, wrt)
            P.copy(wrt2, whi)
            P.tt(wlo, wrt, wrt2, ALU.subtract)
            umat = ar.bf16(128)
            tmpU = ar.f32(128)
            P.dma(tmpU, umat_d)
            P.copy(umat, tmpU)
            thr = ar.f32(NTL)
            P.dma(thr, thr_d)
            pidx = ar.f32(1)
            P.dma(pidx, pidx_d)
            hib = [ar.bf16(D) for _ in range(2)]
            LG = ar.f32(NT * 36)
            LGv = LG.rearrange("p (t n) -> p t n", t=NT)
            h2 = [ar.f32(D) for _ in range(2)]
            lo = [ar.bf16(D) for _ in range(2)]
            hiT = [ar.bf16(8 * 128) for _ in range(2)]
            loT = [ar.bf16(8 * 128) for _ in range(2)]
            junk = ar.f32(D)
            ssq = ar.f32(NT)
            rsq = ar.f32(NT)
            def a_s1(t):
                b = t % 2
                xin = x1t[t % 3]
                P.act(junk, xin, AF.Square, accum_out=ssq[:, t:t + 1])
                P.act(rsq[:, t:t + 1], ssq[:, t:t + 1], AF.Ln, scale=1.0 / D, bias=epsT[:, 0:1])
                P.act(rsq[:, t:t + 1], rsq[:, t:t + 1], AF.Exp, scale=-0.5)
                P.stt(h2[b], xin, rsq[:, t:t + 1], g2rep, ALU.mult, ALU.mult)
                hi = hib[b]
                P.op('scalar', lambda e, o=hi, i_=h2[b]: e.copy(o, i_), reads=[h2[b]], writes=[hi])
                P.tt(lo[b], h2[b], hi, ALU.subtract)
                P.dma(h2_d[t * 128:(t + 1) * 128, :], hi, eng='gpsimd')

            def a_s2(t):
                b = t % 2
                hi = hib[b]
                pb = bank_bf(2 * b)
                pl = bank_bf(2 * b + 1)
                for fc in range(8):
                    P.transpose(pb[:, fc * 128:(fc + 1) * 128], hi[:, fc * 128:(fc + 1) * 128], ident)
                for fc in range(8):
                    P.transpose(pl[:, fc * 128:(fc + 1) * 128], lo[b][:, fc * 128:(fc + 1) * 128], ident)
                P.copy(hiT[b], pb)
                P.op('scalar', lambda e, o=loT[b], i=pl: e.copy(o, i), reads=[pl], writes=[loT[b]])

            def a_s3(t):
                b = t % 2
                hv = hiT[b].rearrange("p (f t) -> p f t", f=8)
                lv = loT[b].rearrange("p (f t) -> p f t", f=8)
                plg = bank(4 + b)[:, 0:36]
                n = 0
                for fc in range(8):
                    for (lh, rh) in ((hv[:, fc, :], whiv[:, fc, :]), (hv[:, fc, :], wlov[:, fc, :]), (lv[:, fc, :], whiv[:, fc, :])):
                        P.mm(plg, lh, rh, start=(n == 0), stop=(n == 23))
                        n += 1
                P.copy(LGv[:, t, :], plg)

            pipeline(NT, [p5_stage, a_s1, a_s2, a_s3])
            if debug:
                P.dma(dbg['lg'], LG)
            GL = LGv[:, :, 0:4]
            EL = LGv[:, :, 4:36].rearrange("p t (g e) -> p t g e", g=4)
            T1 = ar.f32(NT * 32)
            T2 = ar.f32(NT * 32)
            MA = ar.f32(NT * 32)
            MS = ar.f32(NT * 32)
            ELM = ar.f32(NT * 32)
            v3 = lambda a: a.rearrange("p (t e) -> p t e", t=NT)
            v4 = lambda a: a.rearrange("p (t g e) -> p t g e", t=NT, g=4)
            g4 = ar.f32(NT * 4)
            g4b = ar.f32(NT * 4)
            g4v = g4.rearrange("p (t g) -> p t g", g=4)
            g4bv = g4b.rearrange("p (t g) -> p t g", g=4)
            gmax = ar.f32(NT)
            gtp = ar.f32(NT)
            v1 = ar.f32(NT)
            v2 = ar.f32(NT)
            d21 = ar.f32(NT)
            bc3 = lambda a, n: a.unsqueeze(2).to_broadcast([128, NT, n])
            red = lambda o, i, op: P.op('vector', lambda e: e.tensor_reduce(o, i, AX.X, op), reads=[i], writes=[o])
            red(gmax, GL, ALU.max)
            P.tt(g4v, GL, bc3(gmax, 4), ALU.subtract)
            P.act(g4b, g4, AF.Exp)
            red(gtp, g4bv, ALU.add)
            P.recip(gtp, gtp)
            P.tt(g4v, GL, bc3(gmax, 4), ALU.is_ge)
            P.ts(g4, g4, BIG, -BIG, ALU.mult, ALU.add)
            P.tt(v4(ELM), EL, g4v.unsqueeze(3).to_broadcast([128, NT, 4, 8]), ALU.add)
            red(v1, v3(ELM), ALU.max)
            P.tt(v3(MA), v3(ELM), bc3(v1, 32), ALU.is_ge)
            P.stt(T1, MA, -BIG, ELM, ALU.mult, ALU.add)
            red(v2, v3(T1), ALU.max)
            P.tt(v3(MS), v3(ELM), bc3(v2, 32), ALU.is_ge)
            P.tt(T2, MS, MA, ALU.subtract)
            P.tt(d21, v2, v1, ALU.subtract)
            P.act(d21, d21, AF.Exp)
            P.ts(T1[:, 0:NT], d21, 1.0, None, ALU.add)
            P.recip(T1[:, 0:NT], T1[:, 0:NT])
            P.tt(wA, T1[:, 0:NT], gtp, ALU.mult)
            P.tt(wB, wA, d21, ALU.mult)
            MSb = ar.bf16(NT * 32)
            P.copy(MSb, MS)
            MSbv = v3(MSb)
            ppos = PS[:, 0:2, :].rearrange("p a n -> p (a n)")
            pcs = PS[:, 2:4, :].rearrange("p a n -> p (a n)")
            for t in range(NT):
                P.mm(ppos[:, t * 32:(t + 1) * 32], umat, MSbv[:, t, :])
                P.mm(pcs[:, t * 32:(t + 1) * 32], ones, MSbv[:, t, :])
            CS = ar.f32(NT * 32)
            P.copy(CS, pcs)
            BASE = ar.f32((NT + 1) * 32)
            P.memset(BASE[:, 0:32], 0.0)
            for t in range(NT):
                P.tt(BASE[:, (t + 1) * 32:(t + 2) * 32], BASE[:, t * 32:(t + 1) * 32], CS[:, t * 32:(t + 1) * 32], ALU.add)
            ntot = BASE[:, NT * 32:(NT + 1) * 32]
            npad = ar.f32(32)
            P.ts(npad, ntot, 0.0, None, ALU.is_gt)
            for kk in range(1, S // TS):
                P.stt(npad, ntot, float(kk * TS), npad, ALU.is_gt, ALU.add)
            P.ts(npad, npad, float(TS), None, ALU.mult)
            onesf = ar.f32(32)
            P.memset(onesf, 1.0)
            endp = ar.f32(32)
            P.op('vector', lambda e: e.tensor_tensor_scan(endp, onesf, npad, 0.0, ALU.mult, ALU.add), reads=[onesf, npad], writes=[endp])
            startp = ar.f32(32)
            P.tt(startp, endp, npad, ALU.subtract)
            P.tt(T1, ppos, BASE[:, 0:NT * 32], ALU.add)
            P.tt(v3(T1), v3(T1), startp.unsqueeze(1).to_broadcast([128, NT, 32]), ALU.add)
            P.tt(ELM, T1, MA, ALU.mult)
            sf = ar.f32(NT)
            red(sf, v3(ELM), ALU.add)
            P.copy(slotA, sf)
            P.tt(ELM, T1, T2, ALU.mult)
            red(sf, v3(ELM), ALU.add)
            P.copy(slotB, sf)
            TE = ar.f32(NTL * 32)
            TEv = TE.rearrange("p (i e) -> p i e", i=NTL)
            P.tt(TEv, endp.unsqueeze(1).to_broadcast([128, NTL, 32]), thr.unsqueeze(2).to_broadcast([128, NTL, 32]), ALU.is_le)
            eif = ar.f32(NTL)
            red(eif, TEv, ALU.add)
            P.ts(eif, eif, 31.0, None, ALU.min)
            P.ts(eif, eif, 128.0, pidx[:, 0:1], ALU.mult, ALU.add)
            inval = ar.f32(NTL)
            P.ts(inval, thr, endp[:, 31:32], 1.0e6, ALU.is_ge, ALU.mult)
            P.tt(eif, eif, inval, ALU.add)
            P.copy(EI, eif)
            if debug:
                dtmp = ar.f32(192)
                P.copy(dtmp[:, 0:32], slotA)
                P.copy(dtmp[:, 32:64], slotB)
                P.copy(dtmp[:, 64:96], wA)
                P.copy(dtmp[:, 96:128], wB)
                P.copy(dtmp[:, 128:192], EI)
                P.dma(dbg['route'], dtmp)
            hst = [ar.bf16(D) for _ in range(3)]
            for t in range(NT if stop6 >= 2 else 0):
                hb_ = hst[t % 3]
                P.dma(hb_, h2_d[t * 128:(t + 1) * 128, :])
                for sl_ in (slotA, slotB):
                    P.op('gpsimd', lambda e, sl_=sl_, t=t, hb_=hb_: e.indirect_dma_start(
                        out=xs_d[:, :], out_offset=bass.IndirectOffsetOnAxis(ap=sl_[:, t:t + 1], axis=0),
                        in_=hb_, in_offset=None),
                        reads=[hb_, sl_[:, t:t + 1]], writes=[xs_d], is_dma=True, partial=True)
            ar.release(mA)
            mB = ar.mark()
            NWB = 3
            wB_ = [ar.bf16(6144) for _ in range(NWB)]
            xg = [ar.bf16(2 * D) for _ in range(3)]
            xgT = [ar.bf16(8 * TS) for _ in range(2)]
            sg = [ar.f32(TS) for _ in range(2)]
            hid = [ar.bf16(2 * TS) for _ in range(2)]
            ysb = [ar.bf16(2 * D) for _ in range(2)]

            def b_load(i):
                kw_ = dict(bounds_check=NE * 128 - 1, oob_is_err=False) if i >= 32 else {}
                wb = wB_[i % NWB]
                P.op('gpsimd', lambda e, wb=wb, i=i, kw_=kw_: e.indirect_dma_start(
                    out=wb, out_offset=None, in_=wall_d[:, :],
                    in_offset=bass.IndirectOffsetOnAxis(ap=EI[:, i:i + 1], axis=0), **kw_),
                    reads=[wall_d, EI[:, i:i + 1]], writes=[wb], is_dma=True)
                P.dma(xg[i % 3].rearrange("p (a n) -> p a n", a=2), xs_d[i * TS:(i + 1) * TS, :].rearrange("(a p) n -> p a n", p=128))

            def b_s1(i):
                xgv = xg[i % 3].rearrange("p (a n) -> p a n", a=2)
                xTv = xgT[i % 2].rearrange("p (f t) -> p f t", f=8)
                for a in range(2):
                    pt = bank_bf(a)
                    for fc in range(8):
                        P.transpose(pt[:, fc * 128:(fc + 1) * 128], xgv[:, a, fc * 128:(fc + 1) * 128], ident)
                    src = pt.rearrange("p (f t) -> p f t", f=8)
                    dst = xTv[:, :, a * 128:(a + 1) * 128]
                    if a == 0:
                        P.copy(dst, src)
                    else:
                        P.op('scalar', lambda e, o=dst, i_=src: e.copy(o, i_), reads=[src], writes=[dst])

            def b_s2(i):
                wb = wB_[i % NWB]
                xTv = xgT[i % 2].rearrange("p (f t) -> p f t", f=8)
                wgv = wb[:, 0:2048].rearrange("p (f n) -> p f n", f=8)
                wuv = wb[:, 2048:4096].rearrange("p (f n) -> p f n", f=8)
                hv = hid[i % 2].rearrange("p (j t) -> p j t", j=2)
                for jj in range(2):
                    pg = bank(2 + jj)[:, 0:TS]
                    pu = bank(4 + jj)[:, 0:TS]
                    for fc in range(8):
                        P.mm(pg, wgv[:, fc, jj * 128:(jj + 1) * 128], xTv[:, fc, :], start=(fc == 0), stop=(fc == 7))
                    for fc in range(8):
                        P.mm(pu, wuv[:, fc, jj * 128:(jj + 1) * 128], xTv[:, fc, :], start=(fc == 0), stop=(fc == 7))
                    P.act(sg[jj], pg, AF.Silu)
                    P.tt(hv[:, jj, :], sg[jj], pu, ALU.mult)

            def b_s3(i):
                wb = wB_[i % NWB]
                wdv = wb[:, 4096:6144].rearrange("p (j n) -> p j n", j=2)
                hv = hid[i % 2].rearrange("p (j t) -> p j t", j=2)
                yv = ysb[i % 2].rearrange("p (a n) -> p a n", a=2)
                k = 0
                for a in range(2):
                    for nch in range(2):
                        pd = bank(6 + (k % 2))
                        for jj in range(2):
                            P.mm(pd, hv[:, jj, a * 128:(a + 1) * 128], wdv[:, jj, nch * 512:(nch + 1) * 512], start=(jj == 0), stop=(jj == 1))
                        o_ = yv[:, a, nch * 512:(nch + 1) * 512]
                        if k % 2 == 0:
                            P.copy(o_, pd)
                        else:
                            P.op('scalar', lambda e, o=o_, i_=pd: e.copy(o, i_), reads=[pd], writes=[o_])
                        k += 1
                P.dma(ys_d[i * TS:(i + 1) * TS, :].rearrange("(a p) n -> p a n", p=128), yv)

            if stop6 >= 3:
                b_load(0)
                for it in range(NTL + 1):
                    if it + 1 < NTL:
                        b_load(it + 1)
                    if it >= 1:
                        b_s2(it - 1)
                    if it < NTL:
                        b_s1(it)
                    if it >= 1:
                        b_s3(it - 1)
            ar.release(mB)
            YA = [ar.bf16(D) for _ in range(3)]
            YB = [ar.bf16(D) for _ in range(3)]
            x1c = [ar.f32(D) for _ in range(3)]
            outb = [ar.f32(D) for _ in range(2)]
            junk2 = ar.f32(D)
            ss3 = ar.f32(NT)
            rs3 = ar.f32(NT)
            if stop6 < 4:
                zz = ar.f32(D)
                P.memset(zz, 0.0)
                P.dma(out[0:128, :], zz)

            def c_s1(t):
                b = t % 3
                P.dma(x1c[b], x1_d[t * 128:(t + 1) * 128, :])
                for (yy, sl_) in ((YA[b], slotA), (YB[b], slotB)):
                    P.op('gpsimd', lambda e, yy=yy, sl_=sl_, t=t: e.indirect_dma_start(
                        out=yy, out_offset=None, in_=ys_d[:, :],
                        in_offset=bass.IndirectOffsetOnAxis(ap=sl_[:, t:t + 1], axis=0)),
                        reads=[ys_d, sl_[:, t:t + 1]], writes=[yy], is_dma=True)

            def c_s2(t):
                b = t % 3
                P.stt(x1c[b], YA[b], wA[:, t:t + 1], x1c[b], ALU.mult, ALU.add)
                P.stt(x1c[b], YB[b], wB[:, t:t + 1], x1c[b], ALU.mult, ALU.add)
                P.act(junk2, x1c[b], AF.Square, accum_out=ss3[:, t:t + 1])
                P.act(rs3[:, t:t + 1], ss3[:, t:t + 1], AF.Ln, scale=1.0 / D, bias=epsT[:, 0:1])
                P.act(rs3[:, t:t + 1], rs3[:, t:t + 1], AF.Exp, scale=-0.5)

            def c_s3(t):
                b = t % 3
                P.stt(outb[t % 2], x1c[b], rs3[:, t:t + 1], fgrep, ALU.mult, ALU.mult)
                P.dma(out[t * 128:(t + 1) * 128, :], outb[t % 2])

            if stop6 >= 4:
                pipeline(NT, [c_s1, c_s2, c_s3])
        else:
            m = ar.mark()
            z = ar.f32(D)
            P.memset(z, 0.0)
            P.dma(out[0:128, :], z)
            ar.release(m)

        P.emit()
    return nc


def _rope_tables():
    pos = np.arange(S, dtype=np.float32)
    inv = (np.float32(500000.0) ** (-np.arange(0, 16, 2, dtype=np.float32) / np.float32(16))).astype(np.float32)
    ang = pos[:, None] * inv[None, :]
    cos = np.cos(ang).astype(np.float32)
    sin = np.sin(ang).astype(np.float32)
    C = np.ones((128, S), np.float32)
    Sn = np.zeros((128, S), np.float32)
    for mth in range(2):
        for d in range(16):
            p = mth * 64 + d
            C[p] = cos[:, d % 8]
            Sn[p] = -sin[:, d % 8] if d < 8 else sin[:, d % 8]
    return C, Sn


def _shared_inputs(norm1_g, w_in, lambda_q1, lambda_k1, lambda_q2, lambda_k2, subln_g, conv_w, conv_b,
                   lru_w_r, lru_b_r, lru_w_i, lru_b_i, lru_lambda, w_out, norm2_g, w_grp, w_exp,
                   w_gate, w_up, w_down, final_g):
    f = np.float32
    c = np.ascontiguousarray
    w_in0 = c(w_in[0], dtype=f)
    perm = np.arange(128)
    for sh in range(2):
        b0 = sh * 64
        perm[b0:b0 + 8] = np.arange(b0 + 8, b0 + 16)
        perm[b0 + 8:b0 + 16] = np.arange(b0, b0 + 8)
    permm = np.zeros((128, 128), f)
    permm[perm, np.arange(128)] = 1.0
    C, Sn = _rope_tables()
    lam4 = np.stack([lambda_q1[0], lambda_k1[0], lambda_q2[0], lambda_k2[0]], 0).astype(f)
    lam4 = c(np.broadcast_to(lam4[None], (128, 4, 64)))
    convw = c(conv_w[0].reshape(4, 4, 128).transpose(2, 1, 0), dtype=f)
    convb = c(conv_b[0].reshape(4, 128).T, dtype=f)
    wbd = np.zeros((128, 16, 128), f)
    lrub = np.zeros((128, 16), f)
    for gi, (wm, bm) in enumerate(((lru_w_r[0], lru_b_r[0]), (lru_w_i[0], lru_b_i[0]))):
        for dr in range(2):
            for ct in range(4):
                a = (gi * 2 + dr) * 4 + ct
                for bl in range(2):
                    wbd[bl * 64:(bl + 1) * 64, a, bl * 64:(bl + 1) * 64] = wm[dr, ct * 2 + bl]
                lrub[:, a] = bm[dr, ct * 128:(ct + 1) * 128]
    lrul = np.zeros((128, 8), f)
    for dr in range(2):
        for ct in range(4):
            lrul[:, dr * 4 + ct] = lru_lambda[0, dr, ct * 128:(ct + 1) * 128]
    rep = lambda v: c(np.broadcast_to(np.asarray(v, f).reshape(1, -1), (128, v.size)))
    return {
        "w_in": w_in0,
        "perm": permm,
        "g1rep": rep(norm1_g[0]),
        "g2rep": rep(norm2_g[0]),
        "fgrep": rep(final_g),
        "ropec": C,
        "ropes": Sn,
        "lam4": lam4,
        "subln": c(subln_g[0].reshape(128, 1), dtype=f),
        "convw": convw,
        "convb": convb,
        "wbd": wbd,
        "lrub": lrub,
        "lrul": lrul,
        "w_out": c(w_out[0], dtype=f),
        "w_rt": c(np.concatenate([w_grp[0], w_exp[0]], axis=1), dtype=f),
        "w_gate": c(w_gate[0], dtype=f),
        "w_up": c(w_up[0], dtype=f),
        "w_down": c(w_down[0], dtype=f),
        "ident": np.eye(128, dtype=f),
        "umat": np.triu(np.ones((128, 128), f), 1),
        "thr": c(np.broadcast_to((np.arange(64, dtype=f) * 256.0)[None, :], (128, 64))),
        "pidx": np.arange(128, dtype=f).reshape(128, 1),
    }


def kernel(x, **params):
    x = np.asarray(x, dtype=np.float32)
    params = {k: np.asarray(v, dtype=np.float32) for k, v in params.items()}
    shared = _shared_inputs(**params)
    nc = build_nc()
    in_maps = []
    for b in range(8):
        mp = dict(shared)
        mp["x"] = np.ascontiguousarray(x[b])
        in_maps.append(mp)
    res = run_bass_kernel_spmd(nc, in_maps, core_ids=list(range(8)))
    return np.stack([np.asarray(r["out"], dtype=np.float32) for r in res.results], axis=0)
```
